# Optimizing a Trainium2 kernel written in Bass

```python
import math
import jax, jax.numpy as jnp
from jax import lax
import numpy as np

D_MODEL = 1024
BATCH = 8
SEQ = 4096
DEPTH = 4

CHUNK = 64
N_EVEN = (DEPTH + 1) // 2
N_ODD = DEPTH // 2
EPS = 1e-6

POOL_WIDTH = D_MODEL // 2
POOL_WINDOWS = (2, 4, 8, 16)
POOL_N_GROUPS = len(POOL_WINDOWS)
POOL_GROUP = POOL_WIDTH // POOL_N_GROUPS
ATT_HEAD_DIM = 64
ATT_HEADS = (D_MODEL // 2) // ATT_HEAD_DIM
ATT_WIDTH = ATT_HEADS * ATT_HEAD_DIM
ATT_PREV_CHUNKS = 8
REL_CLIP = 256
MIX_IN = POOL_WIDTH + 3 * ATT_WIDTH
MIX_OUT = POOL_WIDTH + ATT_WIDTH

SSM_EXPAND = 2
SSM_D_INNER = SSM_EXPAND * D_MODEL
SSM_HEAD_DIM = 64
SSM_HEADS = SSM_D_INNER // SSM_HEAD_DIM
SSM_GROUPS = 8
SSM_HEADS_PER_GROUP = SSM_HEADS // SSM_GROUPS
SSM_D_STATE = 128
SSM_D_CONV = 4
SSM_CONV_DIM = SSM_D_INNER + 2 * SSM_GROUPS * SSM_D_STATE
SSM_IN = SSM_D_INNER + SSM_CONV_DIM + SSM_HEADS
DT_MIN = 0.001
DT_MAX = 0.1

MOE_GROUPS = 4
MOE_EXPERTS_PER_GROUP = 8
MOE_EXPERTS = MOE_GROUPS * MOE_EXPERTS_PER_GROUP
MOE_TOP_K = 2
MOE_D_FF = D_MODEL // 8

kernel_name = 'hybrid_pool_chunkattn_ssd_hmoe_adaln'


def rmsnorm(x, w):
    xf = x.astype(jnp.float32)
    y = xf * lax.rsqrt(jnp.mean(xf * xf, axis=-1, keepdims=True) + EPS)
    return (y * w.astype(jnp.float32)).astype(x.dtype)


def modulate(h, shift, scale):
    return h * (1 + scale[:, None, :]) + shift[:, None, :]


def pool_mixer(u, pool_w, pool_scale):
    b, s, _ = u.shape
    uf = u.astype(jnp.float32)
    cs = jnp.pad(jnp.cumsum(uf, axis=1), ((0, 0), (1, 0), (0, 0)))
    pos = jnp.arange(1, s + 1, dtype=jnp.float32)
    outs = []
    for g, w in enumerate(POOL_WINDOWS):
        lo, hi = g * POOL_GROUP, (g + 1) * POOL_GROUP
        upper = cs[:, 1:, lo:hi]
        lower = jnp.pad(cs[:, :s + 1 - w, lo:hi], ((0, 0), (w - 1, 0), (0, 0)))
        count = jnp.minimum(pos, w)[None, :, None]
        outs.append((upper - lower) / count - uf[:, :, lo:hi])
    pooled = jnp.stack(outs, axis=2).astype(u.dtype)
    y = jnp.einsum('bsgc,gcd->bsgd', pooled, pool_w).reshape(b, s, POOL_WIDTH)
    return y * pool_scale


def chunk_attention(q, k, v, rel_bias):
    b, s, _ = q.shape
    nc = s // CHUNK
    band = (ATT_PREV_CHUNKS + 1) * CHUNK
    q = q.reshape(b, nc, CHUNK, ATT_HEADS, ATT_HEAD_DIM)
    k = k.reshape(b, nc, CHUNK, ATT_HEADS, ATT_HEAD_DIM)
    v = v.reshape(b, nc, CHUNK, ATT_HEADS, ATT_HEAD_DIM)
    pad = ((0, 0), (ATT_PREV_CHUNKS, 0), (0, 0), (0, 0), (0, 0))
    kp = jnp.pad(k, pad)
    vp = jnp.pad(v, pad)
    k_band = jnp.concatenate([kp[:, j:j + nc] for j in range(ATT_PREV_CHUNKS + 1)], axis=2)
    v_band = jnp.concatenate([vp[:, j:j + nc] for j in range(ATT_PREV_CHUNKS + 1)], axis=2)
    q_off = jnp.arange(CHUNK)[:, None] + ATT_PREV_CHUNKS * CHUNK
    k_off = jnp.arange(band)[None, :]
    rel_idx = jnp.clip(q_off - k_off, -REL_CLIP, REL_CLIP) + REL_CLIP
    bias = rel_bias[:, rel_idx].astype(jnp.float32)
    key_chunk = jnp.arange(nc)[:, None] - ATT_PREV_CHUNKS + jnp.arange(band)[None, :] // CHUNK
    valid = key_chunk >= 0
    scores = jnp.einsum('bclhd,bcshd->bchls', q, k_band).astype(jnp.float32) * (ATT_HEAD_DIM ** -0.5)
    scores = scores + bias[None, None]
    scores = jnp.where(valid[None, :, None, None, :], scores, -1e30)
    p = jax.nn.softmax(scores, axis=-1).astype(v.dtype)
    o = jnp.einsum('bchls,bcshd->bclhd', p, v_band)
    return o.reshape(b, s, ATT_WIDTH)


def mamba2_mixer(h, w_in, conv_w, conv_b, dt_bias, A_log, D_skip, norm_w, w_out):
    b, s, _ = h.shape
    nc = s // CHUNK
    G, R, P, N = SSM_GROUPS, SSM_HEADS_PER_GROUP, SSM_HEAD_DIM, SSM_D_STATE
    f32 = jnp.float32
    zxbcdt = h @ w_in
    z, xbc, dt = jnp.split(zxbcdt, [SSM_D_INNER, SSM_D_INNER + SSM_CONV_DIM], axis=-1)
    xp = jnp.pad(xbc, ((0, 0), (SSM_D_CONV - 1, 0), (0, 0)))
    conv = xp[:, 0:s] * conv_w[0]
    for tap in range(1, SSM_D_CONV):
        conv = conv + xp[:, tap:tap + s] * conv_w[tap]
    xbc = jax.nn.silu(conv + conv_b)
    xs, Bm, Cm = jnp.split(xbc, [SSM_D_INNER, SSM_D_INNER + G * N], axis=-1)
    dt = jax.nn.softplus(dt.astype(f32) + dt_bias.astype(f32))
    A = -jnp.exp(A_log.astype(f32))
    a = (dt * A).reshape(b, nc, CHUNK, G, R)
    xs = xs.astype(f32).reshape(b, nc, CHUNK, G, R, P)
    x_dt = xs * dt.reshape(b, nc, CHUNK, G, R)[..., None]
    Bm = Bm.astype(f32).reshape(b, nc, CHUNK, G, N)
    Cm = Cm.astype(f32).reshape(b, nc, CHUNK, G, N)
    a_cs = jnp.moveaxis(jnp.cumsum(a, axis=2), 2, -1)
    tril = jnp.tril(jnp.ones((CHUNK, CHUNK), dtype=bool))
    seg = jnp.exp(jnp.where(tril, a_cs[..., :, None] - a_cs[..., None, :], -jnp.inf))
    cb = jnp.einsum('bclgn,bcsgn->bcgls', Cm, Bm)
    y_diag = jnp.einsum('bcgls,bcgrls,bcsgrp->bclgrp', cb, seg, x_dt)
    decay_states = jnp.exp(a_cs[..., -1:] - a_cs)
    states = jnp.einsum('bclgn,bcgrl,bclgrp->bcgrpn', Bm, decay_states, x_dt)
    chunk_decay = jnp.exp(a_cs[..., -1])

    def step(carry, inp):
        st, dec = inp
        return carry * dec[..., None, None] + st, carry

    init = jnp.zeros((b, G, R, P, N), f32)
    _, prev = lax.scan(step, init, (jnp.moveaxis(states, 1, 0), jnp.moveaxis(chunk_decay, 1, 0)))
    prev = jnp.moveaxis(prev, 0, 1)
    y_off = jnp.einsum('bclgn,bcgrpn,bcgrl->bclgrp', Cm, prev, jnp.exp(a_cs))
    y = y_diag + y_off + xs * D_skip.astype(f32).reshape(G, R)[:, :, None]
    y = y.reshape(b, s, SSM_D_INNER)
    g = (y * jax.nn.silu(z.astype(f32))).reshape(b, s, G, SSM_D_INNER // G)
    g = g * lax.rsqrt(jnp.mean(g * g, axis=-1, keepdims=True) + EPS)
    g = g.reshape(b, s, SSM_D_INNER) * norm_w.astype(f32)
    return g.astype(h.dtype) @ w_out


def hier_moe(h, w_group, b_group, w_expert, b_expert, w1, w3, w2):
    b, s, d = h.shape
    t = h.reshape(b * s, d)
    tf = t.astype(jnp.float32)
    g_prob = jax.nn.softmax(tf @ w_group.astype(jnp.float32) + b_group.astype(jnp.float32), axis=-1)
    g_val, g_idx = lax.top_k(g_prob, 1)
    e_logits = jnp.einsum('td,dge->tge', tf, w_expert.astype(jnp.float32)) + b_expert.astype(jnp.float32)
    e_logits = e_logits[jnp.arange(b * s), g_idx[:, 0]]
    e_val, e_idx = lax.top_k(jax.nn.softmax(e_logits, axis=-1), MOE_TOP_K)
    e_val = e_val / jnp.sum(e_val, axis=-1, keepdims=True)
    weights = g_val * e_val
    expert_id = g_idx * MOE_EXPERTS_PER_GROUP + e_idx
    combine = jnp.sum(jax.nn.one_hot(expert_id, MOE_EXPERTS, dtype=jnp.float32) * weights[..., None], axis=1)
    hid = jax.nn.silu(jnp.einsum('td,edf->tef', t, w1)) * jnp.einsum('td,edf->tef', t, w3)
    hid = hid * combine.astype(hid.dtype)[:, :, None]
    y = jnp.einsum('tef,efd->td', hid, w2)
    return y.reshape(b, s, d)


def _normal(key, shape, scale):
    return jax.random.normal(key, shape, jnp.float32) * scale


def setup_inputs(seed: int = 0) -> dict:
    key = jax.random.key(seed)
    ks = jax.random.split(key, 28)
    D = D_MODEL
    f32 = jnp.float32
    dt0 = jnp.exp(jax.random.uniform(ks[14], (N_ODD, SSM_HEADS), f32) * (math.log(DT_MAX) - math.log(DT_MIN)) + math.log(DT_MIN))
    dt0 = jnp.maximum(dt0, 1e-4)
    return {
        'x': _normal(ks[0], (BATCH, SEQ, D), 1.0),
        'c': _normal(ks[1], (BATCH, D), 1.0),
        'ada_w': _normal(ks[2], (DEPTH, D, 6 * D), 0.5 * D ** -0.5),
        'ada_b': _normal(ks[3], (DEPTH, 6 * D), 0.02),
        'norm1_w': 1.0 + _normal(ks[4], (DEPTH, D), 0.05),
        'norm2_w': 1.0 + _normal(ks[5], (DEPTH, D), 0.05),
        'mix_w_in': _normal(ks[6], (N_EVEN, D, MIX_IN), D ** -0.5),
        'pool_w': _normal(ks[7], (N_EVEN, POOL_N_GROUPS, POOL_GROUP, POOL_GROUP), POOL_GROUP ** -0.5),
        'pool_scale': 1.0 + _normal(ks[8], (N_EVEN, POOL_WIDTH), 0.05),
        'rel_bias': _normal(ks[9], (N_EVEN, ATT_HEADS, 2 * REL_CLIP + 1), 0.2),
        'mix_w_out': _normal(ks[10], (N_EVEN, MIX_OUT, D), MIX_OUT ** -0.5),
        'ssm_w_in': _normal(ks[11], (N_ODD, D, SSM_IN), D ** -0.5),
        'ssm_conv_w': _normal(ks[12], (N_ODD, SSM_D_CONV, SSM_CONV_DIM), SSM_D_CONV ** -0.5),
        'ssm_conv_b': _normal(ks[13], (N_ODD, SSM_CONV_DIM), 0.02),
        'ssm_dt_bias': dt0 + jnp.log(-jnp.expm1(-dt0)),
        'ssm_A_log': jnp.log(jax.random.uniform(ks[15], (N_ODD, SSM_HEADS), f32, 1.0, 16.0)),
        'ssm_D': 1.0 + _normal(ks[16], (N_ODD, SSM_HEADS), 0.1),
        'ssm_norm_w': 1.0 + _normal(ks[17], (N_ODD, SSM_D_INNER), 0.05),
        'ssm_w_out': _normal(ks[18], (N_ODD, SSM_D_INNER, D), SSM_D_INNER ** -0.5),
        'moe_w_group': _normal(ks[19], (DEPTH, D, MOE_GROUPS), D ** -0.5),
        'moe_b_group': _normal(ks[20], (DEPTH, MOE_GROUPS), 0.01),
        'moe_w_expert': _normal(ks[21], (DEPTH, D, MOE_GROUPS, MOE_EXPERTS_PER_GROUP), D ** -0.5),
        'moe_b_expert': _normal(ks[22], (DEPTH, MOE_GROUPS, MOE_EXPERTS_PER_GROUP), 0.01),
        'moe_w1': _normal(ks[23], (DEPTH, MOE_EXPERTS, D, MOE_D_FF), D ** -0.5),
        'moe_w3': _normal(ks[24], (DEPTH, MOE_EXPERTS, D, MOE_D_FF), D ** -0.5),
        'moe_w2': _normal(ks[25], (DEPTH, MOE_EXPERTS, MOE_D_FF, D), MOE_D_FF ** -0.5),
        'final_norm_w': 1.0 + _normal(ks[26], (D,), 0.05),
    }


def reference(x, c, ada_w, ada_b, norm1_w, norm2_w, mix_w_in, pool_w, pool_scale, rel_bias, mix_w_out,
              ssm_w_in, ssm_conv_w, ssm_conv_b, ssm_dt_bias, ssm_A_log, ssm_D, ssm_norm_w, ssm_w_out,
              moe_w_group, moe_b_group, moe_w_expert, moe_b_expert, moe_w1, moe_w3, moe_w2, final_norm_w):
    c_act = jax.nn.silu(c)
    for layer in range(DEPTH):
        mod = c_act @ ada_w[layer] + ada_b[layer]
        shift1, scale1, gate1, shift2, scale2, gate2 = jnp.split(mod, 6, axis=-1)
        h = modulate(rmsnorm(x, norm1_w[layer]), shift1, scale1)
        i = layer // 2
        if layer % 2 == 0:
            proj = h @ mix_w_in[i]
            u, q, k, v = jnp.split(proj, [POOL_WIDTH, POOL_WIDTH + ATT_WIDTH, POOL_WIDTH + 2 * ATT_WIDTH], axis=-1)
            a_out = pool_mixer(u, pool_w[i], pool_scale[i])
            b_out = chunk_attention(q, k, v, rel_bias[i])
            mix = jnp.concatenate([a_out, b_out], axis=-1) @ mix_w_out[i]
        else:
            mix = mamba2_mixer(h, ssm_w_in[i], ssm_conv_w[i], ssm_conv_b[i], ssm_dt_bias[i], ssm_A_log[i],
                               ssm_D[i], ssm_norm_w[i], ssm_w_out[i])
        x = x + gate1[:, None, :] * mix
        h2 = modulate(rmsnorm(x, norm2_w[layer]), shift2, scale2)
        ffn = hier_moe(h2, moe_w_group[layer], moe_b_group[layer], moe_w_expert[layer], moe_b_expert[layer],
                       moe_w1[layer], moe_w3[layer], moe_w2[layer])
        x = x + gate2[:, None, :] * ffn
    return rmsnorm(x, final_norm_w)
```

```python
import numpy as np
import concourse.bass as bass
import concourse.mybir as mybir
from concourse.bass_utils import run_bass_kernel_spmd

F32 = mybir.dt.float32
BF16 = mybir.dt.bfloat16
AF = mybir.ActivationFunctionType
ALU = mybir.AluOpType
AX = mybir.AxisListType

ENGS = ("sync", "act", "pool", "pe", "dve")
D = 1024
S = 4096
NBLK = 32
NT = 8
EPS = 1e-6


class Buf:
    __slots__ = ("name", "w", "r")

    def __init__(self, name=""):
        self.name = name
        self.w = None
        self.r = {}


class Prog:
    def __init__(self, nc, same_eng_sync=("pool", "act", "dve")):
        self.nc = nc
        self.ops = {e: [] for e in ENGS}
        self.dma_cnt = {}
        self.same_eng_sync = set(same_eng_sync)
        self.last_real = {e: None for e in ENGS}

    def _deps_for(self, reads, writes):
        deps = set()
        for b in reads:
            if b.w is not None:
                deps.add(b.w)
        for b in writes:
            if b.w is not None:
                deps.add(b.w)
            for t in b.r.values():
                deps.add(t)
        return deps

    def _commit(self, tok, reads, writes):
        for b in writes:
            b.w = tok
            b.r = {}
        for b in reads:
            if b in writes:
                continue
            k = tok[0] if tok[0] != "dma" else ("dma", tok[1])
            b.r[k] = tok

    def op(self, eng, fn, reads=(), writes=()):
        deps = self._deps_for(reads, writes)
        idx = len(self.ops[eng])
        tok = (eng, idx)
        self.ops[eng].append([fn, deps, "op", None])
        self.last_real[eng] = tok
        self._commit(tok, reads, writes)
        return tok

    def dma(self, eng, fn, key, reads=(), writes=()):
        deps = self._deps_for(reads, writes)
        deps = set(d for d in deps if not (d[0] == "dma" and d[1] == key))
        c = self.dma_cnt.get(key, 0) + 1
        self.dma_cnt[key] = c
        tok = ("dma", key, c)
        self.ops[eng].append([fn, deps, "dma", key])
        self._commit(tok, reads, writes)
        return tok

    def barrier(self):
        toks = set(t for t in self.last_real.values() if t is not None)
        toks |= set(("dma", k, c) for k, c in self.dma_cnt.items())
        for e in ENGS:
            self.ops[e].append([None, set(toks), "op", None])

    def emit(self):
        nc = self.nc
        need = {e: set() for e in ENGS}
        for e in ENGS:
            for (fn, deps, kind, key) in self.ops[e]:
                for d in deps:
                    if d[0] != "dma":
                        if d[0] == e and e not in self.same_eng_sync:
                            continue
                        need[d[0]].add(d[1])
        msval = {}
        for e in ENGS:
            c = 0
            m = {}
            for i in sorted(need[e]):
                c += 1
                m[i] = c
            msval[e] = m
        sem = {e: nc.alloc_semaphore("s_" + e) for e in ENGS}
        dsem = {k: nc.alloc_semaphore("d_%d" % i) for i, k in enumerate(self.dma_cnt)}
        engobj = {"sync": "sync", "act": "scalar", "pool": "gpsimd", "pe": "tensor", "dve": "vector"}

        def run(e):
            def body(eng):
                seen = {}
                for i, (fn, deps, kind, key) in enumerate(self.ops[e]):
                    for d in sorted(deps, key=str):
                        if d[0] == "dma":
                            sk = ("dma", d[1]); val = 16 * d[2]; s = dsem[d[1]]
                        else:
                            if d[0] == e and e not in self.same_eng_sync:
                                continue
                            sk = d[0]; val = msval[d[0]][d[1]]; s = sem[d[0]]
                        if seen.get(sk, 0) >= val:
                            continue
                        seen[sk] = val
                        eng.wait_ge(s, val)
                    if fn is None:
                        continue
                    ins = fn(eng)
                    if kind == "dma":
                        ins.then_inc(dsem[key], 16)
                    elif i in msval[e]:
                        ins.then_inc(sem[e], 1)
            return body

        with nc.Block() as block:
            for e in ENGS:
                if self.ops[e]:
                    getattr(block, engobj[e])(run(e))


class Ring:
    def __init__(self, aps, name):
        self.slots = [(ap, Buf(name + str(i))) for i, ap in enumerate(aps)]
        self.i = 0

    def next(self):
        s = self.slots[self.i % len(self.slots)]
        self.i += 1
        return s


INPUT_SHAPES = [
    ("x", [S, D]), ("c", [1, D]), ("ada_w", [4, D, 6 * D]), ("ada_b", [4, 6 * D]),
    ("norm1_w", [4, D]), ("norm2_w", [4, D]), ("mix_w_in", [2, D, 2048]), ("pool_w", [2, 4, 128, 128]),
    ("pool_scale", [2, 512]), ("rel_bias", [2, 8, 513]), ("mix_w_out", [2, 1024, D]),
    ("ssm_w_in", [2, D, 6176]), ("ssm_conv_w", [2, 4, 4096]), ("ssm_conv_b", [2, 4096]),
    ("ssm_dt_bias", [2, 32]), ("ssm_A_log", [2, 32]), ("ssm_D", [2, 32]), ("ssm_norm_w", [2, 2048]),
    ("ssm_w_out", [2, 2048, D]), ("moe_w_group", [4, D, 4]), ("moe_b_group", [4, 4]),
    ("moe_w_expert", [4, D, 4, 8]), ("moe_b_expert", [4, 4, 8]), ("moe_w1", [4, 32, D, 128]),
    ("moe_w3", [4, 32, D, 128]), ("moe_w2", [4, 32, 128, D]), ("final_norm_w", [1, D]),
]


def build(cfg):
    layers = cfg.get("layers", [0, 1, 2, 3])
    do_final = cfg.get("final", True)
    do_mixer = cfg.get("mixer", True)
    do_moe = cfg.get("moe", True)
    NTc = cfg.get("ntiles", NT)
    nc = bass.Bass("TRN2", target_bir_lowering=False)
    P = Prog(nc)
    di = {}
    for name, shp in INPUT_SHAPES:
        di[name] = nc.dram_tensor(name, list(shp), F32, kind="ExternalInput").ap()
    y_d = nc.dram_tensor("y", [S, D], F32, kind="ExternalOutput").ap()
    xr = nc.dram_tensor("xr", [S, D], F32).ap()
    rbx = nc.dram_tensor("rbx", [8, 1024], F32).ap()
    b_rbx = Buf("rbx")
    xr_b = [Buf("xr%d" % i) for i in range(NBLK)]
    y_b = [Buf("y%d" % i) for i in range(NBLK)]

    def act(out, in_, func, r, w, **kw):
        P.op("act", lambda e: e.activation(out=out, in_=in_, func=func, **kw), r, w)

    def tt(eng, out, in0, in1, op, r, w):
        P.op(eng, lambda e: e.tensor_tensor(out=out, in0=in0, in1=in1, op=op), r, w)

    def ts(eng, out, in0, s1, op0, r, w, s2=None, op1=None):
        if op1 is None:
            P.op(eng, lambda e: e.tensor_scalar(out=out, in0=in0, scalar1=s1, scalar2=None, op0=op0), r, w)
        else:
            P.op(eng, lambda e: e.tensor_scalar(out=out, in0=in0, scalar1=s1, scalar2=s2, op0=op0, op1=op1), r, w)

    def stt(eng, out, in0, scalar, in1, op0, op1, r, w):
        P.op(eng, lambda e: e.scalar_tensor_tensor(out=out, in0=in0, scalar=scalar, in1=in1, op0=op0, op1=op1), r, w)

    def mm(out, lhsT, rhs, start, stop, r, w):
        P.op("pe", lambda e: e.matmul(out, lhsT, rhs, start=start, stop=stop), r, w)

    def trn(out, in_, r, w):
        P.op("pe", lambda e: e.transpose(out, in_, ident_bf[:]), r, w)

    def cp(eng, out, in_, r, w):
        P.op(eng, lambda e: e.tensor_copy(out=out, in_=in_), r, w)

    def dma(q, out, in_, key, r, w, **kw):
        P.dma(q, lambda e: e.dma_start(out=out, in_=in_, **kw), key, r, w)

    def memset(eng, ap, val, w):
        P.op(eng, lambda e: e.memset(ap, val), [], w)

    def asel(out, in_, pattern, cmp, fill, base, cm, r, w):
        P.op("pool", lambda e: e.affine_select(out=out, in_=in_, pattern=pattern, compare_op=cmp, fill=fill,
                                               base=base, channel_multiplier=cm), r, w)

    pb = [nc.alloc_psum_tensor("pb%d" % i, [128, 512], F32) for i in range(3)]
    pS = nc.alloc_psum_tensor("pS", [128, 1024], F32)
    pc = [nc.alloc_psum_tensor("pc%d" % i, [128, 512], F32) for i in range(3)]
    b_pb = [Buf("pb%d" % i) for i in range(3)]
    b_pS = [Buf("pS0"), Buf("pS1")]
    b_pc = [Buf("pc%d" % i) for i in range(3)]

    def sb(name, shape, dt):
        return nc.alloc_sbuf_tensor(name, list(shape), dt)

    b_const = Buf("const")
    ident_f = sb("ident_f", [128, 128], F32)
    ident_bf = sb("ident_bf", [128, 128], BF16)
    Jm = sb("Jm", [128, 128], F32)
    ones_f = sb("ones_f", [128, 128], F32)
    Tm = sb("Tm", [128, 128], F32)
    Tm_bf = sb("Tm_bf", [128, 128], BF16)
    negm_f = sb("negm_f", [128, 128], F32)
    negm = sb("negm", [128, 128], BF16)
    SelE = sb("SelE", [32, 32, 128], BF16)
    mod_bc = sb("mod_bc", [128, 6 * D], F32)
    cactB = sb("cactB", [128, 8, 128], F32)
    cT = sb("cT", [128, 8], F32)
    b_mod = Buf("mod")

    memset("pool", ident_f[:], 1.0, [b_const])
    asel(ident_f[:], ident_f[:], [[-1, 128]], ALU.is_equal, 0.0, 0, 1, [b_const], [b_const])
    cp("dve", ident_bf[:], ident_f[:], [b_const], [b_const])
    memset("pool", Jm[:], 1.0, [b_const])
    asel(Jm[:], Jm[:], [[1, 128]], ALU.is_equal, 0.0, -127, 1, [b_const], [b_const])
    memset("pool", ones_f[:], 1.0, [b_const])
    memset("pool", Tm[:], 1.0, [b_const])
    asel(Tm[:], Tm[:], [[1, 128]], ALU.is_ge, 0.0, 0, -1, [b_const], [b_const])
    cp("dve", Tm_bf[:], Tm[:], [b_const], [b_const])
    memset("pool", negm_f[:], 0.0, [b_const])
    asel(negm_f[:], negm_f[:], [[1, 128]], ALU.is_ge, -30000.0, 0, -1, [b_const], [b_const])
    cp("dve", negm[:], negm_f[:], [b_const], [b_const])
    memset("pool", SelE[:], 1.0, [b_const])
    asel(SelE[:], SelE[:], [[-1, 32], [0, 128]], ALU.is_equal, 0.0, 0, 1, [b_const], [b_const])

    dma("sync", cT[:], di["c"][0, :].rearrange("(k p) -> p k", p=128), "cT", [], [b_const],
        allow_slow_non_contiguous=True)
    act(cT[:], cT[:], AF.Silu, [b_const], [b_const])
    for k in range(8):
        cp("dve", cactB[:, k, :], cT[:, k:k + 1].to_broadcast([128, 128]), [b_const], [b_const])

    w1s_ = [nc.dram_tensor("w1s%d" % i, [32, 128, 1024], BF16).ap() for i in range(2)]
    w3s_ = [nc.dram_tensor("w3s%d" % i, [32, 128, 1024], BF16).ap() for i in range(2)]
    w2s_ = [nc.dram_tensor("w2s%d" % i, [4, 128, 32, 256], BF16).ap() for i in range(2)]
    wins = nc.dram_tensor("wins", [48, 128, 1024], BF16).ap()
    b_w1s_ = [[Buf("w1s%d" % e) for e in range(32)] for _ in range(2)]
    b_w3s_ = [[Buf("w3s%d" % e) for e in range(32)] for _ in range(2)]
    b_w2s_ = [[Buf("w2s%d" % q) for q in range(4)] for _ in range(2)]
    b_wins = [Buf("wins%d" % m) for m in range(48)]
    NSTG = 3
    stg = Ring([sb("stg%d" % i, [128, 1024], BF16)[:] for i in range(NSTG)], "stg")
    bg = {"steps": [], "queued": set()}

    def _mk_pre(src_ap, src_is_kpf, dst_ap, dst_bufs):
        slot = {}

        def L():
            s_, bs_ = stg.next()
            slot["s"] = (s_, bs_, (stg.i - 1) % NSTG)
            if src_is_kpf:
                dma("pool", s_.rearrange("p (k f) -> p k f", k=8), src_ap, "stgL%d" % slot["s"][2], [], [bs_])
            else:
                dma("pool", s_, src_ap, "stgL%d" % slot["s"][2], [], [bs_])

        def S_():
            s_, bs_, i = slot["s"]
            if src_is_kpf:
                dma("pool", dst_ap, s_, "stgS%d" % i, [bs_], dst_bufs)
            else:
                dma("pool", dst_ap, s_.rearrange("p (q d) -> p q d", q=4), "stgS%d" % i, [bs_], dst_bufs)
        return L, S_

    def queue_precast(kind, l):
        if (kind, l) in bg["queued"]:
            return
        bg["queued"].add((kind, l))
        pairs = []
        if kind == "moe":
            w1s, w3s, w2s = w1s_[l % 2], w3s_[l % 2], w2s_[l % 2]
            b_w1s, b_w3s, b_w2s = b_w1s_[l % 2], b_w3s_[l % 2], b_w2s_[l % 2]
            for e in range(32):
                pairs.append(_mk_pre(di["moe_w1"][l, e].rearrange("(k p) f -> p k f", p=128), True, w1s[e], [b_w1s[e]]))
                pairs.append(_mk_pre(di["moe_w3"][l, e].rearrange("(k p) f -> p k f", p=128), True, w3s[e], [b_w3s[e]]))
            for e in range(32):
                pairs.append(_mk_pre(di["moe_w2"][l, e], False, w2s[:, :, e, :].rearrange("q f d -> f q d"), b_w2s))
        else:
            i_ = l // 2
            for m in range(48):
                pairs.append(_mk_pre(di["ssm_w_in"][i_, :, m * 128:(m + 1) * 128].rearrange("(k p) f -> p k f", p=128),
                                     True, wins[m], [b_wins[m]]))
        n = len(pairs)
        for i in range(n + 1):
            if i < n:
                bg["steps"].append(pairs[i][0])
            if i >= 1:
                bg["steps"].append(pairs[i - 1][1])

    def bg_pump(n):
        while n > 0 and bg["steps"]:
            bg["steps"].pop(0)()
            n -= 1

    def bg_flush():
        bg_pump(1 << 30)

    AW = cfg.get("arena_words", 41616)
    arena = sb("arena", [128, AW], F32)
    ar = {"off": 0}

    def carve(shape, dt):
        n = 1
        for s_ in shape[1:]:
            n *= s_
        words = (n * (4 if dt == F32 else 2) + 3) // 4
        o = ar["off"]
        assert o + words <= AW, ("arena overflow", o, words, AW)
        ar["off"] = o + words
        v = arena[0:shape[0], o:o + words]
        if dt != F32:
            v = v.bitcast(dt)
        if len(shape) == 3:
            v = v.rearrange("p (a b) -> p a b", a=shape[1])
        elif len(shape) == 4:
            v = v.rearrange("p (a b c) -> p a b c", a=shape[1], b=shape[2])
        elif len(shape) == 5:
            v = v.rearrange("p (a b c d) -> p a b c d", a=shape[1], b=shape[2], c=shape[3])
        return v

    def new_phase():
        P.barrier()
        ar["off"] = 0

    SH1, A1, G1, SH2, A2, G2 = [slice(i * D, (i + 1) * D) for i in range(6)]

    def ada_phase(l):
        new_phase()
        ring = Ring([carve([128, 8, 512], F32) for _ in range(2)], "adaw")
        tmpw = carve([128, D], F32)
        b_tmpw = Buf("tmpw")
        dma("sync", mod_bc[:], di["ada_b"][l, :].partition_broadcast(128), "modb", [], [b_mod])
        for n in range(12):
            sl, bsl = ring.next()
            dma("sync", sl, di["ada_w"][l, :, n * 512:(n + 1) * 512].rearrange("(k p) n -> p k n", p=128),
                "adaw%d" % (n % 2), [], [bsl])
            for k in range(8):
                mm(pb[n % 2][:], cactB[:, k, :], sl[:, k, :], k == 0, k == 7, [bsl, b_const], [b_pb[n % 2]])
            tt("dve", mod_bc[:, n * 512:(n + 1) * 512], pb[n % 2][:], mod_bc[:, n * 512:(n + 1) * 512], ALU.add,
               [b_pb[n % 2], b_mod], [b_mod])
        for (nm, sl_) in (("norm1_w", A1), ("norm2_w", A2)):
            dma("sync", tmpw, di[nm][l, :].partition_broadcast(128), "tmpw", [], [b_tmpw])
            stt("dve", mod_bc[:, sl_], mod_bc[:, sl_], 1.0, tmpw, ALU.add, ALU.mult, [b_mod, b_tmpw], [b_mod])

    def make_norm(src_rows, src_bufs, Asl, Ssl, lean=False):
        st = {}
        st["xin"] = Ring([carve([128, D], F32) for _ in range(2)], "xin")
        st["hf"] = None if lean else Ring([carve([128, D], F32)], "hf")
        st["hb"] = Ring([carve([128, D], BF16) for _ in range(2)], "hb")
        st["junk"] = (carve([128, D], BF16), Buf("junk"))
        st["sm"] = Ring([carve([128, 4], F32) for _ in range(2)], "nsm")
        st["cnt"] = 0

        def norm_block(blk, hT, hT_buf, col0):
            xin, bx = st["xin"].next()
            hb, bhb = st["hb"].next()
            hf, bhf = (hb, bhb) if lean else st["hf"].next()
            junk, bj = st["junk"]
            sm, bsm = st["sm"].next()
            pbank = pc[1 + st["cnt"] % 2]
            bbank = b_pc[1 + st["cnt"] % 2]
            st["cnt"] += 1
            dma("sync", xin, src_rows(blk), "xin%d" % ((st["xin"].i - 1) % 2), [src_bufs[blk]], [bx])
            memset("dve", sm[:, 0:1], 0.0, [bsm])
            act(junk, xin, AF.Square, [bx, bsm], [bj, bsm], accum_out=sm[:, 0:1])
            act(sm[:, 1:2], sm[:, 0:1], AF.Ln, [bsm], [bsm], bias=EPS, scale=1.0 / D)
            act(sm[:, 2:3], sm[:, 1:2], AF.Exp, [bsm], [bsm], scale=-0.5)
            stt("dve", hf, xin, sm[:, 2:3], mod_bc[:, Asl], ALU.mult, ALU.mult, [bx, bsm, b_mod], [bhf])
            tt("dve", hb, hf, mod_bc[:, Ssl], ALU.add, [bhf, b_mod] if not lean else [bhb, b_mod], [bhb])
            pv = pbank[:].bitcast(BF16)
            for k in range(8):
                P.op("pe", (lambda k_: lambda e: e.transpose(pv[:, k_ * 128:(k_ + 1) * 128],
                                                              hb[:, k_ * 128:(k_ + 1) * 128], ident_bf[:]))(k),
                     [bhb, b_const], [bbank])
            act(hT[:, :, col0:col0 + 128], pv.rearrange("p (k t) -> p k t", k=8), AF.Copy, [bbank], [hT_buf])
        return norm_block

    def moe_phase(l, src_rows, src_bufs):
        queue_precast("moe", l)
        bg_flush()
        new_phase()
        w1s, w3s, w2s = w1s_[l % 2], w3s_[l % 2], w2s_[l % 2]
        b_w1s, b_w3s, b_w2s = b_w1s_[l % 2], b_w3s_[l % 2], b_w2s_[l % 2]
        if l + 1 in layers and (l + 1) % 2 == 1:
            if do_mixer:
                queue_precast("ssd", l + 1)
            if do_moe:
                queue_precast("moe", l + 1)
        per_tile = (len(bg["steps"]) + NTc - 1) // max(NTc, 1)
        hTs = [carve([128, 8, 512], BF16) for _ in range(2)]
        hTbs = [[Buf("hT%d_%d" % (i, j)) for j in range(4)] for i in range(2)]
        norm_block = make_norm(src_rows, src_bufs, A2, SH2)
        wr = carve([128, 8, 36], BF16)
        b_wr = Buf("wr")
        rb_bc = carve([128, 36], F32)
        hid = carve([128, 32, 512], BF16)
        hid_b = [Buf("hid%d" % e) for e in range(32)]
        w2r = Ring([carve([128, 32, 256], BF16) for _ in range(3)], "w2q")
        w13 = Ring([carve([128, 2, 8, 128], BF16) for _ in range(3)], "w13")
        sTr = Ring([carve([128, 512], BF16) for _ in range(2)], "sT")
        tTr = Ring([carve([128, 512], BF16) for _ in range(2)], "tT")
        combT = Ring([carve([32, 512], BF16) for _ in range(2)], "combT")
        xout = carve([128, 4, D], F32)
        xout_b = [Buf("xout%d" % j) for j in range(4)]
        tmpq = Ring([carve([128, 256], F32) for _ in range(2)], "tmpq")
        rs = Ring([carve([128, 160], F32) for _ in range(2)], "rs")
        combb = Ring([carve([128, 32], BF16) for _ in range(2)], "combb")

        dma("pool", wr[:, :, 0:4], di["moe_w_group"][l].rearrange("(k p) g -> p k g", p=128), "wr", [], [b_wr])
        dma("pool", wr[:, :, 4:36], di["moe_w_expert"][l].rearrange("(k p) g e -> p k (g e)", p=128), "wr", [], [b_wr])
        dma("sync", rb_bc[:, 0:4], di["moe_b_group"][l, :].partition_broadcast(128), "rbbc", [], [b_wr])
        dma("sync", rb_bc[:, 4:36], di["moe_b_expert"][l].rearrange("g e -> (g e)").partition_broadcast(128),
            "rbbc", [], [b_wr])

        def norm_router(t):
            hT, hTb = hTs[t % 2], hTbs[t % 2]
            cT_, b_cT = combT.next()
            for j in range(4):
                blk = t * 4 + j
                norm_block(blk, hT, hTb[j], j * 128)
                r, br = rs.next()
                lg = r[:, 0:36]
                for k in range(8):
                    mm(pc[0][:, 0:36], hT[:, k, j * 128:(j + 1) * 128], wr[:, k, :], k == 0, k == 7,
                       [hTb[j], b_wr], [b_pc[0]])
                tt("dve", lg, pc[0][:, 0:36], rb_bc, ALU.add, [b_pc[0], b_wr], [br])
                gmax = r[:, 40:41]; ngmax = r[:, 41:42]; gsum = r[:, 42:43]; gval = r[:, 43:44]
                P.op("dve", lambda e, o=gmax, i=r[:, 0:4]: e.tensor_reduce(out=o, in_=i, axis=AX.X, op=ALU.max), [br], [br])
                ts("dve", ngmax, gmax, -1.0, ALU.mult, [br], [br])
                memset("dve", gsum, 0.0, [br])
                act(r[:, 44:48], r[:, 0:4], AF.Exp, [br], [br], bias=ngmax, scale=1.0, accum_out=gsum)
                P.op("dve", lambda e, o=gval, i=gsum: e.reciprocal(out=o, in_=i), [br], [br])
                gmask = r[:, 48:52]
                ts("dve", gmask, r[:, 0:4], gmax, ALU.is_equal, [br], [br])
                ts("dve", gmask, gmask, 1.0, ALU.subtract, [br], [br], s2=30000.0, op1=ALU.mult)
                em = r[:, 56:88]
                tt("dve", em.rearrange("p (g e) -> p g e", g=4), r[:, 4:36].rearrange("p (g e) -> p g e", g=4),
                   gmask.unsqueeze(2).to_broadcast([128, 4, 8]), ALU.add, [br], [br])
                top8 = r[:, 88:96]
                P.op("dve", lambda e, o=top8, i=em: e.max(out=o, in_=i), [br], [br])
                eq1 = r[:, 96:128]; eq2 = r[:, 128:160]
                ts("dve", eq1, em, top8[:, 0:1], ALU.is_equal, [br], [br])
                ts("dve", eq2, em, top8[:, 1:2], ALU.is_equal, [br], [br])
                dd = r[:, 36:37]; ex = r[:, 37:38]; w1g = r[:, 38:39]; w2g = r[:, 39:40]
                tt("dve", dd, top8[:, 1:2], top8[:, 0:1], ALU.subtract, [br], [br])
                act(ex, dd, AF.Exp, [br], [br])
                ts("dve", w1g, ex, 1.0, ALU.add, [br], [br])
                P.op("dve", lambda e, o=w1g, i=w1g: e.reciprocal(out=o, in_=i), [br], [br])
                tt("dve", w2g, ex, w1g, ALU.mult, [br], [br])
                tt("dve", w1g, w1g, gval, ALU.mult, [br], [br])
                tt("dve", w2g, w2g, gval, ALU.mult, [br], [br])
                ts("dve", eq1, eq1, w1g, ALU.mult, [br], [br])
                stt("dve", eq1, eq2, w2g, eq1, ALU.mult, ALU.add, [br], [br])
                cb_, bcb = combb.next()
                cp("dve", cb_, eq1, [br], [bcb])
                pv = pc[0][:].bitcast(BF16)
                P.op("pe", lambda e, o=pv[0:32, 128:256], i=cb_: e.transpose(o, i, ident_bf[:]), [bcb, b_const], [b_pc[0]])
                cp("dve", cT_[:, j * 128:(j + 1) * 128], pv[0:32, 128:256], [b_pc[0]], [b_cT])
            return cT_, b_cT

        def pass1(t, cT_, b_cT):
            hT, hTb = hTs[t % 2], hTbs[t % 2]
            for e_ in range(32):
                w_, bw = w13.next()
                key = "w13_%d" % ((w13.i - 1) % 3)
                dma("sync", w_[:, 0], w1s[e_].rearrange("p (k f) -> p k f", k=8), key, [b_w1s[e_]], [bw])
                dma("sync", w_[:, 1], w3s[e_].rearrange("p (k f) -> p k f", k=8), key, [b_w3s[e_]], [bw])
                if e_ % 2 == 0:
                    h1, bh1, h3, bh3, cbk, bcbk = pb[0][:], b_pb[0], pb[1][:], b_pb[1], pb[2][:], b_pb[2]
                else:
                    h1, bh1, h3, bh3, cbk, bcbk = pS[:, 0:512], b_pS[0], pS[:, 512:1024], b_pS[1], pc[0][:], b_pc[0]
                for k in range(8):
                    mm(h1, w_[:, 0, k, :], hT[:, k, :], k == 0, k == 7, [bw] + hTb, [bh1])
                for k in range(8):
                    mm(h3, w_[:, 1, k, :], hT[:, k, :], k == 0, k == 7, [bw] + hTb, [bh3])
                mm(cbk, SelE[:, e_, :], cT_, True, True, [b_const, b_cT], [bcbk])
                sT, bsT = sTr.next()
                tT, btT = tTr.next()
                act(sT, h1, AF.Silu, [bh1], [bsT])
                tt("dve", tT, h3, sT, ALU.mult, [bh3, bsT], [btT])
                tt("dve", hid[:, e_, :], cbk, tT, ALU.mult, [bcbk, btT], [hid_b[e_]])

        w2slots = {}

        def load_w2(t, q):
            w2_, bw2 = w2r.next()
            dma("sync", w2_, w2s[q], "w2q_%d" % ((w2r.i - 1) % 3), [b_w2s[q]], [bw2])
            w2slots[(t, q)] = (w2_, bw2)

        def pass2(t):
            bg_pump(per_tile)
            for j in range(4):
                blk = t * 4 + j
                dma("sync", xout[:, j, :], src_rows(blk), "xout%d" % j, [src_bufs[blk]], [xout_b[j]])
            for q in range(4):
                w2_, bw2 = w2slots[(t, q)]
                if q + 2 < 4:
                    load_w2(t, q + 2)
                elif t + 1 < NTc:
                    load_w2(t + 1, q - 2)
                for j in range(4):
                    bi = 1 + (q * 4 + j) % 2
                    for e_ in range(32):
                        mm(pc[bi][:, 0:256], hid[:, e_, j * 128:(j + 1) * 128], w2_[:, e_, :], e_ == 0, e_ == 31,
                           [hid_b[e_], bw2], [b_pc[bi]])
                    tq, btq = tmpq.next()
                    tt("dve", tq, pc[bi][:, 0:256], mod_bc[:, 5 * D + q * 256:5 * D + (q + 1) * 256], ALU.mult,
                       [b_pc[bi], b_mod], [btq])
                    tt("dve", xout[:, j, q * 256:(q + 1) * 256], tq, xout[:, j, q * 256:(q + 1) * 256], ALU.add,
                       [btq, xout_b[j]], [xout_b[j]])
            for j in range(4):
                blk = t * 4 + j
                dma("sync", xr[blk * 128:(blk + 1) * 128, :], xout[:, j, :], "xo%d" % j, [xout_b[j]], [xr_b[blk]])

        pend = norm_router(0)
        load_w2(0, 0)
        load_w2(0, 1)
        for t in range(NTc):
            pass1(t, *pend)
            if t + 1 < NTc:
                pend = norm_router(t + 1)
            pass2(t)

    def even_phase(l, src_rows, src_bufs):
        i_ = l // 2
        new_phase()
        if do_moe:
            queue_precast("moe", l)
        per_tile = (len(bg["steps"]) + NTc - 1) // max(NTc, 1)
        hT = carve([128, 8, 512], BF16)
        hTb = [Buf("hT%d" % j) for j in range(4)]
        norm_block = make_norm(src_rows, src_bufs, A1, SH1)
        Win = carve([128, 8, 2048], BF16); b_win = Buf("win")
        Wout = carve([128, 8, D], BF16); b_wout = Buf("wout")
        poolw = carve([128, 4, 128], BF16)
        pscale = carve([128, 4], F32)
        expb = carve([128, 8, 640], BF16); b_expb = Buf("expb")
        kT = carve([128, 4, 1024], BF16)
        kT_b = [Buf("kT%d" % s_) for s_ in range(8)]
        vaug = carve([128, 8, 4, 2, 128], BF16)
        va_b = [Buf("va%d" % s_) for s_ in range(8)]
        qT = carve([128, 4, 512], BF16); b_q = Buf("qT")
        ubuf = carve([128, 4, 527], F32); b_u = [Buf("u%d" % m) for m in range(4)]
        sA = carve([128, 527], F32); sB = carve([128, 527], F32); b_sA = Buf("sA"); b_sB = Buf("sB")
        pooled = Ring([carve([128, 512], BF16) for _ in range(2)], "pooled")
        invc0 = carve([128, 4, 16], F32)
        catT = carve([128, 8, 512], BF16); cat_b = [Buf("cat%d" % m) for m in range(8)]
        Pexp = Ring([carve([128, 640], BF16) for _ in range(2)], "Pexp")
        PT = Ring([carve([128, 640], BF16) for _ in range(2)], "PT")
        rsr = Ring([carve([128, 128], F32) for _ in range(2)], "rsr")
        xres = Ring([carve([128, D], F32) for _ in range(1)], "xres")
        xo = Ring([carve([128, D], F32) for _ in range(1)], "xo")
        hk = carve([128, 5, 128], F32); b_hk = Buf("hk")
        rb8 = xo.slots[0][0][0:8, :]; b_rb8 = xo.slots[0][1]

        for k in range(8):
            dma("pool", Win[:, k, :], di["mix_w_in"][i_, k * 128:(k + 1) * 128, :], "win", [], [b_win])
            dma("pool", Wout[:, k, :], di["mix_w_out"][i_, k * 128:(k + 1) * 128, :], "wout", [], [b_wout])
        dma("pool", poolw, di["pool_w"][i_].rearrange("g c d -> c g d"), "win", [], [b_win])
        dma("sync", pscale, di["pool_scale"][i_, :].rearrange("(m p) -> p m", p=128), "pscale", [], [b_win],
            allow_slow_non_contiguous=True)
        memset("pool", vaug[:, :, :, 0, 64:128], 1.0, va_b)
        memset("pool", vaug[:, :, :, 1, 0:64], 1.0, va_b)
        memset("pool", ubuf[:], 0.0, b_u)
        for m in range(4):
            w = 2 ** (m + 1)
            memset("pool", invc0[:, m, :], 1.0 / w, [b_const])
            for pos in range(w - 1):
                memset("pool", invc0[:, m, pos:pos + 1], 1.0 / (pos + 1), [b_const])
        dma("sync", rb8[:, 0:513], di["rel_bias"][i_], "rb8", [], [b_rb8])
        cp("dve", rb8[:, 513:897], rb8[:, 512:513].to_broadcast([8, 384]), [b_rb8], [b_rb8])
        dma("sync", rbx[:, 0:768], rb8[:, 129:897], "rbx", [b_rb8], [b_rbx])
        for h in range(8):
            src = bass.AP(rbx.tensor, h * 1024, [[1, 128], [128, 5], [1, 128]])
            dma("sync", hk, src, "hk", [b_rbx], [b_hk])
            hk2 = hk.rearrange("p a b -> p (a b)")
            mm(pS[:, 0:512], Jm[:], hk2[:, 0:512], True, True, [b_const, b_hk], [b_pS[0]])
            mm(pS[:, 512:640], Jm[:], hk2[:, 512:640], True, True, [b_const, b_hk], [b_pS[1]])
            act(expb[:, h, :], pS[:, 0:640], AF.Exp, b_pS, [b_expb])
        memset("pool", expb[64:128, :, 0:64], 0.0, [b_expb])
        memset("pool", expb[0:64, :, 4 * 128 + 64:5 * 128], 0.0, [b_expb])

        obr = Ring([pc[0][:, i * 128:(i + 1) * 128] for i in range(4)], "ob")
        pcnt = [0]

        def pbank():
            i = pcnt[0] % 3
            pcnt[0] += 1
            return pb[i], b_pb[i]

        for t in range(NTc):
            for j in range(4):
                norm_block(t * 4 + j, hT, hTb[j], j * 128)
            bg_pump(per_tile)
            for m in range(4):
                bank, bb = pbank()
                for k in range(8):
                    mm(bank[:], Win[:, k, m * 128:(m + 1) * 128], hT[:, k, :], k == 0, k == 7, [b_win] + hTb, [bb])
                if t > 0:
                    cp("dve", ubuf[:, m, 0:15], ubuf[:, m, 512:527], [b_u[m]], [b_u[m]])
                act(ubuf[:, m, 15:527], bank[:], AF.Copy, [bb], [b_u[m]])
                u = ubuf[:, m, :]
                tt("dve", sA[:, 1:527], u[:, 1:527], u[:, 0:526], ALU.add, [b_u[m]], [b_sA])
                cur, bcur = sA, b_sA
                if m >= 1:
                    tt("dve", sB[:, 3:527], sA[:, 3:527], sA[:, 1:525], ALU.add, [b_sA], [b_sB])
                    cur, bcur = sB, b_sB
                if m >= 2:
                    tt("dve", sA[:, 7:527], sB[:, 7:527], sB[:, 3:523], ALU.add, [b_sB], [b_sA])
                    cur, bcur = sA, b_sA
                if m >= 3:
                    tt("dve", sB[:, 15:527], sA[:, 15:527], sA[:, 7:519], ALU.add, [b_sA], [b_sB])
                    cur, bcur = sB, b_sB
                pl, bpl = pooled.next()
                w = 2 ** (m + 1)
                stt("dve", pl, cur[:, 15:527], 1.0 / w, u[:, 15:527], ALU.mult, ALU.subtract, [bcur, b_u[m]], [bpl])
                if t == 0:
                    tt("dve", cur[:, 15:31], cur[:, 15:31], invc0[:, m, :], ALU.mult, [bcur, b_const], [bcur])
                    tt("dve", pl[:, 0:16], cur[:, 15:31], u[:, 15:31], ALU.subtract, [bcur, b_u[m]], [bpl])
                bank, bb = pbank()
                mm(bank[:], poolw[:, m, :], pl, True, True, [b_win, bpl], [bb])
                ts("dve", catT[:, m, :], bank[:], pscale[:, m:m + 1], ALU.mult, [bb, b_win], [cat_b[m]])
            for m in range(4):
                bank, bb = pbank()
                for k in range(8):
                    mm(bank[:], Win[:, k, 512 + m * 128:512 + (m + 1) * 128], hT[:, k, :], k == 0, k == 7,
                       [b_win] + hTb, [bb])
                act(qT[:, m, :], bank[:], AF.Copy, [bb], [b_q])
            ks0 = (t % 2) * 4
            for m in range(4):
                bank, bb = pbank()
                for k in range(8):
                    mm(bank[:], Win[:, k, 1024 + m * 128:1024 + (m + 1) * 128], hT[:, k, :], k == 0, k == 7,
                       [b_win] + hTb, [bb])
                act(kT[:, m, ks0 * 128:(ks0 + 4) * 128], bank[:], AF.Copy, [bb], kT_b[ks0:ks0 + 4])
            for j in range(4):
                sl = ks0 + j
                bank, bb = pbank()
                for k in range(8):
                    mm(bank[:], hT[:, k, j * 128:(j + 1) * 128], Win[:, k, 1536:2048], k == 0, k == 7,
                       [b_win, hTb[j]], [bb])
                pvv = bank[:].rearrange("p (c two d) -> p c two d", c=4, two=2)
                cp("dve", vaug[:, sl, :, 0, 0:64], pvv[:, :, 0, :], [bb], [va_b[sl]])
                cp("dve", vaug[:, sl, :, 1, 64:128], pvv[:, :, 1, :], [bb], [va_b[sl]])
            for j in range(4):
                jq = t * 4 + j
                blocks = [i for i in range(jq - 4, jq + 1) if i >= 0]
                o_hi = jq - blocks[0]
                c1 = (o_hi + 1) * 128

                def qk(h):
                    c, hp = h // 2, h % 2
                    p0 = 64 * hp
                    par = h % 2
                    for i in blocks:
                        o = jq - i
                        sl = i % 8
                        if par == 0:
                            dst_s, bdst = pS[:, o * 128:(o + 1) * 128], b_pS[o // 4]
                        elif o < 4:
                            dst_s, bdst = pb[1][:, o * 128:(o + 1) * 128], b_pb[1]
                        else:
                            dst_s, bdst = pb[2][:, 0:128], b_pb[2]
                        mm(dst_s, kT[p0:p0 + 64, c, sl * 128:(sl + 1) * 128],
                           qT[p0:p0 + 64, c, j * 128:(j + 1) * 128], True, True, [kT_b[sl], b_q], [bdst])
                    pe_, bpe = Pexp.next()
                    if par == 0:
                        rd = [b_pS[0]] + ([b_pS[1]] if o_hi == 4 else [])
                        act(pe_[:, 0:c1], pS[:, 0:c1], AF.Exp, rd, [bpe], scale=0.125)
                    else:
                        c1a = min(c1, 512)
                        act(pe_[:, 0:c1a], pb[1][:, 0:c1a], AF.Exp, [b_pb[1]], [bpe], scale=0.125)
                        if o_hi == 4:
                            act(pe_[:, 512:640], pb[2][:, 0:128], AF.Exp, [b_pb[2]], [bpe], scale=0.125)
                    return pe_, bpe

                def pmul(h, pe_, bpe):
                    pt_, bpt = PT.next()
                    tt("dve", pt_[:, 0:c1], pe_[:, 0:c1], expb[:, h, 0:c1], ALU.mult, [bpe, b_expb], [bpt])
                    return pt_, bpt

                def pv(h, pt_, bpt):
                    c, hp = h // 2, h % 2
                    p0 = 64 * hp
                    if h % 2 == 0:
                        ob, bob = pc[0][:, 0:128], b_pc[0]
                    else:
                        ob, bob = pb[0][:, 0:128], b_pb[0]
                    for n_, i in enumerate(blocks):
                        o = jq - i
                        sl = i % 8
                        mm(ob, vaug[:, sl, c, hp, :], pt_[:, o * 128:(o + 1) * 128], n_ == 0,
                           n_ == len(blocks) - 1, [va_b[sl], bpt], [bob])
                    r_, br_ = rsr.next()
                    q0 = 64 * (1 - hp)
                    P.op("dve", lambda e, o_=r_[p0:p0 + 64, :], i_2=ob[q0:q0 + 64, :]: e.reciprocal(out=o_, in_=i_2),
                         [bob], [br_])
                    tt("dve", catT[p0:p0 + 64, 4 + c, j * 128:(j + 1) * 128], ob[p0:p0 + 64, :], r_[p0:p0 + 64, :],
                       ALU.mult, [bob, br_], [cat_b[4 + c]])

                pe0 = qk(0)
                ptc = pmul(0, *pe0)
                for h in range(8):
                    if h + 1 < 8:
                        pen = qk(h + 1)
                    pv(h, *ptc)
                    if h + 1 < 8:
                        ptc = pmul(h + 1, *pen)
            for j in range(4):
                blk = t * 4 + j
                xr_, bxr = xres.next()
                xo_, bxo = xo.next()
                dma("sync", xr_, src_rows(blk), "xres0", [src_bufs[blk]], [bxr])
                for half in range(2):
                    bank, bb = pbank()
                    for kk in range(8):
                        mm(bank[:], catT[:, kk, j * 128:(j + 1) * 128], Wout[:, kk, half * 512:(half + 1) * 512],
                           kk == 0, kk == 7, [cat_b[kk], b_wout], [bb])
                    tt("dve", xo_[:, half * 512:(half + 1) * 512], bank[:],
                       mod_bc[:, 2 * D + half * 512:2 * D + (half + 1) * 512], ALU.mult, [bb, b_mod], [bxo])
                    tt("dve", xo_[:, half * 512:(half + 1) * 512], xo_[:, half * 512:(half + 1) * 512],
                       xr_[:, half * 512:(half + 1) * 512], ALU.add, [bxo, bxr], [bxo])
                dma("sync", xr[blk * 128:(blk + 1) * 128, :], xo_, "xo0", [bxo], [xr_b[blk]])

    def ssd_phase(l, src_rows, src_bufs):
        i_ = l // 2
        queue_precast("ssd", l)
        bg_flush()
        new_phase()
        per_tile = (len(bg["steps"]) + NTc * 2 - 1) // max(NTc * 2, 1)
        TT_ = 256
        hT = carve([128, 8, TT_], BF16)
        hTb = [Buf("hT%d" % j) for j in range(2)]
        norm_block = make_norm(src_rows, src_bufs, A1, SH1, lean=True)
        Wout = carve([128, 16, D], BF16); b_wout = Buf("wout")
        wch = Ring([carve([128, 8, 128], BF16) for _ in range(3)], "wch")
        wz = Ring([carve([128, 2, 8, 128], BF16) for _ in range(2)], "wz")
        wdt = carve([128, 8, 32], BF16); b_wdt = Buf("wdt")
        cw = carve([128, 4, 32], F32); cbias = carve([128, 32], F32); b_cw = Buf("cw")
        halo = carve([128, 32, 3], F32); b_halo = [Buf("halo%d" % m) for m in range(32)]
        rawb = Ring([carve([128, TT_ + 3], F32) for _ in range(2)], "rawb")
        accr = Ring([carve([128, TT_], F32) for _ in range(2)], "acc")
        xT = carve([128, 16, TT_], BF16); BT = carve([128, 8, TT_], BF16); CT = carve([128, 8, TT_], BF16)
        xT_b = [Buf("xT%d" % m) for m in range(16)]; BT_b = [Buf("BT%d" % m) for m in range(8)]
        CT_b = [Buf("CT%d" % m) for m in range(8)]
        xtm = Ring([carve([128, 2048], BF16)], "xtm")
        xdt = Ring([carve([128, 2048], BF16)], "xdt")
        xdd = Ring([carve([128, 2048], BF16)], "xdd")
        Btm = Ring([carve([128, 1024], BF16)], "Btm")
        zs = Ring([carve([128, 2048], BF16) for _ in range(2)], "zs")
        smr = Ring([carve([128, 320], F32) for _ in range(2)], "ssm_sm")
        ahl = Ring([carve([128, 64], BF16) for _ in range(2)], "ahl")
        LTr = Ring([carve([128, 4, 128], BF16) for _ in range(2)], "LT")
        MTr = Ring([carve([128, 4, 128], BF16) for _ in range(2)], "MT")
        Gsr = Ring([carve([128, 128], BF16) for _ in range(2)], "Gs")
        state = carve([128, 8, 256], F32); stateb = carve([128, 8, 256], BF16)
        st_b = [Buf("st%d" % g) for g in range(8)]; stb_b = [Buf("stb%d" % g) for g in range(8)]
        ya = carve([128, 2048], F32); b_ya = [Buf("ya%d" % g) for g in range(8)]
        xD = carve([128, 2048], BF16); b_xD = Buf("xD")
        gn = carve([128, 2048], BF16); b_gn_h = [Buf("gn0"), Buf("gn1")]
        gnT = carve([128, 16, 128], BF16); b_gnT = [Buf("gnT0"), Buf("gnT1")]
        nw_bc = carve([128, 2048], BF16); vec_bc = carve([128, 96], F32); b_vec = Buf("vec")
        xres = Ring([carve([128, D], F32)], "xres")
        xo = Ring([carve([128, D], F32)], "xo")
        junk2 = carve([128, 256], BF16); b_j2 = Buf("junk2")

        for k in range(16):
            dma("pool", Wout[:, k, :], di["ssm_w_out"][i_, k * 128:(k + 1) * 128, :], "wout", [], [b_wout])
        dma("pool", wdt, di["ssm_w_in"][i_, :, 6144:6176].rearrange("(k p) f -> p k f", p=128), "wdt", [], [b_wdt])
        cwr = xo.slots[0][0][0:32, 0:640].rearrange("p (t c) -> p t c", t=5)
        b_cwr = xo.slots[0][1]
        for tap in range(4):
            dma("sync", cwr[:, tap, :], di["ssm_conv_w"][i_, tap, :].rearrange("(m p) -> m p", p=128), "cwr", [], [b_cwr])
        dma("sync", cwr[:, 4, :], di["ssm_conv_b"][i_, :].rearrange("(m p) -> m p", p=128), "cwr", [], [b_cwr])
        for tap in range(5):
            P.op("pe", (lambda t_: lambda e: e.transpose(pb[0][:, t_ * 32:(t_ + 1) * 32], cwr[:, t_, :], ident_f[0:32, 0:32]))(tap),
                 [b_cwr, b_const], [b_pb[0]])
        cp("dve", cw.rearrange("p t m -> p (t m)"), pb[0][:, 0:128], [b_pb[0]], [b_cw])
        cp("dve", cbias, pb[0][:, 128:160], [b_pb[0]], [b_cw])
        dma("pool", nw_bc, di["ssm_norm_w"][i_, :].partition_broadcast(128), "vecn", [], [b_vec])
        dtb_bc = vec_bc[:, 0:32]; A_bc = vec_bc[:, 32:64]; D_bc = vec_bc[:, 64:96]
        dma("sync", dtb_bc, di["ssm_dt_bias"][i_, :].partition_broadcast(128), "vec", [], [b_vec])
        dma("sync", A_bc, di["ssm_A_log"][i_, :].partition_broadcast(128), "vec", [], [b_vec])
        dma("sync", D_bc, di["ssm_D"][i_, :].partition_broadcast(128), "vec", [], [b_vec])
        act(A_bc, A_bc, AF.Exp, [b_vec], [b_vec])
        ts("dve", A_bc, A_bc, -1.0, ALU.mult, [b_vec], [b_vec])
        memset("pool", halo[:], 0.0, b_halo)
        memset("pool", state[:], 0.0, st_b)
        memset("pool", stateb[:], 0.0, stb_b)

        G_ps = pb[0][:, 0:128]
        b_small = b_pb[0]
        st_ps, b_stp = pb[0][:, 256:512], b_pb[0]
        yd_ps = pc[0][:, 0:256]
        yo_ps, b_yo = pc[0][:, 256:512], b_pc[0]
        lcnt = [0]
        hcnt = [0]

        def half():
            i = hcnt[0] % 2
            hcnt[0] += 1
            return pS[:, i * 512:(i + 1) * 512], b_pS[i]

        stop_ = cfg.get("ssd_stop", 99)
        if stop_ <= 1:
            return
        for t in range(NTc * 2):
            for j in range(2):
                norm_block(t * 2 + j, hT, hTb[j], j * 128)
            bg_pump(per_tile)
            smv = []
            zv = []
            for j in range(2):
                jc = slice(j * 128, (j + 1) * 128)
                sm, bsm = smr.next()
                dt_ = sm[:, 0:32]; a_ = sm[:, 32:64]; acs = sm[:, 64:96]; nacs = sm[:, 96:128]
                tot = sm[:, 128:160]; eacs = sm[:, 160:192]; dstt = sm[:, 192:224]; cdec = sm[:, 224:256]
                ssq = sm[:, 256:264]; lnv = sm[:, 264:272]; rstd = sm[:, 272:280]
                sp = pb[0][:, 128:160]
                for k in range(8):
                    mm(sp, hT[:, k, jc], wdt[:, k, :], k == 0, k == 7, [hTb[j], b_wdt], [b_small])
                tt("dve", dt_, sp, dtb_bc, ALU.add, [b_small, b_vec], [bsm])
                act(dt_, dt_, AF.Exp, [bsm], [bsm])
                act(dt_, dt_, AF.Ln, [bsm], [bsm], bias=1.0)
                tt("dve", a_, dt_, A_bc, ALU.mult, [bsm, b_vec], [bsm])
                mm(pb[0][:, 160:192], Tm[:], a_, True, True, [b_const, bsm], [b_small])
                mm(pb[0][:, 192:224], ones_f[:], a_, True, True, [b_const, bsm], [b_small])
                cp("dve", acs, pb[0][:, 160:192], [b_small], [bsm])
                ts("dve", nacs, acs, -1.0, ALU.mult, [bsm], [bsm])
                cp("dve", tot, pb[0][:, 192:224], [b_small], [bsm])
                act(eacs, acs, AF.Exp, [bsm], [bsm])
                tt("dve", dstt, tot, acs, ALU.subtract, [bsm], [bsm])
                act(dstt, dstt, AF.Exp, [bsm], [bsm])
                act(cdec, tot, AF.Exp, [bsm], [bsm])
                ah_, bah = ahl.next()
                cp("dve", ah_[:, 0:32], a_, [bsm], [bah])
                tt("dve", sm[:, 288:320], a_, ah_[:, 0:32], ALU.subtract, [bsm, bah], [bsm])
                cp("dve", ah_[:, 32:64], sm[:, 288:320], [bsm], [bah])
                smv.append((sm, bsm, ah_, bah))
                z_, bz = zs.next()
                for pz in range(8):
                    wz_, bwz = wz.next()
                    dma("sync", wz_, wins[2 * pz:2 * pz + 2].rearrange("c p (k f) -> p c k f", k=8),
                        "wz%d" % ((wz.i - 1) % 2), [b_wins[2 * pz], b_wins[2 * pz + 1]], [bwz])
                    bank, bb = half()
                    for k in range(8):
                        mm(bank[:, 0:256].rearrange("p (c f) -> p c f", c=2), hT[:, k, jc], wz_[:, :, k, :], k == 0, k == 7,
                           [hTb[j], bwz], [bb])
                    act(z_[:, pz * 256:(pz + 1) * 256], bank[:, 0:256], AF.Silu, [bb], [bz])
                zv.append((z_, bz))
            pend_silu = None
            for m in range(32):
                w_, bw = wch.next()
                dma("sync", w_, wins[16 + m].rearrange("p (k f) -> p k f", k=8), "wch%d" % ((wch.i - 1) % 3),
                    [b_wins[16 + m]], [bw])
                bank, bb = half()
                for k in range(8):
                    mm(bank[:, 0:TT_], w_[:, k, :], hT[:, k, :], k == 0, k == 7, [bw] + hTb, [bb])
                rw, brw = rawb.next()
                ac, bac = accr.next()
                cp("dve", rw[:, 0:3], halo[:, m, :], [b_halo[m]], [brw])
                act(rw[:, 3:TT_ + 3], bank[:, 0:TT_], AF.Copy, [bb], [brw])
                if pend_silu is not None:
                    pend_silu()
                cp("dve", halo[:, m, :], rw[:, TT_:TT_ + 3], [brw], [b_halo[m]])
                ts("dve", ac, rw[:, 0:TT_], cw[:, 0, m:m + 1], ALU.mult, [brw, b_cw], [bac])
                for tap in range(1, 4):
                    stt("dve", ac, rw[:, tap:TT_ + tap], cw[:, tap, m:m + 1], ac, ALU.mult, ALU.add,
                        [brw, b_cw, bac], [bac])
                if m < 16:
                    dst_, bd = xT[:, m, :], xT_b[m]
                elif m < 24:
                    dst_, bd = BT[:, m - 16, :], BT_b[m - 16]
                else:
                    dst_, bd = CT[:, m - 24, :], CT_b[m - 24]

                def _silu(dst_=dst_, ac=ac, bac=bac, bd=bd, m=m):
                    act(dst_, ac, AF.Silu, [bac, b_cw], [bd], bias=cbias[:, m:m + 1])
                pend_silu = _silu
            pend_silu()
            if stop_ <= 2:
                return
            for j in range(2):
                blk = t * 2 + j
                jc = slice(j * 128, (j + 1) * 128)
                sm, bsm, ah_, bah = smv[j]
                z_, bz = zv[j]
                dt_ = sm[:, 0:32]; a_ = sm[:, 32:64]; acs = sm[:, 64:96]; nacs = sm[:, 96:128]
                tot = sm[:, 128:160]; eacs = sm[:, 160:192]; dstt = sm[:, 192:224]; cdec = sm[:, 224:256]
                ssq = sm[:, 256:264]; lnv = sm[:, 264:272]; rstd = sm[:, 272:280]
                if stop_ <= 3:
                    return
                xt_, bxt = xtm.next(); xd_, bxd = xdt.next(); xdd_, bxdd = xdd.next(); bt_, bbt = Btm.next()
                for hf_ in range(2):
                    pbank, bbank = pc[1 + hf_], b_pc[1 + hf_]
                    pv = pbank[:].bitcast(BF16)
                    for k in range(8):
                        trn(pv[:, k * 128:(k + 1) * 128], xT[:, hf_ * 8 + k, jc], [xT_b[hf_ * 8 + k], b_const], [bbank])
                    cs = slice(hf_ * 1024, (hf_ + 1) * 1024)
                    act(xt_[:, cs], pv, AF.Copy, [bbank], [bxt])
                    if stop_ <= 3.2:
                        continue
                    tt("dve", xd_[:, cs].rearrange("p (h d) -> p h d", h=16), xt_[:, cs].rearrange("p (h d) -> p h d", h=16),
                       dt_[:, hf_ * 16:(hf_ + 1) * 16].unsqueeze(2).to_broadcast([128, 16, 64]), ALU.mult,
                       [bxt, bsm], [bxd])
                if stop_ <= 3.4:
                    return
                pv = pc[1][:].bitcast(BF16)
                for g in range(8):
                    trn(pv[:, g * 128:(g + 1) * 128], BT[:, g, jc], [BT_b[g], b_const], [b_pc[1]])
                act(bt_, pv, AF.Copy, [b_pc[1]], [bbt])
                tt("dve", xdd_.rearrange("p (h d) -> p h d", h=32), xd_.rearrange("p (h d) -> p h d", h=32),
                   dstt.unsqueeze(2).to_broadcast([128, 32, 64]), ALU.mult, [bxd, bsm], [bxdd])
                tt("dve", xD.rearrange("p (h d) -> p h d", h=32), xt_.rearrange("p (h d) -> p h d", h=32),
                   D_bc.unsqueeze(2).to_broadcast([128, 32, 64]), ALU.mult, [bxt, b_vec], [b_xD])
                if stop_ <= 4:
                    return
                def banks(g):
                    if g % 2 == 0:
                        return (pb[0][:, 0:128], b_pb[0], pb[0][:, 256:512], b_pb[0], pb[1], b_pb[1],
                                pc[0][:, 0:256], b_pc[0], pc[0][:, 256:512], b_pc[0])
                    return (pS[:, 0:128], b_pS[0], pS[:, 256:512], b_pS[0], pb[2], b_pb[2],
                            pS[:, 512:768], b_pS[1], pS[:, 768:1024], b_pS[1])

                def preA(g):
                    Gp, bG, st_ps, b_stp, Dp, bDp, yd, byd, yo_ps, b_yo = banks(g)
                    mm(Gp, BT[:, g, jc], CT[:, g, jc], True, True, [BT_b[g], CT_b[g]], [bG])
                    LT, bLT = LTr.next()
                    for r in range(4):
                        h = 4 * g + r
                        reg = Dp[:, r * 128:(r + 1) * 128]
                        mm(reg, ah_[:, h:h + 1].to_broadcast([128, 128]), Tm_bf[:], True, False, [bah, b_const], [bDp])
                        mm(reg, ah_[:, 32 + h:33 + h].to_broadcast([128, 128]), Tm_bf[:], False, False, [bah, b_const], [bDp])
                        mm(reg, ident_bf[:], negm[:], False, True, [b_const], [bDp])
                    for r in range(4):
                        h = 4 * g + r
                        act(LT[:, r, :], Dp[:, r * 128:(r + 1) * 128], AF.Exp, [bDp, bsm], [bLT], bias=nacs[:, h:h + 1])
                    Gs, bGs = Gsr.next()
                    act(Gs, Gp, AF.Copy, [bG], [bGs])
                    return LT, bLT, Gs, bGs

                def preB(g, LT, bLT, Gs, bGs):
                    MT, bMT = MTr.next()
                    tt("dve", MT, LT, Gs.unsqueeze(1).to_broadcast([128, 4, 128]), ALU.mult, [bLT, bGs], [bMT])
                    return MT, bMT

                def post(g, MT, bMT):
                    Gp, bG, st_ps, b_stp, Dp, bDp, yd, byd, yo_ps, b_yo = banks(g)
                    for r in range(4):
                        h = 4 * g + r
                        mm(yd[:, r * 64:(r + 1) * 64], MT[:, r, :], xd_[:, h * 64:(h + 1) * 64], True, True, [bMT, bxd], [byd])
                    gs = slice(g * 256, (g + 1) * 256)
                    mm(yo_ps, CT[:, g, jc], stateb[:, g, :], True, True, [CT_b[g], stb_b[g]], [b_yo])
                    mm(st_ps, bt_[:, g * 128:(g + 1) * 128], xdd_[:, gs], True, True, [bbt, bxdd], [b_stp])

                def post_ew(g):
                    Gp, bG, st_ps, b_stp, Dp, bDp, yd, byd, yo_ps, b_yo = banks(g)
                    gs = slice(g * 256, (g + 1) * 256)
                    tt("dve", ya[:, gs].rearrange("p (h d) -> p h d", h=4), yo_ps.rearrange("p (h d) -> p h d", h=4),
                       eacs[:, 4 * g:4 * g + 4].unsqueeze(2).to_broadcast([128, 4, 64]), ALU.mult, [b_yo, bsm], [b_ya[g]])
                    tt("dve", ya[:, gs], ya[:, gs], yd, ALU.add, [b_ya[g], byd], [b_ya[g]])
                    tt("dve", state[:, g, :].rearrange("p (h d) -> p h d", h=4),
                       state[:, g, :].rearrange("p (h d) -> p h d", h=4),
                       cdec[:, 4 * g:4 * g + 4].unsqueeze(2).to_broadcast([128, 4, 64]), ALU.mult, [st_b[g], bsm], [st_b[g]])
                    tt("dve", state[:, g, :], state[:, g, :], st_ps, ALU.add, [st_b[g], b_stp], [st_b[g]])
                    cp("pool", stateb[:, g, :], state[:, g, :], [st_b[g]], [stb_b[g]])
                    tt("dve", ya[:, gs], ya[:, gs], xD[:, gs], ALU.add, [b_ya[g], b_xD], [b_ya[g]])
                    tt("dve", ya[:, gs], ya[:, gs], z_[:, gs], ALU.mult, [b_ya[g], bz], [b_ya[g]])
                    act(junk2, ya[:, gs], AF.Square, [b_ya[g], bsm], [b_j2, bsm], accum_out=ssq[:, g:g + 1])
                    tt("pool", ya[:, gs], ya[:, gs], nw_bc[:, gs], ALU.mult, [b_ya[g], b_vec], [b_ya[g]])

                memset("dve", ssq, 0.0, [bsm])
                pa = preA(0)
                mt = preB(0, *pa)
                for g in range(8):
                    if g + 1 < 8:
                        pa = preA(g + 1)
                    post(g, *mt)
                    post_ew(g)
                    if g + 1 < 8:
                        mt = preB(g + 1, *pa)
                if stop_ <= 5:
                    return
                act(lnv, ssq, AF.Ln, [bsm], [bsm], bias=EPS, scale=1.0 / 256)
                act(rstd, lnv, AF.Exp, [bsm], [bsm], scale=-0.5)
                for g in range(8):
                    gs = slice(g * 256, (g + 1) * 256)
                    ts("dve", gn[:, gs], ya[:, gs], rstd[:, g:g + 1], ALU.mult,
                       [b_ya[g], bsm], [b_gn_h[g // 4]])
                for hf_ in range(2):
                    pbank, bbank = pc[1 + hf_], b_pc[1 + hf_]
                    pv = pbank[:].bitcast(BF16)
                    for k in range(8):
                        trn(pv[:, k * 128:(k + 1) * 128], gn[:, (hf_ * 8 + k) * 128:(hf_ * 8 + k + 1) * 128],
                            [b_gn_h[hf_], b_const], [bbank])
                    act(gnT[:, hf_ * 8:(hf_ + 1) * 8, :], pv.rearrange("p (k t) -> p k t", k=8), AF.Copy, [bbank],
                        [b_gnT[hf_]])
                xr_, bxr = xres.next()
                xo_, bxo = xo.next()
                dma("sync", xr_, src_rows(blk), "xres0", [src_bufs[blk]], [bxr])
                for hh in range(2):
                    bank, bb = half()
                    for kk in range(16):
                        mm(bank, gnT[:, kk, :], Wout[:, kk, hh * 512:(hh + 1) * 512], kk == 0, kk == 15,
                           b_gnT + [b_wout], [bb])
                    cs = slice(hh * 512, (hh + 1) * 512)
                    tt("dve", xo_[:, cs], bank, mod_bc[:, 2 * D + hh * 512:2 * D + (hh + 1) * 512], ALU.mult,
                       [bb, b_mod], [bxo])
                    tt("dve", xo_[:, cs], xo_[:, cs], xr_[:, cs], ALU.add, [bxo, bxr], [bxo])
                dma("sync", xr[blk * 128:(blk + 1) * 128, :], xo_, "xo0", [bxo], [xr_b[blk]])

    def final_phase(src_rows, src_bufs):
        new_phase()
        xin = Ring([carve([128, D], F32) for _ in range(2)], "fxin")
        xo = Ring([carve([128, D], F32) for _ in range(2)], "fxo")
        junk = carve([128, D], BF16); bj = Buf("fj")
        sm = Ring([carve([128, 4], F32) for _ in range(2)], "fsm")
        fw = carve([128, D], F32); b_fw = Buf("fw")
        dma("sync", fw, di["final_norm_w"][0, :].partition_broadcast(128), "fw", [], [b_fw])
        for blk in range(NTc * 4):
            x_, bx = xin.next()
            o_, bo = xo.next()
            s_, bs = sm.next()
            dma("sync", x_, src_rows(blk), "fxin%d" % (blk % 2), [src_bufs[blk]], [bx])
            if do_final:
                memset("dve", s_[:, 0:1], 0.0, [bs])
                act(junk, x_, AF.Square, [bx, bs], [bj, bs], accum_out=s_[:, 0:1])
                act(s_[:, 1:2], s_[:, 0:1], AF.Ln, [bs], [bs], bias=EPS, scale=1.0 / D)
                act(s_[:, 2:3], s_[:, 1:2], AF.Exp, [bs], [bs], scale=-0.5)
                stt("dve", o_, x_, s_[:, 2:3], fw, ALU.mult, ALU.mult, [bx, bs, b_fw], [bo])
            else:
                cp("dve", o_, x_, [bx], [bo])
            dma("sync", y_d[blk * 128:(blk + 1) * 128, :], o_, "fxo%d" % (blk % 2), [bo], [y_b[blk]])

    src = lambda blk: di["x"][blk * 128:(blk + 1) * 128, :]
    src_b = [Buf("xsrc%d" % i) for i in range(NBLK)]
    xr_rows = lambda blk: xr[blk * 128:(blk + 1) * 128, :]
    for l in layers:
        ada_phase(l)
        if do_mixer:
            if l % 2 == 0:
                even_phase(l, src, src_b)
            else:
                ssd_phase(l, src, src_b)
            src, src_b = xr_rows, xr_b
        if do_moe:
            moe_phase(l, src, src_b)
            src, src_b = xr_rows, xr_b
    bg_flush()
    final_phase(src, src_b)
    P.barrier()
    P.emit()
    return nc


_CACHE = {}


def kernel(**inputs):
    cfg = inputs.pop("_cfg", None) or {}
    ncores = cfg.get("ncores", 8)
    key = repr(sorted(cfg.items()))
    if key not in _CACHE:
        _CACHE[key] = build(cfg)
    nc = _CACHE[key]
    shared = {}
    for name, shp in INPUT_SHAPES:
        if name in ("x", "c"):
            continue
        shared[name] = np.ascontiguousarray(np.asarray(inputs[name], dtype=np.float32).reshape(shp))
    x = np.asarray(inputs["x"], dtype=np.float32)
    c = np.asarray(inputs["c"], dtype=np.float32)
    in_maps = []
    for b in range(ncores):
        m = dict(shared)
        m["x"] = np.ascontiguousarray(x[b])
        m["c"] = np.ascontiguousarray(c[b:b + 1])
        in_maps.append(m)
    res = run_bass_kernel_spmd(nc, in_maps, core_ids=list(range(ncores)))
    out = np.stack([np.asarray(res.results[b]["y"], dtype=np.float32).reshape(S, D) for b in range(ncores)], axis=0)
    return out
```

```python
import numpy as np
import concourse.bass as bass
import concourse.mybir as mybir
from concourse.bass_utils import run_bass_kernel_spmd

F32 = mybir.dt.float32
BF16 = mybir.dt.bfloat16
AF = mybir.ActivationFunctionType
ALU = mybir.AluOpType
AX = mybir.AxisListType

ENGS = ("sync", "act", "pool", "pe", "dve")
D = 1024
S = 4096
NBLK = 32
NT = 8
EPS = 1e-6


class Buf:
    __slots__ = ("name", "w", "r")

    def __init__(self, name=""):
        self.name = name
        self.w = None
        self.r = {}


class Prog:
    def __init__(self, nc, same_eng_sync=("pool", "act", "dve")):
        self.nc = nc
        self.ops = {e: [] for e in ENGS}
        self.dma_cnt = {}
        self.same_eng_sync = set(same_eng_sync)
        self.last_real = {e: None for e in ENGS}

    def _deps_for(self, reads, writes):
        deps = set()
        for b in reads:
            if b.w is not None:
                deps.add(b.w)
        for b in writes:
            if b.w is not None:
                deps.add(b.w)
            for t in b.r.values():
                deps.add(t)
        return deps

    def _commit(self, tok, reads, writes):
        for b in writes:
            b.w = tok
            b.r = {}
        for b in reads:
            if b in writes:
                continue
            k = tok[0] if tok[0] != "dma" else ("dma", tok[1])
            b.r[k] = tok

    def op(self, eng, fn, reads=(), writes=()):
        deps = self._deps_for(reads, writes)
        idx = len(self.ops[eng])
        tok = (eng, idx)
        self.ops[eng].append([fn, deps, "op", None])
        self.last_real[eng] = tok
        self._commit(tok, reads, writes)
        return tok

    def dma(self, eng, fn, key, reads=(), writes=()):
        deps = self._deps_for(reads, writes)
        deps = set(d for d in deps if not (d[0] == "dma" and d[1] == key))
        c = self.dma_cnt.get(key, 0) + 1
        self.dma_cnt[key] = c
        tok = ("dma", key, c)
        self.ops[eng].append([fn, deps, "dma", key])
        self._commit(tok, reads, writes)
        return tok

    def barrier(self):
        toks = set(t for t in self.last_real.values() if t is not None)
        toks |= set(("dma", k, c) for k, c in self.dma_cnt.items())
        for e in ENGS:
            self.ops[e].append([None, set(toks), "op", None])

    def emit(self):
        nc = self.nc
        need = {e: set() for e in ENGS}
        for e in ENGS:
            for (fn, deps, kind, key) in self.ops[e]:
                for d in deps:
                    if d[0] != "dma":
                        if d[0] == e and e not in self.same_eng_sync:
                            continue
                        need[d[0]].add(d[1])
        msval = {}
        for e in ENGS:
            c = 0
            m = {}
            for i in sorted(need[e]):
                c += 1
                m[i] = c
            msval[e] = m
        sem = {e: nc.alloc_semaphore("s_" + e) for e in ENGS}
        dsem = {k: nc.alloc_semaphore("d_%d" % i) for i, k in enumerate(self.dma_cnt)}
        engobj = {"sync": "sync", "act": "scalar", "pool": "gpsimd", "pe": "tensor", "dve": "vector"}

        def run(e):
            def body(eng):
                seen = {}
                for i, (fn, deps, kind, key) in enumerate(self.ops[e]):
                    for d in sorted(deps, key=str):
                        if d[0] == "dma":
                            sk = ("dma", d[1]); val = 16 * d[2]; s = dsem[d[1]]
                        else:
                            if d[0] == e and e not in self.same_eng_sync:
                                continue
                            sk = d[0]; val = msval[d[0]][d[1]]; s = sem[d[0]]
                        if seen.get(sk, 0) >= val:
                            continue
                        seen[sk] = val
                        eng.wait_ge(s, val)
                    if fn is None:
                        continue
                    ins = fn(eng)
                    if kind == "dma":
                        ins.then_inc(dsem[key], 16)
                    elif i in msval[e]:
                        ins.then_inc(sem[e], 1)
            return body

        with nc.Block() as block:
            for e in ENGS:
                if self.ops[e]:
                    getattr(block, engobj[e])(run(e))


class Ring:
    def __init__(self, aps, name):
        self.slots = [(ap, Buf(name + str(i))) for i, ap in enumerate(aps)]
        self.i = 0

    def next(self):
        s = self.slots[self.i % len(self.slots)]
        self.i += 1
        return s


INPUT_SHAPES = [
    ("x", [S, D]), ("c", [1, D]), ("ada_w", [4, D, 6 * D]), ("ada_b", [4, 6 * D]),
    ("norm1_w", [4, D]), ("norm2_w", [4, D]), ("mix_w_in", [2, D, 2048]), ("pool_w", [2, 4, 128, 128]),
    ("pool_scale", [2, 512]), ("rel_bias", [2, 8, 513]), ("mix_w_out", [2, 1024, D]),
    ("ssm_w_in", [2, D, 6176]), ("ssm_conv_w", [2, 4, 4096]), ("ssm_conv_b", [2, 4096]),
    ("ssm_dt_bias", [2, 32]), ("ssm_A_log", [2, 32]), ("ssm_D", [2, 32]), ("ssm_norm_w", [2, 2048]),
    ("ssm_w_out", [2, 2048, D]), ("moe_w_group", [4, D, 4]), ("moe_b_group", [4, 4]),
    ("moe_w_expert", [4, D, 4, 8]), ("moe_b_expert", [4, 4, 8]), ("moe_w1", [4, 32, D, 128]),
    ("moe_w3", [4, 32, D, 128]), ("moe_w2", [4, 32, 128, D]), ("final_norm_w", [1, D]),
]


def build(cfg):
    layers = cfg.get("layers", [0, 1, 2, 3])
    do_final = cfg.get("final", True)
    do_mixer = cfg.get("mixer", True)
    do_moe = cfg.get("moe", True)
    NTc = cfg.get("ntiles", NT)
    nc = bass.Bass("TRN2", target_bir_lowering=False)
    P = Prog(nc)
    di = {}
    for name, shp in INPUT_SHAPES:
        di[name] = nc.dram_tensor(name, list(shp), F32, kind="ExternalInput").ap()
    y_d = nc.dram_tensor("y", [S, D], F32, kind="ExternalOutput").ap()
    xr = nc.dram_tensor("xr", [S, D], F32).ap()
    rbx = nc.dram_tensor("rbx", [8, 1024], F32).ap()
    b_rbx = Buf("rbx")
    xr_b = [Buf("xr%d" % i) for i in range(NBLK)]
    y_b = [Buf("y%d" % i) for i in range(NBLK)]

    def act(out, in_, func, r, w, **kw):
        P.op("act", lambda e: e.activation(out=out, in_=in_, func=func, **kw), r, w)

    def tt(eng, out, in0, in1, op, r, w):
        P.op(eng, lambda e: e.tensor_tensor(out=out, in0=in0, in1=in1, op=op), r, w)

    def ts(eng, out, in0, s1, op0, r, w, s2=None, op1=None):
        if op1 is None:
            P.op(eng, lambda e: e.tensor_scalar(out=out, in0=in0, scalar1=s1, scalar2=None, op0=op0), r, w)
        else:
            P.op(eng, lambda e: e.tensor_scalar(out=out, in0=in0, scalar1=s1, scalar2=s2, op0=op0, op1=op1), r, w)

    def stt(eng, out, in0, scalar, in1, op0, op1, r, w):
        P.op(eng, lambda e: e.scalar_tensor_tensor(out=out, in0=in0, scalar=scalar, in1=in1, op0=op0, op1=op1), r, w)

    def mm(out, lhsT, rhs, start, stop, r, w):
        P.op("pe", lambda e: e.matmul(out, lhsT, rhs, start=start, stop=stop), r, w)

    def trn(out, in_, r, w):
        P.op("pe", lambda e: e.transpose(out, in_, ident_bf[:]), r, w)

    def cp(eng, out, in_, r, w):
        P.op(eng, lambda e: e.tensor_copy(out=out, in_=in_), r, w)

    def dma(q, out, in_, key, r, w, **kw):
        P.dma(q, lambda e: e.dma_start(out=out, in_=in_, **kw), key, r, w)

    def memset(eng, ap, val, w):
        P.op(eng, lambda e: e.memset(ap, val), [], w)

    def asel(out, in_, pattern, cmp, fill, base, cm, r, w):
        P.op("pool", lambda e: e.affine_select(out=out, in_=in_, pattern=pattern, compare_op=cmp, fill=fill,
                                               base=base, channel_multiplier=cm), r, w)

    pb = [nc.alloc_psum_tensor("pb%d" % i, [128, 512], F32) for i in range(3)]
    pS = nc.alloc_psum_tensor("pS", [128, 1024], F32)
    pc = [nc.alloc_psum_tensor("pc%d" % i, [128, 512], F32) for i in range(3)]
    b_pb = [Buf("pb%d" % i) for i in range(3)]
    b_pS = [Buf("pS0"), Buf("pS1")]
    b_pc = [Buf("pc%d" % i) for i in range(3)]

    def sb(name, shape, dt):
        return nc.alloc_sbuf_tensor(name, list(shape), dt)

    b_const = Buf("const")
    ident_f = sb("ident_f", [128, 128], F32)
    ident_bf = sb("ident_bf", [128, 128], BF16)
    Jm = sb("Jm", [128, 128], F32)
    ones_f = sb("ones_f", [128, 128], F32)
    Tm = sb("Tm", [128, 128], F32)
    Tm_bf = sb("Tm_bf", [128, 128], BF16)
    negm_f = sb("negm_f", [128, 128], F32)
    negm = sb("negm", [128, 128], BF16)
    SelE = sb("SelE", [32, 32, 128], BF16)
    mod_bc = sb("mod_bc", [128, 6 * D], F32)
    cactB = sb("cactB", [128, 8, 128], F32)
    cT = sb("cT", [128, 8], F32)
    b_mod = Buf("mod")

    memset("pool", ident_f[:], 1.0, [b_const])
    asel(ident_f[:], ident_f[:], [[-1, 128]], ALU.is_equal, 0.0, 0, 1, [b_const], [b_const])
    cp("dve", ident_bf[:], ident_f[:], [b_const], [b_const])
    memset("pool", Jm[:], 1.0, [b_const])
    asel(Jm[:], Jm[:], [[1, 128]], ALU.is_equal, 0.0, -127, 1, [b_const], [b_const])
    memset("pool", ones_f[:], 1.0, [b_const])
    memset("pool", Tm[:], 1.0, [b_const])
    asel(Tm[:], Tm[:], [[1, 128]], ALU.is_ge, 0.0, 0, -1, [b_const], [b_const])
    cp("dve", Tm_bf[:], Tm[:], [b_const], [b_const])
    memset("pool", negm_f[:], 0.0, [b_const])
    asel(negm_f[:], negm_f[:], [[1, 128]], ALU.is_ge, -30000.0, 0, -1, [b_const], [b_const])
    cp("dve", negm[:], negm_f[:], [b_const], [b_const])
    memset("pool", SelE[:], 1.0, [b_const])
    asel(SelE[:], SelE[:], [[-1, 32], [0, 128]], ALU.is_equal, 0.0, 0, 1, [b_const], [b_const])

    dma("sync", cT[:], di["c"][0, :].rearrange("(k p) -> p k", p=128), "cT", [], [b_const],
        allow_slow_non_contiguous=True)
    act(cT[:], cT[:], AF.Silu, [b_const], [b_const])
    for k in range(8):
        cp("dve", cactB[:, k, :], cT[:, k:k + 1].to_broadcast([128, 128]), [b_const], [b_const])

    w1s_ = [nc.dram_tensor("w1s%d" % i, [32, 128, 1024], BF16).ap() for i in range(2)]
    w3s_ = [nc.dram_tensor("w3s%d" % i, [32, 128, 1024], BF16).ap() for i in range(2)]
    w2s_ = [nc.dram_tensor("w2s%d" % i, [4, 128, 32, 256], BF16).ap() for i in range(2)]
    wins = nc.dram_tensor("wins", [48, 128, 1024], BF16).ap()
    b_w1s_ = [[Buf("w1s%d" % e) for e in range(32)] for _ in range(2)]
    b_w3s_ = [[Buf("w3s%d" % e) for e in range(32)] for _ in range(2)]
    b_w2s_ = [[Buf("w2s%d" % q) for q in range(4)] for _ in range(2)]
    b_wins = [Buf("wins%d" % m) for m in range(48)]
    NSTG = 3
    stg = Ring([sb("stg%d" % i, [128, 1024], BF16)[:] for i in range(NSTG)], "stg")
    bg = {"steps": [], "queued": set()}

    def _mk_pre(src_ap, src_is_kpf, dst_ap, dst_bufs):
        slot = {}

        def L():
            s_, bs_ = stg.next()
            slot["s"] = (s_, bs_, (stg.i - 1) % NSTG)
            if src_is_kpf:
                dma("pool", s_.rearrange("p (k f) -> p k f", k=8), src_ap, "stgL%d" % slot["s"][2], [], [bs_])
            else:
                dma("pool", s_, src_ap, "stgL%d" % slot["s"][2], [], [bs_])

        def S_():
            s_, bs_, i = slot["s"]
            if src_is_kpf:
                dma("pool", dst_ap, s_, "stgS%d" % i, [bs_], dst_bufs)
            else:
                dma("pool", dst_ap, s_.rearrange("p (q d) -> p q d", q=4), "stgS%d" % i, [bs_], dst_bufs)
        return L, S_

    def queue_precast(kind, l):
        if (kind, l) in bg["queued"]:
            return
        bg["queued"].add((kind, l))
        pairs = []
        if kind == "moe":
            w1s, w3s, w2s = w1s_[l % 2], w3s_[l % 2], w2s_[l % 2]
            b_w1s, b_w3s, b_w2s = b_w1s_[l % 2], b_w3s_[l % 2], b_w2s_[l % 2]
            for e in range(32):
                pairs.append(_mk_pre(di["moe_w1"][l, e].rearrange("(k p) f -> p k f", p=128), True, w1s[e], [b_w1s[e]]))
                pairs.append(_mk_pre(di["moe_w3"][l, e].rearrange("(k p) f -> p k f", p=128), True, w3s[e], [b_w3s[e]]))
            for e in range(32):
                pairs.append(_mk_pre(di["moe_w2"][l, e], False, w2s[:, :, e, :].rearrange("q f d -> f q d"), b_w2s))
        else:
            i_ = l // 2
            for m in range(48):
                pairs.append(_mk_pre(di["ssm_w_in"][i_, :, m * 128:(m + 1) * 128].rearrange("(k p) f -> p k f", p=128),
                                     True, wins[m], [b_wins[m]]))
        n = len(pairs)
        for i in range(n + 1):
            if i < n:
                bg["steps"].append(pairs[i][0])
            if i >= 1:
                bg["steps"].append(pairs[i - 1][1])

    def bg_pump(n):
        while n > 0 and bg["steps"]:
            bg["steps"].pop(0)()
            n -= 1

    def bg_flush():
        bg_pump(1 << 30)

    AW = cfg.get("arena_words", 41616)
    arena = sb("arena", [128, AW], F32)
    ar = {"off": 0}

    def carve(shape, dt):
        n = 1
        for s_ in shape[1:]:
            n *= s_
        words = (n * (4 if dt == F32 else 2) + 3) // 4
        o = ar["off"]
        assert o + words <= AW, ("arena overflow", o, words, AW)
        ar["off"] = o + words
        v = arena[0:shape[0], o:o + words]
        if dt != F32:
            v = v.bitcast(dt)
        if len(shape) == 3:
            v = v.rearrange("p (a b) -> p a b", a=shape[1])
        elif len(shape) == 4:
            v = v.rearrange("p (a b c) -> p a b c", a=shape[1], b=shape[2])
        elif len(shape) == 5:
            v = v.rearrange("p (a b c d) -> p a b c d", a=shape[1], b=shape[2], c=shape[3])
        return v

    def new_phase():
        P.barrier()
        ar["off"] = 0

    SH1, A1, G1, SH2, A2, G2 = [slice(i * D, (i + 1) * D) for i in range(6)]

    def ada_phase(l):
        new_phase()
        ring = Ring([carve([128, 8, 512], F32) for _ in range(2)], "adaw")
        tmpw = carve([128, D], F32)
        b_tmpw = Buf("tmpw")
        dma("sync", mod_bc[:], di["ada_b"][l, :].partition_broadcast(128), "modb", [], [b_mod])
        for n in range(12):
            sl, bsl = ring.next()
            dma("sync", sl, di["ada_w"][l, :, n * 512:(n + 1) * 512].rearrange("(k p) n -> p k n", p=128),
                "adaw%d" % (n % 2), [], [bsl])
            for k in range(8):
                mm(pb[n % 2][:], cactB[:, k, :], sl[:, k, :], k == 0, k == 7, [bsl, b_const], [b_pb[n % 2]])
            tt("dve", mod_bc[:, n * 512:(n + 1) * 512], pb[n % 2][:], mod_bc[:, n * 512:(n + 1) * 512], ALU.add,
               [b_pb[n % 2], b_mod], [b_mod])
        for (nm, sl_) in (("norm1_w", A1), ("norm2_w", A2)):
            dma("sync", tmpw, di[nm][l, :].partition_broadcast(128), "tmpw", [], [b_tmpw])
            stt("dve", mod_bc[:, sl_], mod_bc[:, sl_], 1.0, tmpw, ALU.add, ALU.mult, [b_mod, b_tmpw], [b_mod])

    def make_norm(src_rows, src_bufs, Asl, Ssl, lean=False):
        st = {}
        st["xin"] = Ring([carve([128, D], F32) for _ in range(2)], "xin")
        st["hf"] = None if lean else Ring([carve([128, D], F32)], "hf")
        st["hb"] = Ring([carve([128, D], BF16) for _ in range(2)], "hb")
        st["junk"] = (carve([128, D], BF16), Buf("junk"))
        st["sm"] = Ring([carve([128, 4], F32) for _ in range(2)], "nsm")
        st["cnt"] = 0

        def norm_ew(blk):
            xin, bx = st["xin"].next()
            hb, bhb = st["hb"].next()
            hf, bhf = (hb, bhb) if lean else st["hf"].next()
            junk, bj = st["junk"]
            sm, bsm = st["sm"].next()
            dma("sync", xin, src_rows(blk), "xin%d" % ((st["xin"].i - 1) % 2), [src_bufs[blk]], [bx])
            memset("dve", sm[:, 0:1], 0.0, [bsm])
            act(junk, xin, AF.Square, [bx, bsm], [bj, bsm], accum_out=sm[:, 0:1])
            act(sm[:, 1:2], sm[:, 0:1], AF.Ln, [bsm], [bsm], bias=EPS, scale=1.0 / D)
            act(sm[:, 2:3], sm[:, 1:2], AF.Exp, [bsm], [bsm], scale=-0.5)
            stt("dve", hf, xin, sm[:, 2:3], mod_bc[:, Asl], ALU.mult, ALU.mult, [bx, bsm, b_mod], [bhf])
            tt("dve", hb, hf, mod_bc[:, Ssl], ALU.add, [bhf, b_mod] if not lean else [bhb, b_mod], [bhb])
            return hb, bhb

        def norm_tr(hb, bhb, hT, hT_buf, col0):
            pbank = pc[1 + st["cnt"] % 2]
            bbank = b_pc[1 + st["cnt"] % 2]
            st["cnt"] += 1
            pv = pbank[:].bitcast(BF16)
            for k in range(8):
                trn(pv[:, k * 128:(k + 1) * 128], hb[:, k * 128:(k + 1) * 128], [bhb, b_const], [bbank])
            act(hT[:, :, col0:col0 + 128], pv.rearrange("p (k t) -> p k t", k=8), AF.Copy, [bbank], [hT_buf])

        def norm_block(blk, hT, hT_buf, col0):
            hb, bhb = norm_ew(blk)
            norm_tr(hb, bhb, hT, hT_buf, col0)
        norm_block.ew = norm_ew
        norm_block.tr = norm_tr
        return norm_block

    def moe_phase(l, src_rows, src_bufs):
        queue_precast("moe", l)
        bg_flush()
        new_phase()
        w1s, w3s, w2s = w1s_[l % 2], w3s_[l % 2], w2s_[l % 2]
        b_w1s, b_w3s, b_w2s = b_w1s_[l % 2], b_w3s_[l % 2], b_w2s_[l % 2]
        if l + 1 in layers and (l + 1) % 2 == 1:
            if do_mixer:
                queue_precast("ssd", l + 1)
            if do_moe:
                queue_precast("moe", l + 1)
        per_tile = (len(bg["steps"]) + NTc - 1) // max(NTc, 1)
        hTs = [carve([128, 8, 512], BF16) for _ in range(2)]
        hTbs = [[Buf("hT%d_%d" % (i, j)) for j in range(4)] for i in range(2)]
        norm_block = make_norm(src_rows, src_bufs, A2, SH2)
        wr = carve([128, 8, 36], BF16)
        b_wr = Buf("wr")
        rb_bc = carve([128, 36], F32)
        hid = carve([128, 32, 512], BF16)
        hid_b = [Buf("hid%d" % e) for e in range(32)]
        w2r = Ring([carve([128, 32, 256], BF16) for _ in range(3)], "w2q")
        w13 = Ring([carve([128, 2, 8, 128], BF16) for _ in range(3)], "w13")
        sTr = Ring([carve([128, 512], BF16) for _ in range(2)], "sT")
        tTr = Ring([carve([128, 512], BF16) for _ in range(2)], "tT")
        combT = Ring([carve([32, 512], BF16) for _ in range(2)], "combT")
        xout = carve([128, 4, D], F32)
        xout_b = [Buf("xout%d" % j) for j in range(4)]
        tmpq = Ring([carve([128, 256], F32) for _ in range(2)], "tmpq")
        rs = Ring([carve([128, 160], F32) for _ in range(2)], "rs")
        combb = Ring([carve([128, 32], BF16) for _ in range(2)], "combb")

        dma("pool", wr[:, :, 0:4], di["moe_w_group"][l].rearrange("(k p) g -> p k g", p=128), "wr", [], [b_wr])
        dma("pool", wr[:, :, 4:36], di["moe_w_expert"][l].rearrange("(k p) g e -> p k (g e)", p=128), "wr", [], [b_wr])
        dma("sync", rb_bc[:, 0:4], di["moe_b_group"][l, :].partition_broadcast(128), "rbbc", [], [b_wr])
        dma("sync", rb_bc[:, 4:36], di["moe_b_expert"][l].rearrange("g e -> (g e)").partition_broadcast(128),
            "rbbc", [], [b_wr])

        def nr_parts(t):
            hT, hTb = hTs[t % 2], hTbs[t % 2]
            cT_, b_cT = combT.next()
            stt_ = {}

            def A(j):
                stt_[("hb", j)] = norm_block.ew(t * 4 + j)

            def B(j):
                hb, bhb = stt_[("hb", j)]
                norm_block.tr(hb, bhb, hT, hTb[j], j * 128)
                r, br = rs.next()
                lg = r[:, 0:36]
                for k in range(8):
                    mm(pc[0][:, 0:36], hT[:, k, j * 128:(j + 1) * 128], wr[:, k, :], k == 0, k == 7,
                       [hTb[j], b_wr], [b_pc[0]])
                tt("dve", lg, pc[0][:, 0:36], rb_bc, ALU.add, [b_pc[0], b_wr], [br])
                gmax = r[:, 40:41]; ngmax = r[:, 41:42]; gsum = r[:, 42:43]; gval = r[:, 43:44]
                P.op("dve", lambda e, o=gmax, i=r[:, 0:4]: e.tensor_reduce(out=o, in_=i, axis=AX.X, op=ALU.max), [br], [br])
                ts("dve", ngmax, gmax, -1.0, ALU.mult, [br], [br])
                memset("dve", gsum, 0.0, [br])
                act(r[:, 44:48], r[:, 0:4], AF.Exp, [br], [br], bias=ngmax, scale=1.0, accum_out=gsum)
                P.op("dve", lambda e, o=gval, i=gsum: e.reciprocal(out=o, in_=i), [br], [br])
                gmask = r[:, 48:52]
                ts("dve", gmask, r[:, 0:4], gmax, ALU.is_equal, [br], [br])
                ts("dve", gmask, gmask, 1.0, ALU.subtract, [br], [br], s2=30000.0, op1=ALU.mult)
                em = r[:, 56:88]
                tt("dve", em.rearrange("p (g e) -> p g e", g=4), r[:, 4:36].rearrange("p (g e) -> p g e", g=4),
                   gmask.unsqueeze(2).to_broadcast([128, 4, 8]), ALU.add, [br], [br])
                top8 = r[:, 88:96]
                P.op("dve", lambda e, o=top8, i=em: e.max(out=o, in_=i), [br], [br])
                eq1 = r[:, 96:128]; eq2 = r[:, 128:160]
                ts("dve", eq1, em, top8[:, 0:1], ALU.is_equal, [br], [br])
                ts("dve", eq2, em, top8[:, 1:2], ALU.is_equal, [br], [br])
                dd = r[:, 36:37]; ex = r[:, 37:38]; w1g = r[:, 38:39]; w2g = r[:, 39:40]
                tt("dve", dd, top8[:, 1:2], top8[:, 0:1], ALU.subtract, [br], [br])
                act(ex, dd, AF.Exp, [br], [br])
                ts("dve", w1g, ex, 1.0, ALU.add, [br], [br])
                P.op("dve", lambda e, o=w1g, i=w1g: e.reciprocal(out=o, in_=i), [br], [br])
                tt("dve", w2g, ex, w1g, ALU.mult, [br], [br])
                tt("dve", w1g, w1g, gval, ALU.mult, [br], [br])
                tt("dve", w2g, w2g, gval, ALU.mult, [br], [br])
                ts("dve", eq1, eq1, w1g, ALU.mult, [br], [br])
                stt("dve", eq1, eq2, w2g, eq1, ALU.mult, ALU.add, [br], [br])
                cb_, bcb = combb.next()
                cp("dve", cb_, eq1, [br], [bcb])
                stt_[("cb", j)] = (cb_, bcb)

            def C(j):
                cb_, bcb = stt_[("cb", j)]
                pv = pc[0][:].bitcast(BF16)
                P.op("pe", lambda e, o=pv[0:32, 128:256], i=cb_: e.transpose(o, i, ident_bf[:]), [bcb, b_const], [b_pc[0]])
                cp("dve", cT_[:, j * 128:(j + 1) * 128], pv[0:32, 128:256], [b_pc[0]], [b_cT])
            return A, B, C, (cT_, b_cT)

        def norm_router(t):
            A, B, C, ret = nr_parts(t)
            for j in range(4):
                A(j)
                B(j)
                C(j)
            return ret

        def pass1(t, cT_, b_cT):
            hT, hTb = hTs[t % 2], hTbs[t % 2]
            for e_ in range(32):
                w_, bw = w13.next()
                key = "w13_%d" % ((w13.i - 1) % 3)
                dma("sync", w_[:, 0], w1s[e_].rearrange("p (k f) -> p k f", k=8), key, [b_w1s[e_]], [bw])
                dma("sync", w_[:, 1], w3s[e_].rearrange("p (k f) -> p k f", k=8), key, [b_w3s[e_]], [bw])
                if e_ % 2 == 0:
                    h1, bh1, h3, bh3, cbk, bcbk = pb[0][:], b_pb[0], pb[1][:], b_pb[1], pb[2][:], b_pb[2]
                else:
                    h1, bh1, h3, bh3, cbk, bcbk = pS[:, 0:512], b_pS[0], pS[:, 512:1024], b_pS[1], pc[0][:], b_pc[0]
                for k in range(8):
                    mm(h1, w_[:, 0, k, :], hT[:, k, :], k == 0, k == 7, [bw] + hTb, [bh1])
                for k in range(8):
                    mm(h3, w_[:, 1, k, :], hT[:, k, :], k == 0, k == 7, [bw] + hTb, [bh3])
                mm(cbk, SelE[:, e_, :], cT_, True, True, [b_const, b_cT], [bcbk])
                sT, bsT = sTr.next()
                tT, btT = tTr.next()
                act(sT, h1, AF.Silu, [bh1], [bsT])
                tt("dve", tT, h3, sT, ALU.mult, [bh3, bsT], [btT])
                tt("dve", hid[:, e_, :], cbk, tT, ALU.mult, [bcbk, btT], [hid_b[e_]])

        w2slots = {}

        def load_w2(t, q):
            w2_, bw2 = w2r.next()
            dma("sync", w2_, w2s[q], "w2q_%d" % ((w2r.i - 1) % 3), [b_w2s[q]], [bw2])
            w2slots[(t, q)] = (w2_, bw2)

        def pass2(t, nxt=None):
            bg_pump(per_tile)
            for j in range(4):
                blk = t * 4 + j
                dma("sync", xout[:, j, :], src_rows(blk), "xout%d" % j, [src_bufs[blk]], [xout_b[j]])
            for q in range(4):
                if nxt is not None:
                    nxt[0](q)
                w2_, bw2 = w2slots[(t, q)]
                if q + 2 < 4:
                    load_w2(t, q + 2)
                elif t + 1 < NTc:
                    load_w2(t + 1, q - 2)
                for j in range(4):
                    bi = 1 + (q * 4 + j) % 2
                    for e_ in range(32):
                        mm(pc[bi][:, 0:256], hid[:, e_, j * 128:(j + 1) * 128], w2_[:, e_, :], e_ == 0, e_ == 31,
                           [hid_b[e_], bw2], [b_pc[bi]])
                    tq, btq = tmpq.next()
                    tt("dve", tq, pc[bi][:, 0:256], mod_bc[:, 5 * D + q * 256:5 * D + (q + 1) * 256], ALU.mult,
                       [b_pc[bi], b_mod], [btq])
                    tt("dve", xout[:, j, q * 256:(q + 1) * 256], tq, xout[:, j, q * 256:(q + 1) * 256], ALU.add,
                       [btq, xout_b[j]], [xout_b[j]])
                if nxt is not None:
                    nxt[1](q)
                    if q >= 1:
                        nxt[2](q - 1)
            for j in range(4):
                blk = t * 4 + j
                dma("sync", xr[blk * 128:(blk + 1) * 128, :], xout[:, j, :], "xo%d" % j, [xout_b[j]], [xr_b[blk]])

        pend = norm_router(0)
        load_w2(0, 0)
        load_w2(0, 1)
        for t in range(NTc):
            pass1(t, *pend)
            if t + 1 < NTc:
                A, B, C, pend = nr_parts(t + 1)
                pass2(t, (A, B, C))
                C(3)
            else:
                pass2(t)

    def even_phase(l, src_rows, src_bufs):
        i_ = l // 2
        new_phase()
        if do_moe:
            queue_precast("moe", l)
        per_tile = (len(bg["steps"]) + NTc - 1) // max(NTc, 1)
        hT = carve([128, 8, 512], BF16)
        hTb = [Buf("hT%d" % j) for j in range(4)]
        norm_block = make_norm(src_rows, src_bufs, A1, SH1)
        Win = carve([128, 8, 2048], BF16); b_win = Buf("win")
        Wout = carve([128, 8, D], BF16); b_wout = Buf("wout")
        poolw = carve([128, 4, 128], BF16)
        pscale = carve([128, 4], F32)
        expb = carve([128, 8, 640], BF16); b_expb = Buf("expb")
        kT = carve([128, 4, 1024], BF16)
        kT_b = [Buf("kT%d" % s_) for s_ in range(8)]
        vaug = carve([128, 8, 4, 2, 128], BF16)
        va_b = [Buf("va%d" % s_) for s_ in range(8)]
        qT = carve([128, 4, 512], BF16); b_q = Buf("qT")
        ubuf = carve([128, 4, 527], F32); b_u = [Buf("u%d" % m) for m in range(4)]
        sA = carve([128, 527], F32); sB = carve([128, 527], F32); b_sA = Buf("sA"); b_sB = Buf("sB")
        pooled = Ring([carve([128, 512], BF16) for _ in range(2)], "pooled")
        invc0 = carve([128, 4, 16], F32)
        catT = carve([128, 8, 512], BF16); cat_b = [Buf("cat%d" % m) for m in range(8)]
        Pexp = Ring([carve([128, 640], BF16) for _ in range(2)], "Pexp")
        PT = Ring([carve([128, 640], BF16) for _ in range(2)], "PT")
        rsr = Ring([carve([128, 128], F32) for _ in range(2)], "rsr")
        xres = Ring([carve([128, D], F32) for _ in range(1)], "xres")
        xo = Ring([carve([128, D], F32) for _ in range(1)], "xo")
        hk = carve([128, 5, 128], F32); b_hk = Buf("hk")
        rb8 = xo.slots[0][0][0:8, :]; b_rb8 = xo.slots[0][1]

        for k in range(8):
            dma("pool", Win[:, k, :], di["mix_w_in"][i_, k * 128:(k + 1) * 128, :], "win", [], [b_win])
            dma("pool", Wout[:, k, :], di["mix_w_out"][i_, k * 128:(k + 1) * 128, :], "wout", [], [b_wout])
        dma("pool", poolw, di["pool_w"][i_].rearrange("g c d -> c g d"), "win", [], [b_win])
        dma("sync", pscale, di["pool_scale"][i_, :].rearrange("(m p) -> p m", p=128), "pscale", [], [b_win],
            allow_slow_non_contiguous=True)
        memset("pool", vaug[:, :, :, 0, 64:128], 1.0, va_b)
        memset("pool", vaug[:, :, :, 1, 0:64], 1.0, va_b)
        memset("pool", ubuf[:], 0.0, b_u)
        for m in range(4):
            w = 2 ** (m + 1)
            memset("pool", invc0[:, m, :], 1.0 / w, [b_const])
            for pos in range(w - 1):
                memset("pool", invc0[:, m, pos:pos + 1], 1.0 / (pos + 1), [b_const])
        dma("sync", rb8[:, 0:513], di["rel_bias"][i_], "rb8", [], [b_rb8])
        cp("dve", rb8[:, 513:897], rb8[:, 512:513].to_broadcast([8, 384]), [b_rb8], [b_rb8])
        dma("sync", rbx[:, 0:768], rb8[:, 129:897], "rbx", [b_rb8], [b_rbx])
        for h in range(8):
            src = bass.AP(rbx.tensor, h * 1024, [[1, 128], [128, 5], [1, 128]])
            dma("sync", hk, src, "hk", [b_rbx], [b_hk])
            hk2 = hk.rearrange("p a b -> p (a b)")
            mm(pS[:, 0:512], Jm[:], hk2[:, 0:512], True, True, [b_const, b_hk], [b_pS[0]])
            mm(pS[:, 512:640], Jm[:], hk2[:, 512:640], True, True, [b_const, b_hk], [b_pS[1]])
            act(expb[:, h, :], pS[:, 0:640], AF.Exp, b_pS, [b_expb])
        memset("pool", expb[64:128, :, 0:64], 0.0, [b_expb])
        memset("pool", expb[0:64, :, 4 * 128 + 64:5 * 128], 0.0, [b_expb])

        obr = Ring([pc[0][:, i * 128:(i + 1) * 128] for i in range(4)], "ob")
        pcnt = [0]

        def pbank():
            i = pcnt[0] % 3
            pcnt[0] += 1
            return pb[i], b_pb[i]

        for t in range(NTc):
            for j in range(4):
                norm_block(t * 4 + j, hT, hTb[j], j * 128)
            bg_pump(per_tile)
            for m in range(4):
                bank, bb = pbank()
                for k in range(8):
                    mm(bank[:], Win[:, k, m * 128:(m + 1) * 128], hT[:, k, :], k == 0, k == 7, [b_win] + hTb, [bb])
                if t > 0:
                    cp("dve", ubuf[:, m, 0:15], ubuf[:, m, 512:527], [b_u[m]], [b_u[m]])
                act(ubuf[:, m, 15:527], bank[:], AF.Copy, [bb], [b_u[m]])
                u = ubuf[:, m, :]
                tt("dve", sA[:, 1:527], u[:, 1:527], u[:, 0:526], ALU.add, [b_u[m]], [b_sA])
                cur, bcur = sA, b_sA
                if m >= 1:
                    tt("dve", sB[:, 3:527], sA[:, 3:527], sA[:, 1:525], ALU.add, [b_sA], [b_sB])
                    cur, bcur = sB, b_sB
                if m >= 2:
                    tt("dve", sA[:, 7:527], sB[:, 7:527], sB[:, 3:523], ALU.add, [b_sB], [b_sA])
                    cur, bcur = sA, b_sA
                if m >= 3:
                    tt("dve", sB[:, 15:527], sA[:, 15:527], sA[:, 7:519], ALU.add, [b_sA], [b_sB])
                    cur, bcur = sB, b_sB
                pl, bpl = pooled.next()
                w = 2 ** (m + 1)
                stt("dve", pl, cur[:, 15:527], 1.0 / w, u[:, 15:527], ALU.mult, ALU.subtract, [bcur, b_u[m]], [bpl])
                if t == 0:
                    tt("dve", cur[:, 15:31], cur[:, 15:31], invc0[:, m, :], ALU.mult, [bcur, b_const], [bcur])
                    tt("dve", pl[:, 0:16], cur[:, 15:31], u[:, 15:31], ALU.subtract, [bcur, b_u[m]], [bpl])
                bank, bb = pbank()
                mm(bank[:], poolw[:, m, :], pl, True, True, [b_win, bpl], [bb])
                ts("dve", catT[:, m, :], bank[:], pscale[:, m:m + 1], ALU.mult, [bb, b_win], [cat_b[m]])
            for m in range(4):
                bank, bb = pbank()
                for k in range(8):
                    mm(bank[:], Win[:, k, 512 + m * 128:512 + (m + 1) * 128], hT[:, k, :], k == 0, k == 7,
                       [b_win] + hTb, [bb])
                act(qT[:, m, :], bank[:], AF.Copy, [bb], [b_q])
            ks0 = (t % 2) * 4
            for m in range(4):
                bank, bb = pbank()
                for k in range(8):
                    mm(bank[:], Win[:, k, 1024 + m * 128:1024 + (m + 1) * 128], hT[:, k, :], k == 0, k == 7,
                       [b_win] + hTb, [bb])
                act(kT[:, m, ks0 * 128:(ks0 + 4) * 128], bank[:], AF.Copy, [bb], kT_b[ks0:ks0 + 4])
            for j in range(4):
                sl = ks0 + j
                bank, bb = pbank()
                for k in range(8):
                    mm(bank[:], hT[:, k, j * 128:(j + 1) * 128], Win[:, k, 1536:2048], k == 0, k == 7,
                       [b_win, hTb[j]], [bb])
                pvv = bank[:].rearrange("p (c two d) -> p c two d", c=4, two=2)
                cp("dve", vaug[:, sl, :, 0, 0:64], pvv[:, :, 0, :], [bb], [va_b[sl]])
                cp("dve", vaug[:, sl, :, 1, 64:128], pvv[:, :, 1, :], [bb], [va_b[sl]])
            for j in range(4):
                jq = t * 4 + j
                blocks = [i for i in range(jq - 4, jq + 1) if i >= 0]
                for h in range(8):
                    c, hp = h // 2, h % 2
                    p0 = 64 * hp
                    for i in blocks:
                        o = jq - i
                        sl = i % 8
                        mm(pS[:, o * 128:(o + 1) * 128], kT[p0:p0 + 64, c, sl * 128:(sl + 1) * 128],
                           qT[p0:p0 + 64, c, j * 128:(j + 1) * 128], True, True, [kT_b[sl], b_q], [b_pS[o // 4]])
                    o_lo = jq - blocks[-1]
                    o_hi = jq - blocks[0]
                    c0, c1 = o_lo * 128, (o_hi + 1) * 128
                    pe_, bpe = Pexp.next()
                    pt_, bpt = PT.next()
                    rd = [b_pS[0]] + ([b_pS[1]] if o_hi == 4 else [])
                    act(pe_[:, c0:c1], pS[:, c0:c1], AF.Exp, rd, [bpe], scale=0.125)
                    tt("dve", pt_[:, c0:c1], pe_[:, c0:c1], expb[:, h, c0:c1], ALU.mult, [bpe, b_expb], [bpt])
                    ob, _ = obr.next()
                    bob = b_pc[0]
                    for n_, i in enumerate(blocks):
                        o = jq - i
                        sl = i % 8
                        mm(ob, vaug[:, sl, c, hp, :], pt_[:, o * 128:(o + 1) * 128], n_ == 0,
                           n_ == len(blocks) - 1, [va_b[sl], bpt], [bob])
                    r_, br_ = rsr.next()
                    q0 = 64 * (1 - hp)
                    P.op("dve", lambda e, o_=r_[p0:p0 + 64, :], i_2=ob[q0:q0 + 64, :]: e.reciprocal(out=o_, in_=i_2),
                         [bob], [br_])
                    tt("dve", catT[p0:p0 + 64, 4 + c, j * 128:(j + 1) * 128], ob[p0:p0 + 64, :], r_[p0:p0 + 64, :],
                       ALU.mult, [bob, br_], [cat_b[4 + c]])
            for j in range(4):
                blk = t * 4 + j
                xr_, bxr = xres.next()
                xo_, bxo = xo.next()
                dma("sync", xr_, src_rows(blk), "xres0", [src_bufs[blk]], [bxr])
                for half in range(2):
                    bank, bb = pbank()
                    for kk in range(8):
                        mm(bank[:], catT[:, kk, j * 128:(j + 1) * 128], Wout[:, kk, half * 512:(half + 1) * 512],
                           kk == 0, kk == 7, [cat_b[kk], b_wout], [bb])
                    tt("dve", xo_[:, half * 512:(half + 1) * 512], bank[:],
                       mod_bc[:, 2 * D + half * 512:2 * D + (half + 1) * 512], ALU.mult, [bb, b_mod], [bxo])
                    tt("dve", xo_[:, half * 512:(half + 1) * 512], xo_[:, half * 512:(half + 1) * 512],
                       xr_[:, half * 512:(half + 1) * 512], ALU.add, [bxo, bxr], [bxo])
                dma("sync", xr[blk * 128:(blk + 1) * 128, :], xo_, "xo0", [bxo], [xr_b[blk]])

    def ssd_phase(l, src_rows, src_bufs):
        i_ = l // 2
        queue_precast("ssd", l)
        bg_flush()
        new_phase()
        per_tile = (len(bg["steps"]) + NTc * 2 - 1) // max(NTc * 2, 1)
        TT_ = 256
        hT = carve([128, 8, TT_], BF16)
        hTb = [Buf("hT%d" % j) for j in range(2)]
        norm_block = make_norm(src_rows, src_bufs, A1, SH1, lean=True)
        Wout = carve([128, 16, D], BF16); b_wout = Buf("wout")
        wch = Ring([carve([128, 8, 128], BF16) for _ in range(3)], "wch")
        wz = Ring([carve([128, 2, 8, 128], BF16) for _ in range(2)], "wz")
        wdt = carve([128, 8, 32], BF16); b_wdt = Buf("wdt")
        cw = carve([128, 4, 32], F32); cbias = carve([128, 32], F32); b_cw = Buf("cw")
        halo = carve([128, 32, 3], F32); b_halo = [Buf("halo%d" % m) for m in range(32)]
        rawb = Ring([carve([128, TT_ + 3], F32) for _ in range(2)], "rawb")
        accr = Ring([carve([128, TT_], F32) for _ in range(2)], "acc")
        xT = carve([128, 16, TT_], BF16); BT = carve([128, 8, TT_], BF16); CT = carve([128, 8, TT_], BF16)
        xT_b = [Buf("xT%d" % m) for m in range(16)]; BT_b = [Buf("BT%d" % m) for m in range(8)]
        CT_b = [Buf("CT%d" % m) for m in range(8)]
        xtm = Ring([carve([128, 2048], BF16)], "xtm")
        xdt = Ring([carve([128, 2048], BF16)], "xdt")
        xdd = Ring([carve([128, 2048], BF16)], "xdd")
        Btm = Ring([carve([128, 1024], BF16)], "Btm")
        zs = Ring([carve([128, 2048], BF16) for _ in range(2)], "zs")
        smr = Ring([carve([128, 320], F32) for _ in range(2)], "ssm_sm")
        ahl = Ring([carve([128, 64], BF16) for _ in range(2)], "ahl")
        LTr = Ring([carve([128, 4, 128], BF16) for _ in range(2)], "LT")
        MTr = Ring([carve([128, 4, 128], BF16) for _ in range(2)], "MT")
        Gsr = Ring([carve([128, 128], BF16) for _ in range(2)], "Gs")
        state = carve([128, 8, 256], F32); stateb = carve([128, 8, 256], BF16)
        st_b = [Buf("st%d" % g) for g in range(8)]; stb_b = [Buf("stb%d" % g) for g in range(8)]
        ya = carve([128, 2048], F32); b_ya = [Buf("ya%d" % g) for g in range(8)]
        xD = carve([128, 2048], BF16); b_xD = Buf("xD")
        gn = carve([128, 2048], BF16); b_gn_h = [Buf("gn0"), Buf("gn1")]
        gnT = carve([128, 16, 128], BF16); b_gnT = [Buf("gnT0"), Buf("gnT1")]
        nw_bc = carve([128, 2048], BF16); vec_bc = carve([128, 96], F32); b_vec = Buf("vec")
        xres = Ring([carve([128, D], F32)], "xres")
        xo = Ring([carve([128, D], F32)], "xo")
        junk2 = carve([128, 256], BF16); b_j2 = Buf("junk2")

        for k in range(16):
            dma("pool", Wout[:, k, :], di["ssm_w_out"][i_, k * 128:(k + 1) * 128, :], "wout", [], [b_wout])
        dma("pool", wdt, di["ssm_w_in"][i_, :, 6144:6176].rearrange("(k p) f -> p k f", p=128), "wdt", [], [b_wdt])
        cwr = xo.slots[0][0][0:32, 0:640].rearrange("p (t c) -> p t c", t=5)
        b_cwr = xo.slots[0][1]
        for tap in range(4):
            dma("sync", cwr[:, tap, :], di["ssm_conv_w"][i_, tap, :].rearrange("(m p) -> m p", p=128), "cwr", [], [b_cwr])
        dma("sync", cwr[:, 4, :], di["ssm_conv_b"][i_, :].rearrange("(m p) -> m p", p=128), "cwr", [], [b_cwr])
        for tap in range(5):
            P.op("pe", (lambda t_: lambda e: e.transpose(pb[0][:, t_ * 32:(t_ + 1) * 32], cwr[:, t_, :], ident_f[0:32, 0:32]))(tap),
                 [b_cwr, b_const], [b_pb[0]])
        cp("dve", cw.rearrange("p t m -> p (t m)"), pb[0][:, 0:128], [b_pb[0]], [b_cw])
        cp("dve", cbias, pb[0][:, 128:160], [b_pb[0]], [b_cw])
        dma("pool", nw_bc, di["ssm_norm_w"][i_, :].partition_broadcast(128), "vecn", [], [b_vec])
        dtb_bc = vec_bc[:, 0:32]; A_bc = vec_bc[:, 32:64]; D_bc = vec_bc[:, 64:96]
        dma("sync", dtb_bc, di["ssm_dt_bias"][i_, :].partition_broadcast(128), "vec", [], [b_vec])
        dma("sync", A_bc, di["ssm_A_log"][i_, :].partition_broadcast(128), "vec", [], [b_vec])
        dma("sync", D_bc, di["ssm_D"][i_, :].partition_broadcast(128), "vec", [], [b_vec])
        act(A_bc, A_bc, AF.Exp, [b_vec], [b_vec])
        ts("dve", A_bc, A_bc, -1.0, ALU.mult, [b_vec], [b_vec])
        memset("pool", halo[:], 0.0, b_halo)
        memset("pool", state[:], 0.0, st_b)
        memset("pool", stateb[:], 0.0, stb_b)

        G_ps = pb[0][:, 0:128]
        b_small = b_pb[0]
        st_ps, b_stp = pb[0][:, 256:512], b_pb[0]
        yd_ps = pc[0][:, 0:256]
        yo_ps, b_yo = pc[0][:, 256:512], b_pc[0]
        lcnt = [0]
        hcnt = [0]

        def half():
            i = hcnt[0] % 2
            hcnt[0] += 1
            return pS[:, i * 512:(i + 1) * 512], b_pS[i]

        stop_ = cfg.get("ssd_stop", 99)
        if stop_ <= 1:
            return
        for t in range(NTc * 2):
            for j in range(2):
                norm_block(t * 2 + j, hT, hTb[j], j * 128)
            bg_pump(per_tile)
            smv = []
            zv = []
            for j in range(2):
                jc = slice(j * 128, (j + 1) * 128)
                sm, bsm = smr.next()
                dt_ = sm[:, 0:32]; a_ = sm[:, 32:64]; acs = sm[:, 64:96]; nacs = sm[:, 96:128]
                tot = sm[:, 128:160]; eacs = sm[:, 160:192]; dstt = sm[:, 192:224]; cdec = sm[:, 224:256]
                ssq = sm[:, 256:264]; lnv = sm[:, 264:272]; rstd = sm[:, 272:280]
                sp = pb[0][:, 128:160]
                for k in range(8):
                    mm(sp, hT[:, k, jc], wdt[:, k, :], k == 0, k == 7, [hTb[j], b_wdt], [b_small])
                tt("dve", dt_, sp, dtb_bc, ALU.add, [b_small, b_vec], [bsm])
                act(dt_, dt_, AF.Exp, [bsm], [bsm])
                act(dt_, dt_, AF.Ln, [bsm], [bsm], bias=1.0)
                tt("dve", a_, dt_, A_bc, ALU.mult, [bsm, b_vec], [bsm])
                mm(pb[0][:, 160:192], Tm[:], a_, True, True, [b_const, bsm], [b_small])
                mm(pb[0][:, 192:224], ones_f[:], a_, True, True, [b_const, bsm], [b_small])
                cp("dve", acs, pb[0][:, 160:192], [b_small], [bsm])
                ts("dve", nacs, acs, -1.0, ALU.mult, [bsm], [bsm])
                cp("dve", tot, pb[0][:, 192:224], [b_small], [bsm])
                act(eacs, acs, AF.Exp, [bsm], [bsm])
                tt("dve", dstt, tot, acs, ALU.subtract, [bsm], [bsm])
                act(dstt, dstt, AF.Exp, [bsm], [bsm])
                act(cdec, tot, AF.Exp, [bsm], [bsm])
                ah_, bah = ahl.next()
                cp("dve", ah_[:, 0:32], a_, [bsm], [bah])
                tt("dve", sm[:, 288:320], a_, ah_[:, 0:32], ALU.subtract, [bsm, bah], [bsm])
                cp("dve", ah_[:, 32:64], sm[:, 288:320], [bsm], [bah])
                smv.append((sm, bsm, ah_, bah))
                z_, bz = zs.next()
                for pz in range(8):
                    wz_, bwz = wz.next()
                    dma("sync", wz_, wins[2 * pz:2 * pz + 2].rearrange("c p (k f) -> p c k f", k=8),
                        "wz%d" % ((wz.i - 1) % 2), [b_wins[2 * pz], b_wins[2 * pz + 1]], [bwz])
                    bank, bb = half()
                    for k in range(8):
                        mm(bank[:, 0:256].rearrange("p (c f) -> p c f", c=2), hT[:, k, jc], wz_[:, :, k, :], k == 0, k == 7,
                           [hTb[j], bwz], [bb])
                    act(z_[:, pz * 256:(pz + 1) * 256], bank[:, 0:256], AF.Silu, [bb], [bz])
                zv.append((z_, bz))
            pend_silu = None
            for m in range(32):
                w_, bw = wch.next()
                dma("sync", w_, wins[16 + m].rearrange("p (k f) -> p k f", k=8), "wch%d" % ((wch.i - 1) % 3),
                    [b_wins[16 + m]], [bw])
                bank, bb = half()
                for k in range(8):
                    mm(bank[:, 0:TT_], w_[:, k, :], hT[:, k, :], k == 0, k == 7, [bw] + hTb, [bb])
                rw, brw = rawb.next()
                ac, bac = accr.next()
                cp("dve", rw[:, 0:3], halo[:, m, :], [b_halo[m]], [brw])
                act(rw[:, 3:TT_ + 3], bank[:, 0:TT_], AF.Copy, [bb], [brw])
                if pend_silu is not None:
                    pend_silu()
                cp("dve", halo[:, m, :], rw[:, TT_:TT_ + 3], [brw], [b_halo[m]])
                ts("dve", ac, rw[:, 0:TT_], cw[:, 0, m:m + 1], ALU.mult, [brw, b_cw], [bac])
                for tap in range(1, 4):
                    stt("dve", ac, rw[:, tap:TT_ + tap], cw[:, tap, m:m + 1], ac, ALU.mult, ALU.add,
                        [brw, b_cw, bac], [bac])
                if m < 16:
                    dst_, bd = xT[:, m, :], xT_b[m]
                elif m < 24:
                    dst_, bd = BT[:, m - 16, :], BT_b[m - 16]
                else:
                    dst_, bd = CT[:, m - 24, :], CT_b[m - 24]

                def _silu(dst_=dst_, ac=ac, bac=bac, bd=bd, m=m):
                    act(dst_, ac, AF.Silu, [bac, b_cw], [bd], bias=cbias[:, m:m + 1])
                pend_silu = _silu
            pend_silu()
            if stop_ <= 2:
                return
            for j in range(2):
                blk = t * 2 + j
                jc = slice(j * 128, (j + 1) * 128)
                sm, bsm, ah_, bah = smv[j]
                z_, bz = zv[j]
                dt_ = sm[:, 0:32]; a_ = sm[:, 32:64]; acs = sm[:, 64:96]; nacs = sm[:, 96:128]
                tot = sm[:, 128:160]; eacs = sm[:, 160:192]; dstt = sm[:, 192:224]; cdec = sm[:, 224:256]
                ssq = sm[:, 256:264]; lnv = sm[:, 264:272]; rstd = sm[:, 272:280]
                if stop_ <= 3:
                    return
                xt_, bxt = xtm.next(); xd_, bxd = xdt.next(); xdd_, bxdd = xdd.next(); bt_, bbt = Btm.next()
                for hf_ in range(2):
                    pbank, bbank = pc[1 + hf_], b_pc[1 + hf_]
                    pv = pbank[:].bitcast(BF16)
                    for k in range(8):
                        trn(pv[:, k * 128:(k + 1) * 128], xT[:, hf_ * 8 + k, jc], [xT_b[hf_ * 8 + k], b_const], [bbank])
                    cs = slice(hf_ * 1024, (hf_ + 1) * 1024)
                    act(xt_[:, cs], pv, AF.Copy, [bbank], [bxt])
                    if stop_ <= 3.2:
                        continue
                    tt("dve", xd_[:, cs].rearrange("p (h d) -> p h d", h=16), xt_[:, cs].rearrange("p (h d) -> p h d", h=16),
                       dt_[:, hf_ * 16:(hf_ + 1) * 16].unsqueeze(2).to_broadcast([128, 16, 64]), ALU.mult,
                       [bxt, bsm], [bxd])
                if stop_ <= 3.4:
                    return
                pv = pc[1][:].bitcast(BF16)
                for g in range(8):
                    trn(pv[:, g * 128:(g + 1) * 128], BT[:, g, jc], [BT_b[g], b_const], [b_pc[1]])
                act(bt_, pv, AF.Copy, [b_pc[1]], [bbt])
                tt("dve", xdd_.rearrange("p (h d) -> p h d", h=32), xd_.rearrange("p (h d) -> p h d", h=32),
                   dstt.unsqueeze(2).to_broadcast([128, 32, 64]), ALU.mult, [bxd, bsm], [bxdd])
                tt("dve", xD.rearrange("p (h d) -> p h d", h=32), xt_.rearrange("p (h d) -> p h d", h=32),
                   D_bc.unsqueeze(2).to_broadcast([128, 32, 64]), ALU.mult, [bxt, b_vec], [b_xD])
                if stop_ <= 4:
                    return
                def banks(g):
                    if g % 2 == 0:
                        return (pb[0][:, 0:128], b_pb[0], pb[0][:, 256:512], b_pb[0], pb[1], b_pb[1],
                                pc[0][:, 0:256], b_pc[0], pc[0][:, 256:512], b_pc[0])
                    return (pS[:, 0:128], b_pS[0], pS[:, 256:512], b_pS[0], pb[2], b_pb[2],
                            pS[:, 512:768], b_pS[1], pS[:, 768:1024], b_pS[1])

                def preA(g):
                    Gp, bG, st_ps, b_stp, Dp, bDp, yd, byd, yo_ps, b_yo = banks(g)
                    mm(Gp, BT[:, g, jc], CT[:, g, jc], True, True, [BT_b[g], CT_b[g]], [bG])
                    LT, bLT = LTr.next()
                    for r in range(4):
                        h = 4 * g + r
                        reg = Dp[:, r * 128:(r + 1) * 128]
                        mm(reg, ah_[:, h:h + 1].to_broadcast([128, 128]), Tm_bf[:], True, False, [bah, b_const], [bDp])
                        mm(reg, ah_[:, 32 + h:33 + h].to_broadcast([128, 128]), Tm_bf[:], False, False, [bah, b_const], [bDp])
                        mm(reg, ident_bf[:], negm[:], False, True, [b_const], [bDp])
                    for r in range(4):
                        h = 4 * g + r
                        act(LT[:, r, :], Dp[:, r * 128:(r + 1) * 128], AF.Exp, [bDp, bsm], [bLT], bias=nacs[:, h:h + 1])
                    Gs, bGs = Gsr.next()
                    act(Gs, Gp, AF.Copy, [bG], [bGs])
                    return LT, bLT, Gs, bGs

                def preB(g, LT, bLT, Gs, bGs):
                    MT, bMT = MTr.next()
                    tt("dve", MT, LT, Gs.unsqueeze(1).to_broadcast([128, 4, 128]), ALU.mult, [bLT, bGs], [bMT])
                    return MT, bMT

                def post(g, MT, bMT):
                    Gp, bG, st_ps, b_stp, Dp, bDp, yd, byd, yo_ps, b_yo = banks(g)
                    for r in range(4):
                        h = 4 * g + r
                        mm(yd[:, r * 64:(r + 1) * 64], MT[:, r, :], xd_[:, h * 64:(h + 1) * 64], True, True, [bMT, bxd], [byd])
                    gs = slice(g * 256, (g + 1) * 256)
                    mm(yo_ps, CT[:, g, jc], stateb[:, g, :], True, True, [CT_b[g], stb_b[g]], [b_yo])
                    mm(st_ps, bt_[:, g * 128:(g + 1) * 128], xdd_[:, gs], True, True, [bbt, bxdd], [b_stp])

                def post_ew(g):
                    Gp, bG, st_ps, b_stp, Dp, bDp, yd, byd, yo_ps, b_yo = banks(g)
                    gs = slice(g * 256, (g + 1) * 256)
                    tt("dve", ya[:, gs].rearrange("p (h d) -> p h d", h=4), yo_ps.rearrange("p (h d) -> p h d", h=4),
                       eacs[:, 4 * g:4 * g + 4].unsqueeze(2).to_broadcast([128, 4, 64]), ALU.mult, [b_yo, bsm], [b_ya[g]])
                    tt("dve", ya[:, gs], ya[:, gs], yd, ALU.add, [b_ya[g], byd], [b_ya[g]])
                    tt("dve", state[:, g, :].rearrange("p (h d) -> p h d", h=4),
                       state[:, g, :].rearrange("p (h d) -> p h d", h=4),
                       cdec[:, 4 * g:4 * g + 4].unsqueeze(2).to_broadcast([128, 4, 64]), ALU.mult, [st_b[g], bsm], [st_b[g]])
                    tt("dve", state[:, g, :], state[:, g, :], st_ps, ALU.add, [st_b[g], b_stp], [st_b[g]])
                    cp("pool", stateb[:, g, :], state[:, g, :], [st_b[g]], [stb_b[g]])
                    tt("dve", ya[:, gs], ya[:, gs], xD[:, gs], ALU.add, [b_ya[g], b_xD], [b_ya[g]])
                    tt("dve", ya[:, gs], ya[:, gs], z_[:, gs], ALU.mult, [b_ya[g], bz], [b_ya[g]])
                    act(junk2, ya[:, gs], AF.Square, [b_ya[g], bsm], [b_j2, bsm], accum_out=ssq[:, g:g + 1])
                    tt("pool", ya[:, gs], ya[:, gs], nw_bc[:, gs], ALU.mult, [b_ya[g], b_vec], [b_ya[g]])

                memset("dve", ssq, 0.0, [bsm])
                pa = preA(0)
                mt = preB(0, *pa)
                for g in range(8):
                    if g + 1 < 8:
                        pa = preA(g + 1)
                    post(g, *mt)
                    post_ew(g)
                    if g + 1 < 8:
                        mt = preB(g + 1, *pa)
                if stop_ <= 5:
                    return
                act(lnv, ssq, AF.Ln, [bsm], [bsm], bias=EPS, scale=1.0 / 256)
                act(rstd, lnv, AF.Exp, [bsm], [bsm], scale=-0.5)
                for g in range(8):
                    gs = slice(g * 256, (g + 1) * 256)
                    ts("dve", gn[:, gs], ya[:, gs], rstd[:, g:g + 1], ALU.mult,
                       [b_ya[g], bsm], [b_gn_h[g // 4]])
                for hf_ in range(2):
                    pbank, bbank = pc[1 + hf_], b_pc[1 + hf_]
                    pv = pbank[:].bitcast(BF16)
                    for k in range(8):
                        trn(pv[:, k * 128:(k + 1) * 128], gn[:, (hf_ * 8 + k) * 128:(hf_ * 8 + k + 1) * 128],
                            [b_gn_h[hf_], b_const], [bbank])
                    act(gnT[:, hf_ * 8:(hf_ + 1) * 8, :], pv.rearrange("p (k t) -> p k t", k=8), AF.Copy, [bbank],
                        [b_gnT[hf_]])
                xr_, bxr = xres.next()
                xo_, bxo = xo.next()
                dma("sync", xr_, src_rows(blk), "xres0", [src_bufs[blk]], [bxr])
                for hh in range(2):
                    bank, bb = half()
                    for kk in range(16):
                        mm(bank, gnT[:, kk, :], Wout[:, kk, hh * 512:(hh + 1) * 512], kk == 0, kk == 15,
                           b_gnT + [b_wout], [bb])
                    cs = slice(hh * 512, (hh + 1) * 512)
                    tt("dve", xo_[:, cs], bank, mod_bc[:, 2 * D + hh * 512:2 * D + (hh + 1) * 512], ALU.mult,
                       [bb, b_mod], [bxo])
                    tt("dve", xo_[:, cs], xo_[:, cs], xr_[:, cs], ALU.add, [bxo, bxr], [bxo])
                dma("sync", xr[blk * 128:(blk + 1) * 128, :], xo_, "xo0", [bxo], [xr_b[blk]])

    def final_phase(src_rows, src_bufs):
        new_phase()
        xin = Ring([carve([128, D], F32) for _ in range(2)], "fxin")
        xo = Ring([carve([128, D], F32) for _ in range(2)], "fxo")
        junk = carve([128, D], BF16); bj = Buf("fj")
        sm = Ring([carve([128, 4], F32) for _ in range(2)], "fsm")
        fw = carve([128, D], F32); b_fw = Buf("fw")
        dma("sync", fw, di["final_norm_w"][0, :].partition_broadcast(128), "fw", [], [b_fw])
        for blk in range(NTc * 4):
            x_, bx = xin.next()
            o_, bo = xo.next()
            s_, bs = sm.next()
            dma("sync", x_, src_rows(blk), "fxin%d" % (blk % 2), [src_bufs[blk]], [bx])
            if do_final:
                memset("dve", s_[:, 0:1], 0.0, [bs])
                act(junk, x_, AF.Square, [bx, bs], [bj, bs], accum_out=s_[:, 0:1])
                act(s_[:, 1:2], s_[:, 0:1], AF.Ln, [bs], [bs], bias=EPS, scale=1.0 / D)
                act(s_[:, 2:3], s_[:, 1:2], AF.Exp, [bs], [bs], scale=-0.5)
                stt("dve", o_, x_, s_[:, 2:3], fw, ALU.mult, ALU.mult, [bx, bs, b_fw], [bo])
            else:
                cp("dve", o_, x_, [bx], [bo])
            dma("sync", y_d[blk * 128:(blk + 1) * 128, :], o_, "fxo%d" % (blk % 2), [bo], [y_b[blk]])

    src = lambda blk: di["x"][blk * 128:(blk + 1) * 128, :]
    src_b = [Buf("xsrc%d" % i) for i in range(NBLK)]
    xr_rows = lambda blk: xr[blk * 128:(blk + 1) * 128, :]
    for l in layers:
        ada_phase(l)
        if do_mixer:
            if l % 2 == 0:
                even_phase(l, src, src_b)
            else:
                ssd_phase(l, src, src_b)
            src, src_b = xr_rows, xr_b
        if do_moe:
            moe_phase(l, src, src_b)
            src, src_b = xr_rows, xr_b
    bg_flush()
    final_phase(src, src_b)
    P.barrier()
    P.emit()
    return nc


_CACHE = {}


def kernel(**inputs):
    cfg = inputs.pop("_cfg", None) or {}
    ncores = cfg.get("ncores", 8)
    key = repr(sorted(cfg.items()))
    if key not in _CACHE:
        _CACHE[key] = build(cfg)
    nc = _CACHE[key]
    shared = {}
    for name, shp in INPUT_SHAPES:
        if name in ("x", "c"):
            continue
        shared[name] = np.ascontiguousarray(np.asarray(inputs[name], dtype=np.float32).reshape(shp))
    x = np.asarray(inputs["x"], dtype=np.float32)
    c = np.asarray(inputs["c"], dtype=np.float32)
    in_maps = []
    for b in range(ncores):
        m = dict(shared)
        m["x"] = np.ascontiguousarray(x[b])
        m["c"] = np.ascontiguousarray(c[b:b + 1])
        in_maps.append(m)
    res = run_bass_kernel_spmd(nc, in_maps, core_ids=list(range(ncores)))
    out = np.stack([np.asarray(res.results[b]["y"], dtype=np.float32).reshape(S, D) for b in range(ncores)], axis=0)
    return out
```

```python
import numpy as np
import concourse.bass as bass
import concourse.mybir as mybir
from concourse.bass_utils import run_bass_kernel_spmd

F32 = mybir.dt.float32
BF16 = mybir.dt.bfloat16
AF = mybir.ActivationFunctionType
ALU = mybir.AluOpType
AX = mybir.AxisListType

ENGS = ("sync", "act", "pool", "pe", "dve")
D = 1024
S = 4096
NBLK = 32
NT = 8
EPS = 1e-6


class Buf:
    __slots__ = ("name", "w", "r")

    def __init__(self, name=""):
        self.name = name
        self.w = None
        self.r = {}


class Prog:
    def __init__(self, nc, same_eng_sync=("pool", "act", "dve")):
        self.nc = nc
        self.ops = {e: [] for e in ENGS}
        self.dma_cnt = {}
        self.same_eng_sync = set(same_eng_sync)
        self.last_real = {e: None for e in ENGS}

    def _deps_for(self, reads, writes):
        deps = set()
        for b in reads:
            if b.w is not None:
                deps.add(b.w)
        for b in writes:
            if b.w is not None:
                deps.add(b.w)
            for t in b.r.values():
                deps.add(t)
        return deps

    def _commit(self, tok, reads, writes):
        for b in writes:
            b.w = tok
            b.r = {}
        for b in reads:
            if b in writes:
                continue
            k = tok[0] if tok[0] != "dma" else ("dma", tok[1])
            b.r[k] = tok

    def op(self, eng, fn, reads=(), writes=()):
        deps = self._deps_for(reads, writes)
        idx = len(self.ops[eng])
        tok = (eng, idx)
        self.ops[eng].append([fn, deps, "op", None])
        self.last_real[eng] = tok
        self._commit(tok, reads, writes)
        return tok

    def dma(self, eng, fn, key, reads=(), writes=()):
        deps = self._deps_for(reads, writes)
        deps = set(d for d in deps if not (d[0] == "dma" and d[1] == key))
        c = self.dma_cnt.get(key, 0) + 1
        self.dma_cnt[key] = c
        tok = ("dma", key, c)
        self.ops[eng].append([fn, deps, "dma", key])
        self._commit(tok, reads, writes)
        return tok

    def barrier(self):
        toks = set(t for t in self.last_real.values() if t is not None)
        toks |= set(("dma", k, c) for k, c in self.dma_cnt.items())
        for e in ENGS:
            self.ops[e].append([None, set(toks), "op", None])

    def emit(self):
        nc = self.nc
        need = {e: set() for e in ENGS}
        for e in ENGS:
            for (fn, deps, kind, key) in self.ops[e]:
                for d in deps:
                    if d[0] != "dma":
                        if d[0] == e and e not in self.same_eng_sync:
                            continue
                        need[d[0]].add(d[1])
        msval = {}
        for e in ENGS:
            c = 0
            m = {}
            for i in sorted(need[e]):
                c += 1
                m[i] = c
            msval[e] = m
        sem = {e: nc.alloc_semaphore("s_" + e) for e in ENGS}
        dsem = {k: nc.alloc_semaphore("d_%d" % i) for i, k in enumerate(self.dma_cnt)}
        engobj = {"sync": "sync", "act": "scalar", "pool": "gpsimd", "pe": "tensor", "dve": "vector"}

        def run(e):
            def body(eng):
                seen = {}
                for i, (fn, deps, kind, key) in enumerate(self.ops[e]):
                    for d in sorted(deps, key=str):
                        if d[0] == "dma":
                            sk = ("dma", d[1]); val = 16 * d[2]; s = dsem[d[1]]
                        else:
                            if d[0] == e and e not in self.same_eng_sync:
                                continue
                            sk = d[0]; val = msval[d[0]][d[1]]; s = sem[d[0]]
                        if seen.get(sk, 0) >= val:
                            continue
                        seen[sk] = val
                        eng.wait_ge(s, val)
                    if fn is None:
                        continue
                    ins = fn(eng)
                    if kind == "dma":
                        ins.then_inc(dsem[key], 16)
                    elif i in msval[e]:
                        ins.then_inc(sem[e], 1)
            return body

        with nc.Block() as block:
            for e in ENGS:
                if self.ops[e]:
                    getattr(block, engobj[e])(run(e))


class Ring:
    def __init__(self, aps, name):
        self.slots = [(ap, Buf(name + str(i))) for i, ap in enumerate(aps)]
        self.i = 0

    def next(self):
        s = self.slots[self.i % len(self.slots)]
        self.i += 1
        return s


INPUT_SHAPES = [
    ("x", [S, D]), ("c", [1, D]), ("ada_w", [4, D, 6 * D]), ("ada_b", [4, 6 * D]),
    ("norm1_w", [4, D]), ("norm2_w", [4, D]), ("mix_w_in", [2, D, 2048]), ("pool_w", [2, 4, 128, 128]),
    ("pool_scale", [2, 512]), ("rel_bias", [2, 8, 513]), ("mix_w_out", [2, 1024, D]),
    ("ssm_w_in", [2, D, 6176]), ("ssm_conv_w", [2, 4, 4096]), ("ssm_conv_b", [2, 4096]),
    ("ssm_dt_bias", [2, 32]), ("ssm_A_log", [2, 32]), ("ssm_D", [2, 32]), ("ssm_norm_w", [2, 2048]),
    ("ssm_w_out", [2, 2048, D]), ("moe_w_group", [4, D, 4]), ("moe_b_group", [4, 4]),
    ("moe_w_expert", [4, D, 4, 8]), ("moe_b_expert", [4, 4, 8]), ("moe_w1", [4, 32, D, 128]),
    ("moe_w3", [4, 32, D, 128]), ("moe_w2", [4, 32, 128, D]), ("final_norm_w", [1, D]),
]


def build(cfg):
    layers = cfg.get("layers", [0, 1, 2, 3])
    do_final = cfg.get("final", True)
    do_mixer = cfg.get("mixer", True)
    do_moe = cfg.get("moe", True)
    NTc = cfg.get("ntiles", NT)
    nc = bass.Bass("TRN2", target_bir_lowering=False)
    P = Prog(nc)
    di = {}
    for name, shp in INPUT_SHAPES:
        di[name] = nc.dram_tensor(name, list(shp), F32, kind="ExternalInput").ap()
    y_d = nc.dram_tensor("y", [S, D], F32, kind="ExternalOutput").ap()
    xr = nc.dram_tensor("xr", [S, D], F32).ap()
    rbx = nc.dram_tensor("rbx", [8, 1024], F32).ap()
    b_rbx = Buf("rbx")
    xr_b = [Buf("xr%d" % i) for i in range(NBLK)]
    y_b = [Buf("y%d" % i) for i in range(NBLK)]

    def act(out, in_, func, r, w, **kw):
        P.op("act", lambda e: e.activation(out=out, in_=in_, func=func, **kw), r, w)

    def tt(eng, out, in0, in1, op, r, w):
        P.op(eng, lambda e: e.tensor_tensor(out=out, in0=in0, in1=in1, op=op), r, w)

    def ts(eng, out, in0, s1, op0, r, w, s2=None, op1=None):
        if op1 is None:
            P.op(eng, lambda e: e.tensor_scalar(out=out, in0=in0, scalar1=s1, scalar2=None, op0=op0), r, w)
        else:
            P.op(eng, lambda e: e.tensor_scalar(out=out, in0=in0, scalar1=s1, scalar2=s2, op0=op0, op1=op1), r, w)

    def stt(eng, out, in0, scalar, in1, op0, op1, r, w):
        P.op(eng, lambda e: e.scalar_tensor_tensor(out=out, in0=in0, scalar=scalar, in1=in1, op0=op0, op1=op1), r, w)

    def mm(out, lhsT, rhs, start, stop, r, w):
        P.op("pe", lambda e: e.matmul(out, lhsT, rhs, start=start, stop=stop), r, w)

    def trn(out, in_, r, w):
        P.op("pe", lambda e: e.transpose(out, in_, ident_bf[:]), r, w)

    def cp(eng, out, in_, r, w):
        P.op(eng, lambda e: e.tensor_copy(out=out, in_=in_), r, w)

    def dma(q, out, in_, key, r, w, **kw):
        P.dma(q, lambda e: e.dma_start(out=out, in_=in_, **kw), key, r, w)

    def memset(eng, ap, val, w):
        P.op(eng, lambda e: e.memset(ap, val), [], w)

    def asel(out, in_, pattern, cmp, fill, base, cm, r, w):
        P.op("pool", lambda e: e.affine_select(out=out, in_=in_, pattern=pattern, compare_op=cmp, fill=fill,
                                               base=base, channel_multiplier=cm), r, w)

    pb = [nc.alloc_psum_tensor("pb%d" % i, [128, 512], F32) for i in range(3)]
    pS = nc.alloc_psum_tensor("pS", [128, 1024], F32)
    pc = [nc.alloc_psum_tensor("pc%d" % i, [128, 512], F32) for i in range(3)]
    b_pb = [Buf("pb%d" % i) for i in range(3)]
    b_pS = [Buf("pS0"), Buf("pS1")]
    b_pc = [Buf("pc%d" % i) for i in range(3)]

    def sb(name, shape, dt):
        return nc.alloc_sbuf_tensor(name, list(shape), dt)

    b_const = Buf("const")
    ident_f = sb("ident_f", [128, 128], F32)
    ident_bf = sb("ident_bf", [128, 128], BF16)
    Jm = sb("Jm", [128, 128], F32)
    ones_f = sb("ones_f", [128, 128], F32)
    Tm = sb("Tm", [128, 128], F32)
    Tm_bf = sb("Tm_bf", [128, 128], BF16)
    negm_f = sb("negm_f", [128, 128], F32)
    negm = sb("negm", [128, 128], BF16)
    SelE = sb("SelE", [32, 32, 128], BF16)
    mod_bc = sb("mod_bc", [128, 6 * D], F32)
    cactB = sb("cactB", [128, 8, 128], F32)
    cT = sb("cT", [128, 8], F32)
    b_mod = Buf("mod")

    memset("pool", ident_f[:], 1.0, [b_const])
    asel(ident_f[:], ident_f[:], [[-1, 128]], ALU.is_equal, 0.0, 0, 1, [b_const], [b_const])
    cp("dve", ident_bf[:], ident_f[:], [b_const], [b_const])
    memset("pool", Jm[:], 1.0, [b_const])
    asel(Jm[:], Jm[:], [[1, 128]], ALU.is_equal, 0.0, -127, 1, [b_const], [b_const])
    memset("pool", ones_f[:], 1.0, [b_const])
    memset("pool", Tm[:], 1.0, [b_const])
    asel(Tm[:], Tm[:], [[1, 128]], ALU.is_ge, 0.0, 0, -1, [b_const], [b_const])
    cp("dve", Tm_bf[:], Tm[:], [b_const], [b_const])
    memset("pool", negm_f[:], 0.0, [b_const])
    asel(negm_f[:], negm_f[:], [[1, 128]], ALU.is_ge, -30000.0, 0, -1, [b_const], [b_const])
    cp("dve", negm[:], negm_f[:], [b_const], [b_const])
    memset("pool", SelE[:], 1.0, [b_const])
    asel(SelE[:], SelE[:], [[-1, 32], [0, 128]], ALU.is_equal, 0.0, 0, 1, [b_const], [b_const])

    dma("sync", cT[:], di["c"][0, :].rearrange("(k p) -> p k", p=128), "cT", [], [b_const],
        allow_slow_non_contiguous=True)
    act(cT[:], cT[:], AF.Silu, [b_const], [b_const])
    for k in range(8):
        cp("dve", cactB[:, k, :], cT[:, k:k + 1].to_broadcast([128, 128]), [b_const], [b_const])

    w1s_ = [nc.dram_tensor("w1s%d" % i, [32, 128, 1024], BF16).ap() for i in range(2)]
    w3s_ = [nc.dram_tensor("w3s%d" % i, [32, 128, 1024], BF16).ap() for i in range(2)]
    w2s_ = [nc.dram_tensor("w2s%d" % i, [4, 128, 32, 256], BF16).ap() for i in range(2)]
    wins = nc.dram_tensor("wins", [48, 128, 1024], BF16).ap()
    b_w1s_ = [[Buf("w1s%d" % e) for e in range(32)] for _ in range(2)]
    b_w3s_ = [[Buf("w3s%d" % e) for e in range(32)] for _ in range(2)]
    b_w2s_ = [[Buf("w2s%d" % q) for q in range(4)] for _ in range(2)]
    b_wins = [Buf("wins%d" % m) for m in range(48)]
    NSTG = 3
    stg = Ring([sb("stg%d" % i, [128, 1024], BF16)[:] for i in range(NSTG)], "stg")
    bg = {"steps": [], "queued": set()}

    def _mk_pre(src_ap, src_is_kpf, dst_ap, dst_bufs):
        slot = {}

        def L():
            s_, bs_ = stg.next()
            slot["s"] = (s_, bs_, (stg.i - 1) % NSTG)
            if src_is_kpf:
                dma("pool", s_.rearrange("p (k f) -> p k f", k=8), src_ap, "stgL%d" % slot["s"][2], [], [bs_])
            else:
                dma("pool", s_, src_ap, "stgL%d" % slot["s"][2], [], [bs_])

        def S_():
            s_, bs_, i = slot["s"]
            if src_is_kpf:
                dma("pool", dst_ap, s_, "stgS%d" % i, [bs_], dst_bufs)
            else:
                dma("pool", dst_ap, s_.rearrange("p (q d) -> p q d", q=4), "stgS%d" % i, [bs_], dst_bufs)
        return L, S_

    def queue_precast(kind, l):
        if (kind, l) in bg["queued"]:
            return
        bg["queued"].add((kind, l))
        pairs = []
        if kind == "moe":
            w1s, w3s, w2s = w1s_[l % 2], w3s_[l % 2], w2s_[l % 2]
            b_w1s, b_w3s, b_w2s = b_w1s_[l % 2], b_w3s_[l % 2], b_w2s_[l % 2]
            for e in range(32):
                pairs.append(_mk_pre(di["moe_w1"][l, e].rearrange("(k p) f -> p k f", p=128), True, w1s[e], [b_w1s[e]]))
                pairs.append(_mk_pre(di["moe_w3"][l, e].rearrange("(k p) f -> p k f", p=128), True, w3s[e], [b_w3s[e]]))
            for e in range(32):
                pairs.append(_mk_pre(di["moe_w2"][l, e], False, w2s[:, :, e, :].rearrange("q f d -> f q d"), b_w2s))
        else:
            i_ = l // 2
            for m in range(48):
                pairs.append(_mk_pre(di["ssm_w_in"][i_, :, m * 128:(m + 1) * 128].rearrange("(k p) f -> p k f", p=128),
                                     True, wins[m], [b_wins[m]]))
        n = len(pairs)
        for i in range(n + 1):
            if i < n:
                bg["steps"].append(pairs[i][0])
            if i >= 1:
                bg["steps"].append(pairs[i - 1][1])

    def bg_pump(n):
        while n > 0 and bg["steps"]:
            bg["steps"].pop(0)()
            n -= 1

    def bg_flush():
        bg_pump(1 << 30)

    AW = cfg.get("arena_words", 41616)
    arena = sb("arena", [128, AW], F32)
    ar = {"off": 0}

    def carve(shape, dt):
        n = 1
        for s_ in shape[1:]:
            n *= s_
        words = (n * (4 if dt == F32 else 2) + 3) // 4
        o = ar["off"]
        assert o + words <= AW, ("arena overflow", o, words, AW)
        ar["off"] = o + words
        v = arena[0:shape[0], o:o + words]
        if dt != F32:
            v = v.bitcast(dt)
        if len(shape) == 3:
            v = v.rearrange("p (a b) -> p a b", a=shape[1])
        elif len(shape) == 4:
            v = v.rearrange("p (a b c) -> p a b c", a=shape[1], b=shape[2])
        elif len(shape) == 5:
            v = v.rearrange("p (a b c d) -> p a b c d", a=shape[1], b=shape[2], c=shape[3])
        return v

    def new_phase():
        P.barrier()
        ar["off"] = 0

    SH1, A1, G1, SH2, A2, G2 = [slice(i * D, (i + 1) * D) for i in range(6)]

    def ada_phase(l):
        new_phase()
        ring = Ring([carve([128, 8, 512], F32) for _ in range(2)], "adaw")
        tmpw = carve([128, D], F32)
        b_tmpw = Buf("tmpw")
        dma("sync", mod_bc[:], di["ada_b"][l, :].partition_broadcast(128), "modb", [], [b_mod])
        for n in range(12):
            sl, bsl = ring.next()
            dma("sync", sl, di["ada_w"][l, :, n * 512:(n + 1) * 512].rearrange("(k p) n -> p k n", p=128),
                "adaw%d" % (n % 2), [], [bsl])
            for k in range(8):
                mm(pb[n % 2][:], cactB[:, k, :], sl[:, k, :], k == 0, k == 7, [bsl, b_const], [b_pb[n % 2]])
            tt("dve", mod_bc[:, n * 512:(n + 1) * 512], pb[n % 2][:], mod_bc[:, n * 512:(n + 1) * 512], ALU.add,
               [b_pb[n % 2], b_mod], [b_mod])
        for (nm, sl_) in (("norm1_w", A1), ("norm2_w", A2)):
            dma("sync", tmpw, di[nm][l, :].partition_broadcast(128), "tmpw", [], [b_tmpw])
            stt("dve", mod_bc[:, sl_], mod_bc[:, sl_], 1.0, tmpw, ALU.add, ALU.mult, [b_mod, b_tmpw], [b_mod])

    def make_norm(src_rows, src_bufs, Asl, Ssl, lean=False):
        st = {}
        st["xin"] = Ring([carve([128, D], F32) for _ in range(2)], "xin")
        st["hf"] = None if lean else Ring([carve([128, D], F32)], "hf")
        st["hb"] = Ring([carve([128, D], BF16) for _ in range(2)], "hb")
        st["junk"] = (carve([128, D], BF16), Buf("junk"))
        st["sm"] = Ring([carve([128, 4], F32) for _ in range(2)], "nsm")
        st["cnt"] = 0

        def norm_ew(blk):
            xin, bx = st["xin"].next()
            hb, bhb = st["hb"].next()
            hf, bhf = (hb, bhb) if lean else st["hf"].next()
            junk, bj = st["junk"]
            sm, bsm = st["sm"].next()
            dma("sync", xin, src_rows(blk), "xin%d" % ((st["xin"].i - 1) % 2), [src_bufs[blk]], [bx])
            memset("dve", sm[:, 0:1], 0.0, [bsm])
            act(junk, xin, AF.Square, [bx, bsm], [bj, bsm], accum_out=sm[:, 0:1])
            act(sm[:, 1:2], sm[:, 0:1], AF.Ln, [bsm], [bsm], bias=EPS, scale=1.0 / D)
            act(sm[:, 2:3], sm[:, 1:2], AF.Exp, [bsm], [bsm], scale=-0.5)
            stt("dve", hf, xin, sm[:, 2:3], mod_bc[:, Asl], ALU.mult, ALU.mult, [bx, bsm, b_mod], [bhf])
            tt("dve", hb, hf, mod_bc[:, Ssl], ALU.add, [bhf, b_mod] if not lean else [bhb, b_mod], [bhb])
            return hb, bhb

        def norm_tr(hb, bhb, hT, hT_buf, col0):
            pbank = pc[1 + st["cnt"] % 2]
            bbank = b_pc[1 + st["cnt"] % 2]
            st["cnt"] += 1
            pv = pbank[:].bitcast(BF16)
            for k in range(8):
                trn(pv[:, k * 128:(k + 1) * 128], hb[:, k * 128:(k + 1) * 128], [bhb, b_const], [bbank])
            act(hT[:, :, col0:col0 + 128], pv.rearrange("p (k t) -> p k t", k=8), AF.Copy, [bbank], [hT_buf])

        def norm_block(blk, hT, hT_buf, col0):
            hb, bhb = norm_ew(blk)
            norm_tr(hb, bhb, hT, hT_buf, col0)
        norm_block.ew = norm_ew
        norm_block.tr = norm_tr
        return norm_block

    def moe_phase(l, src_rows, src_bufs):
        queue_precast("moe", l)
        bg_flush()
        new_phase()
        w1s, w3s, w2s = w1s_[l % 2], w3s_[l % 2], w2s_[l % 2]
        b_w1s, b_w3s, b_w2s = b_w1s_[l % 2], b_w3s_[l % 2], b_w2s_[l % 2]
        if l + 1 in layers and (l + 1) % 2 == 1:
            if do_mixer:
                queue_precast("ssd", l + 1)
            if do_moe:
                queue_precast("moe", l + 1)
        per_tile = (len(bg["steps"]) + NTc - 1) // max(NTc, 1)
        hTs = [carve([128, 8, 512], BF16) for _ in range(2)]
        hTbs = [[Buf("hT%d_%d" % (i, j)) for j in range(4)] for i in range(2)]
        norm_block = make_norm(src_rows, src_bufs, A2, SH2)
        wr = carve([128, 8, 36], BF16)
        b_wr = Buf("wr")
        rb_bc = carve([128, 36], F32)
        hid = carve([128, 32, 512], BF16)
        hid_b = [Buf("hid%d" % e) for e in range(32)]
        w2r = Ring([carve([128, 32, 256], BF16) for _ in range(3)], "w2q")
        w13 = Ring([carve([128, 2, 8, 128], BF16) for _ in range(3)], "w13")
        sTr = Ring([carve([128, 512], BF16) for _ in range(2)], "sT")
        tTr = Ring([carve([128, 512], BF16) for _ in range(2)], "tT")
        combT = Ring([carve([32, 512], BF16) for _ in range(2)], "combT")
        xout = carve([128, 4, D], F32)
        xout_b = [Buf("xout%d" % j) for j in range(4)]
        tmpq = Ring([carve([128, 256], F32) for _ in range(2)], "tmpq")
        rs = Ring([carve([128, 160], F32) for _ in range(2)], "rs")
        combb = Ring([carve([128, 32], BF16) for _ in range(2)], "combb")

        dma("pool", wr[:, :, 0:4], di["moe_w_group"][l].rearrange("(k p) g -> p k g", p=128), "wr", [], [b_wr])
        dma("pool", wr[:, :, 4:36], di["moe_w_expert"][l].rearrange("(k p) g e -> p k (g e)", p=128), "wr", [], [b_wr])
        dma("sync", rb_bc[:, 0:4], di["moe_b_group"][l, :].partition_broadcast(128), "rbbc", [], [b_wr])
        dma("sync", rb_bc[:, 4:36], di["moe_b_expert"][l].rearrange("g e -> (g e)").partition_broadcast(128),
            "rbbc", [], [b_wr])

        def nr_parts(t):
            hT, hTb = hTs[t % 2], hTbs[t % 2]
            cT_, b_cT = combT.next()
            stt_ = {}

            def A(j):
                stt_[("hb", j)] = norm_block.ew(t * 4 + j)

            def B(j):
                hb, bhb = stt_[("hb", j)]
                norm_block.tr(hb, bhb, hT, hTb[j], j * 128)
                r, br = rs.next()
                lg = r[:, 0:36]
                for k in range(8):
                    mm(pc[0][:, 0:36], hT[:, k, j * 128:(j + 1) * 128], wr[:, k, :], k == 0, k == 7,
                       [hTb[j], b_wr], [b_pc[0]])
                tt("dve", lg, pc[0][:, 0:36], rb_bc, ALU.add, [b_pc[0], b_wr], [br])
                gmax = r[:, 40:41]; ngmax = r[:, 41:42]; gsum = r[:, 42:43]; gval = r[:, 43:44]
                P.op("dve", lambda e, o=gmax, i=r[:, 0:4]: e.tensor_reduce(out=o, in_=i, axis=AX.X, op=ALU.max), [br], [br])
                ts("dve", ngmax, gmax, -1.0, ALU.mult, [br], [br])
                memset("dve", gsum, 0.0, [br])
                act(r[:, 44:48], r[:, 0:4], AF.Exp, [br], [br], bias=ngmax, scale=1.0, accum_out=gsum)
                P.op("dve", lambda e, o=gval, i=gsum: e.reciprocal(out=o, in_=i), [br], [br])
                gmask = r[:, 48:52]
                ts("dve", gmask, r[:, 0:4], gmax, ALU.is_equal, [br], [br])
                ts("dve", gmask, gmask, 1.0, ALU.subtract, [br], [br], s2=30000.0, op1=ALU.mult)
                em = r[:, 56:88]
                tt("dve", em.rearrange("p (g e) -> p g e", g=4), r[:, 4:36].rearrange("p (g e) -> p g e", g=4),
                   gmask.unsqueeze(2).to_broadcast([128, 4, 8]), ALU.add, [br], [br])
                top8 = r[:, 88:96]
                P.op("dve", lambda e, o=top8, i=em: e.max(out=o, in_=i), [br], [br])
                eq1 = r[:, 96:128]; eq2 = r[:, 128:160]
                ts("dve", eq1, em, top8[:, 0:1], ALU.is_equal, [br], [br])
                ts("dve", eq2, em, top8[:, 1:2], ALU.is_equal, [br], [br])
                dd = r[:, 36:37]; ex = r[:, 37:38]; w1g = r[:, 38:39]; w2g = r[:, 39:40]
                tt("dve", dd, top8[:, 1:2], top8[:, 0:1], ALU.subtract, [br], [br])
                act(ex, dd, AF.Exp, [br], [br])
                ts("dve", w1g, ex, 1.0, ALU.add, [br], [br])
                P.op("dve", lambda e, o=w1g, i=w1g: e.reciprocal(out=o, in_=i), [br], [br])
                tt("dve", w2g, ex, w1g, ALU.mult, [br], [br])
                tt("dve", w1g, w1g, gval, ALU.mult, [br], [br])
                tt("dve", w2g, w2g, gval, ALU.mult, [br], [br])
                ts("dve", eq1, eq1, w1g, ALU.mult, [br], [br])
                stt("dve", eq1, eq2, w2g, eq1, ALU.mult, ALU.add, [br], [br])
                cb_, bcb = combb.next()
                cp("dve", cb_, eq1, [br], [bcb])
                stt_[("cb", j)] = (cb_, bcb)

            def C(j):
                cb_, bcb = stt_[("cb", j)]
                pv = pc[0][:].bitcast(BF16)
                P.op("pe", lambda e, o=pv[0:32, 128:256], i=cb_: e.transpose(o, i, ident_bf[:]), [bcb, b_const], [b_pc[0]])
                cp("dve", cT_[:, j * 128:(j + 1) * 128], pv[0:32, 128:256], [b_pc[0]], [b_cT])
            return A, B, C, (cT_, b_cT)

        def norm_router(t):
            A, B, C, ret = nr_parts(t)
            for j in range(4):
                A(j)
                B(j)
                C(j)
            return ret

        def pass1(t, cT_, b_cT):
            hT, hTb = hTs[t % 2], hTbs[t % 2]
            for e_ in range(32):
                w_, bw = w13.next()
                key = "w13_%d" % ((w13.i - 1) % 3)
                dma("sync", w_[:, 0], w1s[e_].rearrange("p (k f) -> p k f", k=8), key, [b_w1s[e_]], [bw])
                dma("sync", w_[:, 1], w3s[e_].rearrange("p (k f) -> p k f", k=8), key, [b_w3s[e_]], [bw])
                if e_ % 2 == 0:
                    h1, bh1, h3, bh3, cbk, bcbk = pb[0][:], b_pb[0], pb[1][:], b_pb[1], pb[2][:], b_pb[2]
                else:
                    h1, bh1, h3, bh3, cbk, bcbk = pS[:, 0:512], b_pS[0], pS[:, 512:1024], b_pS[1], pc[0][:], b_pc[0]
                for k in range(8):
                    mm(h1, w_[:, 0, k, :], hT[:, k, :], k == 0, k == 7, [bw] + hTb, [bh1])
                for k in range(8):
                    mm(h3, w_[:, 1, k, :], hT[:, k, :], k == 0, k == 7, [bw] + hTb, [bh3])
                mm(cbk, SelE[:, e_, :], cT_, True, True, [b_const, b_cT], [bcbk])
                sT, bsT = sTr.next()
                tT, btT = tTr.next()
                act(sT, h1, AF.Silu, [bh1], [bsT])
                tt("dve", tT, h3, sT, ALU.mult, [bh3, bsT], [btT])
                tt("dve", hid[:, e_, :], cbk, tT, ALU.mult, [bcbk, btT], [hid_b[e_]])

        w2slots = {}

        def load_w2(t, q):
            w2_, bw2 = w2r.next()
            dma("sync", w2_, w2s[q], "w2q_%d" % ((w2r.i - 1) % 3), [b_w2s[q]], [bw2])
            w2slots[(t, q)] = (w2_, bw2)

        def pass2(t, nxt=None):
            bg_pump(per_tile)
            for j in range(4):
                blk = t * 4 + j
                dma("sync", xout[:, j, :], src_rows(blk), "xout%d" % j, [src_bufs[blk]], [xout_b[j]])
            for q in range(4):
                if nxt is not None:
                    nxt[0](q)
                w2_, bw2 = w2slots[(t, q)]
                if q + 2 < 4:
                    load_w2(t, q + 2)
                elif t + 1 < NTc:
                    load_w2(t + 1, q - 2)
                for j in range(4):
                    bi = 1 + (q * 4 + j) % 2
                    for e_ in range(32):
                        mm(pc[bi][:, 0:256], hid[:, e_, j * 128:(j + 1) * 128], w2_[:, e_, :], e_ == 0, e_ == 31,
                           [hid_b[e_], bw2], [b_pc[bi]])
                    tq, btq = tmpq.next()
                    tt("dve", tq, pc[bi][:, 0:256], mod_bc[:, 5 * D + q * 256:5 * D + (q + 1) * 256], ALU.mult,
                       [b_pc[bi], b_mod], [btq])
                    tt("dve", xout[:, j, q * 256:(q + 1) * 256], tq, xout[:, j, q * 256:(q + 1) * 256], ALU.add,
                       [btq, xout_b[j]], [xout_b[j]])
                if nxt is not None:
                    nxt[1](q)
                    if q >= 1:
                        nxt[2](q - 1)
            for j in range(4):
                blk = t * 4 + j
                dma("sync", xr[blk * 128:(blk + 1) * 128, :], xout[:, j, :], "xo%d" % j, [xout_b[j]], [xr_b[blk]])

        pend = norm_router(0)
        load_w2(0, 0)
        load_w2(0, 1)
        for t in range(NTc):
            pass1(t, *pend)
            if t + 1 < NTc:
                A, B, C, pend = nr_parts(t + 1)
                pass2(t, (A, B, C))
                C(3)
            else:
                pass2(t)

    def even_phase(l, src_rows, src_bufs):
        i_ = l // 2
        new_phase()
        if do_moe:
            queue_precast("moe", l)
        per_tile = (len(bg["steps"]) + NTc - 1) // max(NTc, 1)
        hT = carve([128, 8, 512], BF16)
        hTb = [Buf("hT%d" % j) for j in range(4)]
        norm_block = make_norm(src_rows, src_bufs, A1, SH1)
        Win = carve([128, 8, 2048], BF16); b_win = Buf("win")
        Wout = carve([128, 8, D], BF16); b_wout = Buf("wout")
        poolw = carve([128, 4, 128], BF16)
        pscale = carve([128, 4], F32)
        expb = carve([128, 8, 640], BF16); b_expb = Buf("expb")
        kT = carve([128, 4, 1024], BF16)
        kT_b = [Buf("kT%d" % s_) for s_ in range(8)]
        vaug = carve([128, 8, 4, 2, 128], BF16)
        va_b = [Buf("va%d" % s_) for s_ in range(8)]
        qT = carve([128, 4, 512], BF16); b_q = Buf("qT")
        ubuf = carve([128, 4, 527], F32); b_u = [Buf("u%d" % m) for m in range(4)]
        sA = carve([128, 527], F32); sB = carve([128, 527], F32); b_sA = Buf("sA"); b_sB = Buf("sB")
        pooled = Ring([carve([128, 512], BF16) for _ in range(2)], "pooled")
        invc0 = carve([128, 4, 16], F32)
        catT = carve([128, 8, 512], BF16); cat_b = [Buf("cat%d" % m) for m in range(8)]
        Pexp = Ring([carve([128, 640], BF16) for _ in range(2)], "Pexp")
        PT = Ring([carve([128, 640], BF16) for _ in range(2)], "PT")
        rsr = Ring([carve([128, 128], F32) for _ in range(2)], "rsr")
        xres = Ring([carve([128, D], F32) for _ in range(1)], "xres")
        xo = Ring([carve([128, D], F32) for _ in range(1)], "xo")
        hk = carve([128, 5, 128], F32); b_hk = Buf("hk")
        rb8 = xo.slots[0][0][0:8, :]; b_rb8 = xo.slots[0][1]

        for k in range(8):
            dma("pool", Win[:, k, :], di["mix_w_in"][i_, k * 128:(k + 1) * 128, :], "win", [], [b_win])
            dma("pool", Wout[:, k, :], di["mix_w_out"][i_, k * 128:(k + 1) * 128, :], "wout", [], [b_wout])
        dma("pool", poolw, di["pool_w"][i_].rearrange("g c d -> c g d"), "win", [], [b_win])
        dma("sync", pscale, di["pool_scale"][i_, :].rearrange("(m p) -> p m", p=128), "pscale", [], [b_win],
            allow_slow_non_contiguous=True)
        memset("pool", vaug[:, :, :, 0, 64:128], 1.0, va_b)
        memset("pool", vaug[:, :, :, 1, 0:64], 1.0, va_b)
        memset("pool", ubuf[:], 0.0, b_u)
        for m in range(4):
            w = 2 ** (m + 1)
            memset("pool", invc0[:, m, :], 1.0 / w, [b_const])
            for pos in range(w - 1):
                memset("pool", invc0[:, m, pos:pos + 1], 1.0 / (pos + 1), [b_const])
        dma("sync", rb8[:, 0:513], di["rel_bias"][i_], "rb8", [], [b_rb8])
        cp("dve", rb8[:, 513:897], rb8[:, 512:513].to_broadcast([8, 384]), [b_rb8], [b_rb8])
        dma("sync", rbx[:, 0:768], rb8[:, 129:897], "rbx", [b_rb8], [b_rbx])
        for h in range(8):
            src = bass.AP(rbx.tensor, h * 1024, [[1, 128], [128, 5], [1, 128]])
            dma("sync", hk, src, "hk", [b_rbx], [b_hk])
            hk2 = hk.rearrange("p a b -> p (a b)")
            mm(pS[:, 0:512], Jm[:], hk2[:, 0:512], True, True, [b_const, b_hk], [b_pS[0]])
            mm(pS[:, 512:640], Jm[:], hk2[:, 512:640], True, True, [b_const, b_hk], [b_pS[1]])
            act(expb[:, h, :], pS[:, 0:640], AF.Exp, b_pS, [b_expb])
        memset("pool", expb[64:128, :, 0:64], 0.0, [b_expb])
        memset("pool", expb[0:64, :, 4 * 128 + 64:5 * 128], 0.0, [b_expb])

        obr = Ring([pc[0][:, i * 128:(i + 1) * 128] for i in range(4)], "ob")
        pcnt = [0]

        def pbank():
            i = pcnt[0] % 3
            pcnt[0] += 1
            return pb[i], b_pb[i]

        for t in range(NTc):
            if t == 0:
                for j in range(4):
                    norm_block(t * 4 + j, hT, hTb[j], j * 128)
            bg_pump(per_tile)
            for m in range(4):
                bank, bb = pbank()
                for k in range(8):
                    mm(bank[:], Win[:, k, m * 128:(m + 1) * 128], hT[:, k, :], k == 0, k == 7, [b_win] + hTb, [bb])
                if t > 0:
                    cp("dve", ubuf[:, m, 0:15], ubuf[:, m, 512:527], [b_u[m]], [b_u[m]])
                act(ubuf[:, m, 15:527], bank[:], AF.Copy, [bb], [b_u[m]])
                u = ubuf[:, m, :]
                tt("dve", sA[:, 1:527], u[:, 1:527], u[:, 0:526], ALU.add, [b_u[m]], [b_sA])
                cur, bcur = sA, b_sA
                if m >= 1:
                    tt("dve", sB[:, 3:527], sA[:, 3:527], sA[:, 1:525], ALU.add, [b_sA], [b_sB])
                    cur, bcur = sB, b_sB
                if m >= 2:
                    tt("dve", sA[:, 7:527], sB[:, 7:527], sB[:, 3:523], ALU.add, [b_sB], [b_sA])
                    cur, bcur = sA, b_sA
                if m >= 3:
                    tt("dve", sB[:, 15:527], sA[:, 15:527], sA[:, 7:519], ALU.add, [b_sA], [b_sB])
                    cur, bcur = sB, b_sB
                pl, bpl = pooled.next()
                w = 2 ** (m + 1)
                stt("dve", pl, cur[:, 15:527], 1.0 / w, u[:, 15:527], ALU.mult, ALU.subtract, [bcur, b_u[m]], [bpl])
                if t == 0:
                    tt("dve", cur[:, 15:31], cur[:, 15:31], invc0[:, m, :], ALU.mult, [bcur, b_const], [bcur])
                    tt("dve", pl[:, 0:16], cur[:, 15:31], u[:, 15:31], ALU.subtract, [bcur, b_u[m]], [bpl])
                bank, bb = pbank()
                mm(bank[:], poolw[:, m, :], pl, True, True, [b_win, bpl], [bb])
                ts("dve", catT[:, m, :], bank[:], pscale[:, m:m + 1], ALU.mult, [bb, b_win], [cat_b[m]])
            for m in range(4):
                bank, bb = pbank()
                for k in range(8):
                    mm(bank[:], Win[:, k, 512 + m * 128:512 + (m + 1) * 128], hT[:, k, :], k == 0, k == 7,
                       [b_win] + hTb, [bb])
                act(qT[:, m, :], bank[:], AF.Copy, [bb], [b_q])
            ks0 = (t % 2) * 4
            for m in range(4):
                bank, bb = pbank()
                for k in range(8):
                    mm(bank[:], Win[:, k, 1024 + m * 128:1024 + (m + 1) * 128], hT[:, k, :], k == 0, k == 7,
                       [b_win] + hTb, [bb])
                act(kT[:, m, ks0 * 128:(ks0 + 4) * 128], bank[:], AF.Copy, [bb], kT_b[ks0:ks0 + 4])
            for j in range(4):
                sl = ks0 + j
                bank, bb = pbank()
                for k in range(8):
                    mm(bank[:], hT[:, k, j * 128:(j + 1) * 128], Win[:, k, 1536:2048], k == 0, k == 7,
                       [b_win, hTb[j]], [bb])
                pvv = bank[:].rearrange("p (c two d) -> p c two d", c=4, two=2)
                cp("dve", vaug[:, sl, :, 0, 0:64], pvv[:, :, 0, :], [bb], [va_b[sl]])
                cp("dve", vaug[:, sl, :, 1, 64:128], pvv[:, :, 1, :], [bb], [va_b[sl]])
            for j in range(4):
                jq = t * 4 + j
                nxt_hb = norm_block.ew((t + 1) * 4 + j) if t + 1 < NTc else None
                blocks = [i for i in range(jq - 4, jq + 1) if i >= 0]
                for h in range(8):
                    c, hp = h // 2, h % 2
                    p0 = 64 * hp
                    for i in blocks:
                        o = jq - i
                        sl = i % 8
                        mm(pS[:, o * 128:(o + 1) * 128], kT[p0:p0 + 64, c, sl * 128:(sl + 1) * 128],
                           qT[p0:p0 + 64, c, j * 128:(j + 1) * 128], True, True, [kT_b[sl], b_q], [b_pS[o // 4]])
                    o_lo = jq - blocks[-1]
                    o_hi = jq - blocks[0]
                    c0, c1 = o_lo * 128, (o_hi + 1) * 128
                    pe_, bpe = Pexp.next()
                    pt_, bpt = PT.next()
                    rd = [b_pS[0]] + ([b_pS[1]] if o_hi == 4 else [])
                    act(pe_[:, c0:c1], pS[:, c0:c1], AF.Exp, rd, [bpe], scale=0.125)
                    tt("dve", pt_[:, c0:c1], pe_[:, c0:c1], expb[:, h, c0:c1], ALU.mult, [bpe, b_expb], [bpt])
                    ob, _ = obr.next()
                    bob = b_pc[0]
                    for n_, i in enumerate(blocks):
                        o = jq - i
                        sl = i % 8
                        mm(ob, vaug[:, sl, c, hp, :], pt_[:, o * 128:(o + 1) * 128], n_ == 0,
                           n_ == len(blocks) - 1, [va_b[sl], bpt], [bob])
                    r_, br_ = rsr.next()
                    q0 = 64 * (1 - hp)
                    P.op("dve", lambda e, o_=r_[p0:p0 + 64, :], i_2=ob[q0:q0 + 64, :]: e.reciprocal(out=o_, in_=i_2),
                         [bob], [br_])
                    tt("dve", catT[p0:p0 + 64, 4 + c, j * 128:(j + 1) * 128], ob[p0:p0 + 64, :], r_[p0:p0 + 64, :],
                       ALU.mult, [bob, br_], [cat_b[4 + c]])
                if nxt_hb is not None:
                    norm_block.tr(nxt_hb[0], nxt_hb[1], hT, hTb[j], j * 128)
            for j in range(4):
                blk = t * 4 + j
                xr_, bxr = xres.next()
                xo_, bxo = xo.next()
                dma("sync", xr_, src_rows(blk), "xres0", [src_bufs[blk]], [bxr])
                for half in range(2):
                    bank, bb = pbank()
                    for kk in range(8):
                        mm(bank[:], catT[:, kk, j * 128:(j + 1) * 128], Wout[:, kk, half * 512:(half + 1) * 512],
                           kk == 0, kk == 7, [cat_b[kk], b_wout], [bb])
                    tt("dve", xo_[:, half * 512:(half + 1) * 512], bank[:],
                       mod_bc[:, 2 * D + half * 512:2 * D + (half + 1) * 512], ALU.mult, [bb, b_mod], [bxo])
                    tt("dve", xo_[:, half * 512:(half + 1) * 512], xo_[:, half * 512:(half + 1) * 512],
                       xr_[:, half * 512:(half + 1) * 512], ALU.add, [bxo, bxr], [bxo])
                dma("sync", xr[blk * 128:(blk + 1) * 128, :], xo_, "xo0", [bxo], [xr_b[blk]])

    def ssd_phase(l, src_rows, src_bufs):
        i_ = l // 2
        queue_precast("ssd", l)
        bg_flush()
        new_phase()
        per_tile = (len(bg["steps"]) + NTc * 2 - 1) // max(NTc * 2, 1)
        TT_ = 256
        hT = carve([128, 8, TT_], BF16)
        hTb = [Buf("hT%d" % j) for j in range(2)]
        norm_block = make_norm(src_rows, src_bufs, A1, SH1, lean=True)
        Wout = carve([128, 16, D], BF16); b_wout = Buf("wout")
        wch = Ring([carve([128, 8, 128], BF16) for _ in range(3)], "wch")
        wz = Ring([carve([128, 2, 8, 128], BF16) for _ in range(2)], "wz")
        wdt = carve([128, 8, 32], BF16); b_wdt = Buf("wdt")
        cw = carve([128, 4, 32], F32); cbias = carve([128, 32], F32); b_cw = Buf("cw")
        halo = carve([128, 32, 3], F32); b_halo = [Buf("halo%d" % m) for m in range(32)]
        rawb = Ring([carve([128, TT_ + 3], F32) for _ in range(2)], "rawb")
        accr = Ring([carve([128, TT_], F32) for _ in range(2)], "acc")
        xT = carve([128, 16, TT_], BF16); BT = carve([128, 8, TT_], BF16); CT = carve([128, 8, TT_], BF16)
        xT_b = [Buf("xT%d" % m) for m in range(16)]; BT_b = [Buf("BT%d" % m) for m in range(8)]
        CT_b = [Buf("CT%d" % m) for m in range(8)]
        xtm = Ring([carve([128, 2048], BF16)], "xtm")
        xdt = Ring([carve([128, 2048], BF16)], "xdt")
        xdd = Ring([carve([128, 2048], BF16)], "xdd")
        Btm = Ring([carve([128, 1024], BF16)], "Btm")
        zs = Ring([carve([128, 2048], BF16) for _ in range(2)], "zs")
        smr = Ring([carve([128, 320], F32) for _ in range(2)], "ssm_sm")
        ahl = Ring([carve([128, 64], BF16) for _ in range(2)], "ahl")
        LTr = Ring([carve([128, 4, 128], BF16) for _ in range(2)], "LT")
        MTr = Ring([carve([128, 4, 128], BF16) for _ in range(2)], "MT")
        Gsr = Ring([carve([128, 128], BF16) for _ in range(2)], "Gs")
        state = carve([128, 8, 256], F32); stateb = carve([128, 8, 256], BF16)
        st_b = [Buf("st%d" % g) for g in range(8)]; stb_b = [Buf("stb%d" % g) for g in range(8)]
        ya = carve([128, 2048], F32); b_ya = [Buf("ya%d" % g) for g in range(8)]
        xD = carve([128, 2048], BF16); b_xD = Buf("xD")
        gn = carve([128, 2048], BF16); b_gn_h = [Buf("gn0"), Buf("gn1")]
        gnT = carve([128, 16, 128], BF16); b_gnT = [Buf("gnT0"), Buf("gnT1")]
        nw_bc = carve([128, 2048], BF16); vec_bc = carve([128, 96], F32); b_vec = Buf("vec")
        xres = Ring([carve([128, D], F32)], "xres")
        xo = Ring([carve([128, D], F32)], "xo")
        junk2 = carve([128, 256], BF16); b_j2 = Buf("junk2")

        for k in range(16):
            dma("pool", Wout[:, k, :], di["ssm_w_out"][i_, k * 128:(k + 1) * 128, :], "wout", [], [b_wout])
        dma("pool", wdt, di["ssm_w_in"][i_, :, 6144:6176].rearrange("(k p) f -> p k f", p=128), "wdt", [], [b_wdt])
        cwr = xo.slots[0][0][0:32, 0:640].rearrange("p (t c) -> p t c", t=5)
        b_cwr = xo.slots[0][1]
        for tap in range(4):
            dma("sync", cwr[:, tap, :], di["ssm_conv_w"][i_, tap, :].rearrange("(m p) -> m p", p=128), "cwr", [], [b_cwr])
        dma("sync", cwr[:, 4, :], di["ssm_conv_b"][i_, :].rearrange("(m p) -> m p", p=128), "cwr", [], [b_cwr])
        for tap in range(5):
            P.op("pe", (lambda t_: lambda e: e.transpose(pb[0][:, t_ * 32:(t_ + 1) * 32], cwr[:, t_, :], ident_f[0:32, 0:32]))(tap),
                 [b_cwr, b_const], [b_pb[0]])
        cp("dve", cw.rearrange("p t m -> p (t m)"), pb[0][:, 0:128], [b_pb[0]], [b_cw])
        cp("dve", cbias, pb[0][:, 128:160], [b_pb[0]], [b_cw])
        dma("pool", nw_bc, di["ssm_norm_w"][i_, :].partition_broadcast(128), "vecn", [], [b_vec])
        dtb_bc = vec_bc[:, 0:32]; A_bc = vec_bc[:, 32:64]; D_bc = vec_bc[:, 64:96]
        dma("sync", dtb_bc, di["ssm_dt_bias"][i_, :].partition_broadcast(128), "vec", [], [b_vec])
        dma("sync", A_bc, di["ssm_A_log"][i_, :].partition_broadcast(128), "vec", [], [b_vec])
        dma("sync", D_bc, di["ssm_D"][i_, :].partition_broadcast(128), "vec", [], [b_vec])
        act(A_bc, A_bc, AF.Exp, [b_vec], [b_vec])
        ts("dve", A_bc, A_bc, -1.0, ALU.mult, [b_vec], [b_vec])
        memset("pool", halo[:], 0.0, b_halo)
        memset("pool", state[:], 0.0, st_b)
        memset("pool", stateb[:], 0.0, stb_b)

        G_ps = pb[0][:, 0:128]
        b_small = b_pb[0]
        st_ps, b_stp = pb[0][:, 256:512], b_pb[0]
        yd_ps = pc[0][:, 0:256]
        yo_ps, b_yo = pc[0][:, 256:512], b_pc[0]
        lcnt = [0]
        hcnt = [0]

        def half():
            i = hcnt[0] % 2
            hcnt[0] += 1
            return pS[:, i * 512:(i + 1) * 512], b_pS[i]

        stop_ = cfg.get("ssd_stop", 99)
        if stop_ <= 1:
            return
        for t in range(NTc * 2):
            if t == 0:
                for j in range(2):
                    norm_block(t * 2 + j, hT, hTb[j], j * 128)
            bg_pump(per_tile)
            smv = []
            zv = []
            for j in range(2):
                jc = slice(j * 128, (j + 1) * 128)
                sm, bsm = smr.next()
                dt_ = sm[:, 0:32]; a_ = sm[:, 32:64]; acs = sm[:, 64:96]; nacs = sm[:, 96:128]
                tot = sm[:, 128:160]; eacs = sm[:, 160:192]; dstt = sm[:, 192:224]; cdec = sm[:, 224:256]
                ssq = sm[:, 256:264]; lnv = sm[:, 264:272]; rstd = sm[:, 272:280]
                sp = pb[0][:, 128:160]
                for k in range(8):
                    mm(sp, hT[:, k, jc], wdt[:, k, :], k == 0, k == 7, [hTb[j], b_wdt], [b_small])
                tt("dve", dt_, sp, dtb_bc, ALU.add, [b_small, b_vec], [bsm])
                act(dt_, dt_, AF.Exp, [bsm], [bsm])
                act(dt_, dt_, AF.Ln, [bsm], [bsm], bias=1.0)
                tt("dve", a_, dt_, A_bc, ALU.mult, [bsm, b_vec], [bsm])
                mm(pb[0][:, 160:192], Tm[:], a_, True, True, [b_const, bsm], [b_small])
                mm(pb[0][:, 192:224], ones_f[:], a_, True, True, [b_const, bsm], [b_small])
                cp("dve", acs, pb[0][:, 160:192], [b_small], [bsm])
                ts("dve", nacs, acs, -1.0, ALU.mult, [bsm], [bsm])
                cp("dve", tot, pb[0][:, 192:224], [b_small], [bsm])
                act(eacs, acs, AF.Exp, [bsm], [bsm])
                tt("dve", dstt, tot, acs, ALU.subtract, [bsm], [bsm])
                act(dstt, dstt, AF.Exp, [bsm], [bsm])
                act(cdec, tot, AF.Exp, [bsm], [bsm])
                ah_, bah = ahl.next()
                cp("dve", ah_[:, 0:32], a_, [bsm], [bah])
                tt("dve", sm[:, 288:320], a_, ah_[:, 0:32], ALU.subtract, [bsm, bah], [bsm])
                cp("dve", ah_[:, 32:64], sm[:, 288:320], [bsm], [bah])
                smv.append((sm, bsm, ah_, bah))
                z_, bz = zs.next()
                for pz in range(8):
                    wz_, bwz = wz.next()
                    dma("sync", wz_, wins[2 * pz:2 * pz + 2].rearrange("c p (k f) -> p c k f", k=8),
                        "wz%d" % ((wz.i - 1) % 2), [b_wins[2 * pz], b_wins[2 * pz + 1]], [bwz])
                    bank, bb = half()
                    for k in range(8):
                        mm(bank[:, 0:256].rearrange("p (c f) -> p c f", c=2), hT[:, k, jc], wz_[:, :, k, :], k == 0, k == 7,
                           [hTb[j], bwz], [bb])
                    act(z_[:, pz * 256:(pz + 1) * 256], bank[:, 0:256], AF.Silu, [bb], [bz])
                zv.append((z_, bz))
            pend_silu = None
            for m in range(32):
                w_, bw = wch.next()
                dma("sync", w_, wins[16 + m].rearrange("p (k f) -> p k f", k=8), "wch%d" % ((wch.i - 1) % 3),
                    [b_wins[16 + m]], [bw])
                bank, bb = half()
                for k in range(8):
                    mm(bank[:, 0:TT_], w_[:, k, :], hT[:, k, :], k == 0, k == 7, [bw] + hTb, [bb])
                rw, brw = rawb.next()
                ac, bac = accr.next()
                cp("dve", rw[:, 0:3], halo[:, m, :], [b_halo[m]], [brw])
                act(rw[:, 3:TT_ + 3], bank[:, 0:TT_], AF.Copy, [bb], [brw])
                if pend_silu is not None:
                    pend_silu()
                cp("dve", halo[:, m, :], rw[:, TT_:TT_ + 3], [brw], [b_halo[m]])
                ts("dve", ac, rw[:, 0:TT_], cw[:, 0, m:m + 1], ALU.mult, [brw, b_cw], [bac])
                for tap in range(1, 4):
                    stt("dve", ac, rw[:, tap:TT_ + tap], cw[:, tap, m:m + 1], ac, ALU.mult, ALU.add,
                        [brw, b_cw, bac], [bac])
                if m < 16:
                    dst_, bd = xT[:, m, :], xT_b[m]
                elif m < 24:
                    dst_, bd = BT[:, m - 16, :], BT_b[m - 16]
                else:
                    dst_, bd = CT[:, m - 24, :], CT_b[m - 24]

                def _silu(dst_=dst_, ac=ac, bac=bac, bd=bd, m=m):
                    act(dst_, ac, AF.Silu, [bac, b_cw], [bd], bias=cbias[:, m:m + 1])
                pend_silu = _silu
            pend_silu()
            if stop_ <= 2:
                return
            for j in range(2):
                blk = t * 2 + j
                jc = slice(j * 128, (j + 1) * 128)
                sm, bsm, ah_, bah = smv[j]
                z_, bz = zv[j]
                dt_ = sm[:, 0:32]; a_ = sm[:, 32:64]; acs = sm[:, 64:96]; nacs = sm[:, 96:128]
                tot = sm[:, 128:160]; eacs = sm[:, 160:192]; dstt = sm[:, 192:224]; cdec = sm[:, 224:256]
                ssq = sm[:, 256:264]; lnv = sm[:, 264:272]; rstd = sm[:, 272:280]
                if stop_ <= 3:
                    return
                xt_, bxt = xtm.next(); xd_, bxd = xdt.next(); xdd_, bxdd = xdd.next(); bt_, bbt = Btm.next()
                for hf_ in range(2):
                    pbank, bbank = pc[1 + hf_], b_pc[1 + hf_]
                    pv = pbank[:].bitcast(BF16)
                    for k in range(8):
                        trn(pv[:, k * 128:(k + 1) * 128], xT[:, hf_ * 8 + k, jc], [xT_b[hf_ * 8 + k], b_const], [bbank])
                    cs = slice(hf_ * 1024, (hf_ + 1) * 1024)
                    act(xt_[:, cs], pv, AF.Copy, [bbank], [bxt])
                    if stop_ <= 3.2:
                        continue
                    tt("dve", xd_[:, cs].rearrange("p (h d) -> p h d", h=16), xt_[:, cs].rearrange("p (h d) -> p h d", h=16),
                       dt_[:, hf_ * 16:(hf_ + 1) * 16].unsqueeze(2).to_broadcast([128, 16, 64]), ALU.mult,
                       [bxt, bsm], [bxd])
                if stop_ <= 3.4:
                    return
                pv = pc[1][:].bitcast(BF16)
                for g in range(8):
                    trn(pv[:, g * 128:(g + 1) * 128], BT[:, g, jc], [BT_b[g], b_const], [b_pc[1]])
                act(bt_, pv, AF.Copy, [b_pc[1]], [bbt])
                tt("dve", xdd_.rearrange("p (h d) -> p h d", h=32), xd_.rearrange("p (h d) -> p h d", h=32),
                   dstt.unsqueeze(2).to_broadcast([128, 32, 64]), ALU.mult, [bxd, bsm], [bxdd])
                tt("dve", xD.rearrange("p (h d) -> p h d", h=32), xt_.rearrange("p (h d) -> p h d", h=32),
                   D_bc.unsqueeze(2).to_broadcast([128, 32, 64]), ALU.mult, [bxt, b_vec], [b_xD])
                if stop_ <= 4:
                    return
                nxt_hb = norm_block.ew((t + 1) * 2 + j) if t + 1 < NTc * 2 else None
                def banks(g):
                    if g % 2 == 0:
                        return (pb[0][:, 0:128], b_pb[0], pb[0][:, 256:512], b_pb[0], pb[1], b_pb[1],
                                pc[0][:, 0:256], b_pc[0], pc[0][:, 256:512], b_pc[0])
                    return (pS[:, 0:128], b_pS[0], pS[:, 256:512], b_pS[0], pb[2], b_pb[2],
                            pS[:, 512:768], b_pS[1], pS[:, 768:1024], b_pS[1])

                def preA(g):
                    Gp, bG, st_ps, b_stp, Dp, bDp, yd, byd, yo_ps, b_yo = banks(g)
                    mm(Gp, BT[:, g, jc], CT[:, g, jc], True, True, [BT_b[g], CT_b[g]], [bG])
                    LT, bLT = LTr.next()
                    for r in range(4):
                        h = 4 * g + r
                        reg = Dp[:, r * 128:(r + 1) * 128]
                        mm(reg, ah_[:, h:h + 1].to_broadcast([128, 128]), Tm_bf[:], True, False, [bah, b_const], [bDp])
                        mm(reg, ah_[:, 32 + h:33 + h].to_broadcast([128, 128]), Tm_bf[:], False, False, [bah, b_const], [bDp])
                        mm(reg, ident_bf[:], negm[:], False, True, [b_const], [bDp])
                    for r in range(4):
                        h = 4 * g + r
                        act(LT[:, r, :], Dp[:, r * 128:(r + 1) * 128], AF.Exp, [bDp, bsm], [bLT], bias=nacs[:, h:h + 1])
                    Gs, bGs = Gsr.next()
                    act(Gs, Gp, AF.Copy, [bG], [bGs])
                    return LT, bLT, Gs, bGs

                def preB(g, LT, bLT, Gs, bGs):
                    MT, bMT = MTr.next()
                    tt("dve", MT, LT, Gs.unsqueeze(1).to_broadcast([128, 4, 128]), ALU.mult, [bLT, bGs], [bMT])
                    return MT, bMT

                def post(g, MT, bMT):
                    Gp, bG, st_ps, b_stp, Dp, bDp, yd, byd, yo_ps, b_yo = banks(g)
                    for r in range(4):
                        h = 4 * g + r
                        mm(yd[:, r * 64:(r + 1) * 64], MT[:, r, :], xd_[:, h * 64:(h + 1) * 64], True, True, [bMT, bxd], [byd])
                    gs = slice(g * 256, (g + 1) * 256)
                    mm(yo_ps, CT[:, g, jc], stateb[:, g, :], True, True, [CT_b[g], stb_b[g]], [b_yo])
                    mm(st_ps, bt_[:, g * 128:(g + 1) * 128], xdd_[:, gs], True, True, [bbt, bxdd], [b_stp])

                def post_ew(g):
                    Gp, bG, st_ps, b_stp, Dp, bDp, yd, byd, yo_ps, b_yo = banks(g)
                    gs = slice(g * 256, (g + 1) * 256)
                    tt("dve", ya[:, gs].rearrange("p (h d) -> p h d", h=4), yo_ps.rearrange("p (h d) -> p h d", h=4),
                       eacs[:, 4 * g:4 * g + 4].unsqueeze(2).to_broadcast([128, 4, 64]), ALU.mult, [b_yo, bsm], [b_ya[g]])
                    tt("dve", ya[:, gs], ya[:, gs], yd, ALU.add, [b_ya[g], byd], [b_ya[g]])
                    tt("dve", state[:, g, :].rearrange("p (h d) -> p h d", h=4),
                       state[:, g, :].rearrange("p (h d) -> p h d", h=4),
                       cdec[:, 4 * g:4 * g + 4].unsqueeze(2).to_broadcast([128, 4, 64]), ALU.mult, [st_b[g], bsm], [st_b[g]])
                    tt("dve", state[:, g, :], state[:, g, :], st_ps, ALU.add, [st_b[g], b_stp], [st_b[g]])
                    cp("pool", stateb[:, g, :], state[:, g, :], [st_b[g]], [stb_b[g]])
                    tt("dve", ya[:, gs], ya[:, gs], xD[:, gs], ALU.add, [b_ya[g], b_xD], [b_ya[g]])
                    tt("dve", ya[:, gs], ya[:, gs], z_[:, gs], ALU.mult, [b_ya[g], bz], [b_ya[g]])
                    act(junk2, ya[:, gs], AF.Square, [b_ya[g], bsm], [b_j2, bsm], accum_out=ssq[:, g:g + 1])
                    tt("pool", ya[:, gs], ya[:, gs], nw_bc[:, gs], ALU.mult, [b_ya[g], b_vec], [b_ya[g]])

                memset("dve", ssq, 0.0, [bsm])
                pa = preA(0)
                mt = preB(0, *pa)
                for g in range(8):
                    if g + 1 < 8:
                        pa = preA(g + 1)
                    post(g, *mt)
                    post_ew(g)
                    if g + 1 < 8:
                        mt = preB(g + 1, *pa)
                if nxt_hb is not None:
                    norm_block.tr(nxt_hb[0], nxt_hb[1], hT, hTb[j], j * 128)
                if stop_ <= 5:
                    return
                act(lnv, ssq, AF.Ln, [bsm], [bsm], bias=EPS, scale=1.0 / 256)
                act(rstd, lnv, AF.Exp, [bsm], [bsm], scale=-0.5)
                for g in range(8):
                    gs = slice(g * 256, (g + 1) * 256)
                    ts("dve", gn[:, gs], ya[:, gs], rstd[:, g:g + 1], ALU.mult,
                       [b_ya[g], bsm], [b_gn_h[g // 4]])
                for hf_ in range(2):
                    pbank, bbank = pc[1 + hf_], b_pc[1 + hf_]
                    pv = pbank[:].bitcast(BF16)
                    for k in range(8):
                        trn(pv[:, k * 128:(k + 1) * 128], gn[:, (hf_ * 8 + k) * 128:(hf_ * 8 + k + 1) * 128],
                            [b_gn_h[hf_], b_const], [bbank])
                    act(gnT[:, hf_ * 8:(hf_ + 1) * 8, :], pv.rearrange("p (k t) -> p k t", k=8), AF.Copy, [bbank],
                        [b_gnT[hf_]])
                xr_, bxr = xres.next()
                xo_, bxo = xo.next()
                dma("sync", xr_, src_rows(blk), "xres0", [src_bufs[blk]], [bxr])
                for hh in range(2):
                    bank, bb = half()
                    for kk in range(16):
                        mm(bank, gnT[:, kk, :], Wout[:, kk, hh * 512:(hh + 1) * 512], kk == 0, kk == 15,
                           b_gnT + [b_wout], [bb])
                    cs = slice(hh * 512, (hh + 1) * 512)
                    tt("dve", xo_[:, cs], bank, mod_bc[:, 2 * D + hh * 512:2 * D + (hh + 1) * 512], ALU.mult,
                       [bb, b_mod], [bxo])
                    tt("dve", xo_[:, cs], xo_[:, cs], xr_[:, cs], ALU.add, [bxo, bxr], [bxo])
                dma("sync", xr[blk * 128:(blk + 1) * 128, :], xo_, "xo0", [bxo], [xr_b[blk]])

    def final_phase(src_rows, src_bufs):
        new_phase()
        xin = Ring([carve([128, D], F32) for _ in range(2)], "fxin")
        xo = Ring([carve([128, D], F32) for _ in range(2)], "fxo")
        junk = carve([128, D], BF16); bj = Buf("fj")
        sm = Ring([carve([128, 4], F32) for _ in range(2)], "fsm")
        fw = carve([128, D], F32); b_fw = Buf("fw")
        dma("sync", fw, di["final_norm_w"][0, :].partition_broadcast(128), "fw", [], [b_fw])
        for blk in range(NTc * 4):
            x_, bx = xin.next()
            o_, bo = xo.next()
            s_, bs = sm.next()
            dma("sync", x_, src_rows(blk), "fxin%d" % (blk % 2), [src_bufs[blk]], [bx])
            if do_final:
                memset("dve", s_[:, 0:1], 0.0, [bs])
                act(junk, x_, AF.Square, [bx, bs], [bj, bs], accum_out=s_[:, 0:1])
                act(s_[:, 1:2], s_[:, 0:1], AF.Ln, [bs], [bs], bias=EPS, scale=1.0 / D)
                act(s_[:, 2:3], s_[:, 1:2], AF.Exp, [bs], [bs], scale=-0.5)
                stt("dve", o_, x_, s_[:, 2:3], fw, ALU.mult, ALU.mult, [bx, bs, b_fw], [bo])
            else:
                cp("dve", o_, x_, [bx], [bo])
            dma("sync", y_d[blk * 128:(blk + 1) * 128, :], o_, "fxo%d" % (blk % 2), [bo], [y_b[blk]])

    src = lambda blk: di["x"][blk * 128:(blk + 1) * 128, :]
    src_b = [Buf("xsrc%d" % i) for i in range(NBLK)]
    xr_rows = lambda blk: xr[blk * 128:(blk + 1) * 128, :]
    for l in layers:
        ada_phase(l)
        if do_mixer:
            if l % 2 == 0:
                even_phase(l, src, src_b)
            else:
                ssd_phase(l, src, src_b)
            src, src_b = xr_rows, xr_b
        if do_moe:
            moe_phase(l, src, src_b)
            src, src_b = xr_rows, xr_b
    bg_flush()
    final_phase(src, src_b)
    P.barrier()
    P.emit()
    return nc


_CACHE = {}


def kernel(**inputs):
    cfg = inputs.pop("_cfg", None) or {}
    ncores = cfg.get("ncores", 8)
    key = repr(sorted(cfg.items()))
    if key not in _CACHE:
        _CACHE[key] = build(cfg)
    nc = _CACHE[key]
    shared = {}
    for name, shp in INPUT_SHAPES:
        if name in ("x", "c"):
            continue
        shared[name] = np.ascontiguousarray(np.asarray(inputs[name], dtype=np.float32).reshape(shp))
    x = np.asarray(inputs["x"], dtype=np.float32)
    c = np.asarray(inputs["c"], dtype=np.float32)
    in_maps = []
    for b in range(ncores):
        m = dict(shared)
        m["x"] = np.ascontiguousarray(x[b])
        m["c"] = np.ascontiguousarray(c[b:b + 1])
        in_maps.append(m)
    res = run_bass_kernel_spmd(nc, in_maps, core_ids=list(range(ncores)))
    out = np.stack([np.asarray(res.results[b]["y"], dtype=np.float32).reshape(S, D) for b in range(ncores)], axis=0)
    return out
```

```python
import numpy as np
import concourse.bass as bass
import concourse.mybir as mybir
from concourse.bass_utils import run_bass_kernel_spmd

F32 = mybir.dt.float32
BF16 = mybir.dt.bfloat16
AF = mybir.ActivationFunctionType
ALU = mybir.AluOpType
AX = mybir.AxisListType

ENGS = ("sync", "act", "pool", "pe", "dve")
D = 1024
S = 4096
NBLK = 32
NT = 8
EPS = 1e-6


class Buf:
    __slots__ = ("name", "w", "r")

    def __init__(self, name=""):
        self.name = name
        self.w = None
        self.r = {}


class Prog:
    def __init__(self, nc, same_eng_sync=("pool", "act", "dve")):
        self.nc = nc
        self.ops = {e: [] for e in ENGS}
        self.dma_cnt = {}
        self.same_eng_sync = set(same_eng_sync)
        self.last_real = {e: None for e in ENGS}

    def _deps_for(self, reads, writes):
        deps = set()
        for b in reads:
            if b.w is not None:
                deps.add(b.w)
        for b in writes:
            if b.w is not None:
                deps.add(b.w)
            for t in b.r.values():
                deps.add(t)
        return deps

    def _commit(self, tok, reads, writes):
        for b in writes:
            b.w = tok
            b.r = {}
        for b in reads:
            if b in writes:
                continue
            k = tok[0] if tok[0] != "dma" else ("dma", tok[1])
            b.r[k] = tok

    def op(self, eng, fn, reads=(), writes=()):
        deps = self._deps_for(reads, writes)
        idx = len(self.ops[eng])
        tok = (eng, idx)
        self.ops[eng].append([fn, deps, "op", None])
        self.last_real[eng] = tok
        self._commit(tok, reads, writes)
        return tok

    def dma(self, eng, fn, key, reads=(), writes=()):
        deps = self._deps_for(reads, writes)
        deps = set(d for d in deps if not (d[0] == "dma" and d[1] == key))
        c = self.dma_cnt.get(key, 0) + 1
        self.dma_cnt[key] = c
        tok = ("dma", key, c)
        self.ops[eng].append([fn, deps, "dma", key])
        self._commit(tok, reads, writes)
        return tok

    def barrier(self):
        toks = set(t for t in self.last_real.values() if t is not None)
        toks |= set(("dma", k, c) for k, c in self.dma_cnt.items())
        for e in ENGS:
            self.ops[e].append([None, set(toks), "op", None])

    def emit(self):
        nc = self.nc
        need = {e: set() for e in ENGS}
        for e in ENGS:
            for (fn, deps, kind, key) in self.ops[e]:
                for d in deps:
                    if d[0] != "dma":
                        if d[0] == e and e not in self.same_eng_sync:
                            continue
                        need[d[0]].add(d[1])
        msval = {}
        for e in ENGS:
            c = 0
            m = {}
            for i in sorted(need[e]):
                c += 1
                m[i] = c
            msval[e] = m
        sem = {e: nc.alloc_semaphore("s_" + e) for e in ENGS}
        dsem = {k: nc.alloc_semaphore("d_%d" % i) for i, k in enumerate(self.dma_cnt)}
        engobj = {"sync": "sync", "act": "scalar", "pool": "gpsimd", "pe": "tensor", "dve": "vector"}

        def run(e):
            def body(eng):
                seen = {}
                for i, (fn, deps, kind, key) in enumerate(self.ops[e]):
                    for d in sorted(deps, key=str):
                        if d[0] == "dma":
                            sk = ("dma", d[1]); val = 16 * d[2]; s = dsem[d[1]]
                        else:
                            if d[0] == e and e not in self.same_eng_sync:
                                continue
                            sk = d[0]; val = msval[d[0]][d[1]]; s = sem[d[0]]
                        if seen.get(sk, 0) >= val:
                            continue
                        seen[sk] = val
                        eng.wait_ge(s, val)
                    if fn is None:
                        continue
                    ins = fn(eng)
                    if kind == "dma":
                        ins.then_inc(dsem[key], 16)
                    elif i in msval[e]:
                        ins.then_inc(sem[e], 1)
            return body

        with nc.Block() as block:
            for e in ENGS:
                if self.ops[e]:
                    getattr(block, engobj[e])(run(e))


class Ring:
    def __init__(self, aps, name):
        self.slots = [(ap, Buf(name + str(i))) for i, ap in enumerate(aps)]
        self.i = 0

    def next(self):
        s = self.slots[self.i % len(self.slots)]
        self.i += 1
        return s


INPUT_SHAPES = [
    ("x", [S, D]), ("c", [1, D]), ("ada_w", [4, D, 6 * D]), ("ada_b", [4, 6 * D]),
    ("norm1_w", [4, D]), ("norm2_w", [4, D]), ("mix_w_in", [2, D, 2048]), ("pool_w", [2, 4, 128, 128]),
    ("pool_scale", [2, 512]), ("rel_bias", [2, 8, 513]), ("mix_w_out", [2, 1024, D]),
    ("ssm_w_in", [2, D, 6176]), ("ssm_conv_w", [2, 4, 4096]), ("ssm_conv_b", [2, 4096]),
    ("ssm_dt_bias", [2, 32]), ("ssm_A_log", [2, 32]), ("ssm_D", [2, 32]), ("ssm_norm_w", [2, 2048]),
    ("ssm_w_out", [2, 2048, D]), ("moe_w_group", [4, D, 4]), ("moe_b_group", [4, 4]),
    ("moe_w_expert", [4, D, 4, 8]), ("moe_b_expert", [4, 4, 8]), ("moe_w1", [4, 32, D, 128]),
    ("moe_w3", [4, 32, D, 128]), ("moe_w2", [4, 32, 128, D]), ("final_norm_w", [1, D]),
]


def build(cfg):
    layers = cfg.get("layers", [0, 1, 2, 3])
    do_final = cfg.get("final", True)
    do_mixer = cfg.get("mixer", True)
    do_moe = cfg.get("moe", True)
    NTc = cfg.get("ntiles", NT)
    nc = bass.Bass("TRN2", target_bir_lowering=False)
    P = Prog(nc)
    di = {}
    for name, shp in INPUT_SHAPES:
        di[name] = nc.dram_tensor(name, list(shp), F32, kind="ExternalInput").ap()
    y_d = nc.dram_tensor("y", [S, D], F32, kind="ExternalOutput").ap()
    xr = nc.dram_tensor("xr", [S, D], F32).ap()
    rbx = nc.dram_tensor("rbx", [8, 1024], F32).ap()
    b_rbx = Buf("rbx")
    xr_b = [Buf("xr%d" % i) for i in range(NBLK)]
    y_b = [Buf("y%d" % i) for i in range(NBLK)]

    def act(out, in_, func, r, w, **kw):
        P.op("act", lambda e: e.activation(out=out, in_=in_, func=func, **kw), r, w)

    def tt(eng, out, in0, in1, op, r, w):
        P.op(eng, lambda e: e.tensor_tensor(out=out, in0=in0, in1=in1, op=op), r, w)

    def ts(eng, out, in0, s1, op0, r, w, s2=None, op1=None):
        if op1 is None:
            P.op(eng, lambda e: e.tensor_scalar(out=out, in0=in0, scalar1=s1, scalar2=None, op0=op0), r, w)
        else:
            P.op(eng, lambda e: e.tensor_scalar(out=out, in0=in0, scalar1=s1, scalar2=s2, op0=op0, op1=op1), r, w)

    def stt(eng, out, in0, scalar, in1, op0, op1, r, w):
        P.op(eng, lambda e: e.scalar_tensor_tensor(out=out, in0=in0, scalar=scalar, in1=in1, op0=op0, op1=op1), r, w)

    def mm(out, lhsT, rhs, start, stop, r, w):
        P.op("pe", lambda e: e.matmul(out, lhsT, rhs, start=start, stop=stop), r, w)

    def trn(out, in_, r, w):
        P.op("pe", lambda e: e.transpose(out, in_, ident_bf[:]), r, w)

    def cp(eng, out, in_, r, w):
        P.op(eng, lambda e: e.tensor_copy(out=out, in_=in_), r, w)

    def dma(q, out, in_, key, r, w, **kw):
        P.dma(q, lambda e: e.dma_start(out=out, in_=in_, **kw), key, r, w)

    def memset(eng, ap, val, w):
        P.op(eng, lambda e: e.memset(ap, val), [], w)

    def asel(out, in_, pattern, cmp, fill, base, cm, r, w):
        P.op("pool", lambda e: e.affine_select(out=out, in_=in_, pattern=pattern, compare_op=cmp, fill=fill,
                                               base=base, channel_multiplier=cm), r, w)

    pb = [nc.alloc_psum_tensor("pb%d" % i, [128, 512], F32) for i in range(3)]
    pS = nc.alloc_psum_tensor("pS", [128, 1024], F32)
    pc = [nc.alloc_psum_tensor("pc%d" % i, [128, 512], F32) for i in range(3)]
    b_pb = [Buf("pb%d" % i) for i in range(3)]
    b_pS = [Buf("pS0"), Buf("pS1")]
    b_pc = [Buf("pc%d" % i) for i in range(3)]

    def sb(name, shape, dt):
        return nc.alloc_sbuf_tensor(name, list(shape), dt)

    b_const = Buf("const")
    ident_f = sb("ident_f", [128, 128], F32)
    ident_bf = sb("ident_bf", [128, 128], BF16)
    Jm = sb("Jm", [128, 128], F32)
    ones_f = sb("ones_f", [128, 128], F32)
    Tm = sb("Tm", [128, 128], F32)
    Tm_bf = sb("Tm_bf", [128, 128], BF16)
    negm_f = sb("negm_f", [128, 128], F32)
    negm = sb("negm", [128, 128], BF16)
    SelE = sb("SelE", [32, 32, 128], BF16)
    mod_bc = sb("mod_bc", [128, 6 * D], F32)
    cactB = sb("cactB", [128, 8, 128], F32)
    cT = sb("cT", [128, 8], F32)
    b_mod = Buf("mod")

    memset("pool", ident_f[:], 1.0, [b_const])
    asel(ident_f[:], ident_f[:], [[-1, 128]], ALU.is_equal, 0.0, 0, 1, [b_const], [b_const])
    cp("dve", ident_bf[:], ident_f[:], [b_const], [b_const])
    memset("pool", Jm[:], 1.0, [b_const])
    asel(Jm[:], Jm[:], [[1, 128]], ALU.is_equal, 0.0, -127, 1, [b_const], [b_const])
    memset("pool", ones_f[:], 1.0, [b_const])
    memset("pool", Tm[:], 1.0, [b_const])
    asel(Tm[:], Tm[:], [[1, 128]], ALU.is_ge, 0.0, 0, -1, [b_const], [b_const])
    cp("dve", Tm_bf[:], Tm[:], [b_const], [b_const])
    memset("pool", negm_f[:], 0.0, [b_const])
    asel(negm_f[:], negm_f[:], [[1, 128]], ALU.is_ge, -30000.0, 0, -1, [b_const], [b_const])
    cp("dve", negm[:], negm_f[:], [b_const], [b_const])
    memset("pool", SelE[:], 1.0, [b_const])
    asel(SelE[:], SelE[:], [[-1, 32], [0, 128]], ALU.is_equal, 0.0, 0, 1, [b_const], [b_const])

    dma("sync", cT[:], di["c"][0, :].rearrange("(k p) -> p k", p=128), "cT", [], [b_const],
        allow_slow_non_contiguous=True)
    act(cT[:], cT[:], AF.Silu, [b_const], [b_const])
    for k in range(8):
        cp("dve", cactB[:, k, :], cT[:, k:k + 1].to_broadcast([128, 128]), [b_const], [b_const])

    w1s_ = [nc.dram_tensor("w1s%d" % i, [32, 128, 1024], BF16).ap() for i in range(2)]
    w3s_ = [nc.dram_tensor("w3s%d" % i, [32, 128, 1024], BF16).ap() for i in range(2)]
    w2s_ = [nc.dram_tensor("w2s%d" % i, [4, 128, 32, 256], BF16).ap() for i in range(2)]
    wins = nc.dram_tensor("wins", [48, 128, 1024], BF16).ap()
    b_w1s_ = [[Buf("w1s%d" % e) for e in range(32)] for _ in range(2)]
    b_w3s_ = [[Buf("w3s%d" % e) for e in range(32)] for _ in range(2)]
    b_w2s_ = [[Buf("w2s%d" % q) for q in range(4)] for _ in range(2)]
    b_wins = [Buf("wins%d" % m) for m in range(48)]
    NSTG = 3
    stg = Ring([sb("stg%d" % i, [128, 1024], BF16)[:] for i in range(NSTG)], "stg")
    bg = {"steps": [], "queued": set()}

    def _mk_pre(src_ap, src_is_kpf, dst_ap, dst_bufs):
        slot = {}

        def L():
            s_, bs_ = stg.next()
            slot["s"] = (s_, bs_, (stg.i - 1) % NSTG)
            if src_is_kpf:
                dma("pool", s_.rearrange("p (k f) -> p k f", k=8), src_ap, "stgL%d" % slot["s"][2], [], [bs_])
            else:
                dma("pool", s_, src_ap, "stgL%d" % slot["s"][2], [], [bs_])

        def S_():
            s_, bs_, i = slot["s"]
            if src_is_kpf:
                dma("pool", dst_ap, s_, "stgS%d" % i, [bs_], dst_bufs)
            else:
                dma("pool", dst_ap, s_.rearrange("p (q d) -> p q d", q=4), "stgS%d" % i, [bs_], dst_bufs)
        return L, S_

    def queue_precast(kind, l):
        if (kind, l) in bg["queued"]:
            return
        bg["queued"].add((kind, l))
        pairs = []
        if kind == "moe":
            w1s, w3s, w2s = w1s_[l % 2], w3s_[l % 2], w2s_[l % 2]
            b_w1s, b_w3s, b_w2s = b_w1s_[l % 2], b_w3s_[l % 2], b_w2s_[l % 2]
            for e in range(32):
                pairs.append(_mk_pre(di["moe_w1"][l, e].rearrange("(k p) f -> p k f", p=128), True, w1s[e], [b_w1s[e]]))
                pairs.append(_mk_pre(di["moe_w3"][l, e].rearrange("(k p) f -> p k f", p=128), True, w3s[e], [b_w3s[e]]))
            for e in range(32):
                pairs.append(_mk_pre(di["moe_w2"][l, e], False, w2s[:, :, e, :].rearrange("q f d -> f q d"), b_w2s))
        else:
            i_ = l // 2
            for m in range(48):
                pairs.append(_mk_pre(di["ssm_w_in"][i_, :, m * 128:(m + 1) * 128].rearrange("(k p) f -> p k f", p=128),
                                     True, wins[m], [b_wins[m]]))
        n = len(pairs)
        for i in range(n + 1):
            if i < n:
                bg["steps"].append(pairs[i][0])
            if i >= 1:
                bg["steps"].append(pairs[i - 1][1])

    def bg_pump(n):
        while n > 0 and bg["steps"]:
            bg["steps"].pop(0)()
            n -= 1

    def bg_flush():
        bg_pump(1 << 30)

    AW = cfg.get("arena_words", 41616)
    arena = sb("arena", [128, AW], F32)
    ar = {"off": 0}

    def carve(shape, dt):
        n = 1
        for s_ in shape[1:]:
            n *= s_
        words = (n * (4 if dt == F32 else 2) + 3) // 4
        o = ar["off"]
        assert o + words <= AW, ("arena overflow", o, words, AW)
        ar["off"] = o + words
        v = arena[0:shape[0], o:o + words]
        if dt != F32:
            v = v.bitcast(dt)
        if len(shape) == 3:
            v = v.rearrange("p (a b) -> p a b", a=shape[1])
        elif len(shape) == 4:
            v = v.rearrange("p (a b c) -> p a b c", a=shape[1], b=shape[2])
        elif len(shape) == 5:
            v = v.rearrange("p (a b c d) -> p a b c d", a=shape[1], b=shape[2], c=shape[3])
        return v

    def new_phase():
        P.barrier()
        ar["off"] = 0

    SH1, A1, G1, SH2, A2, G2 = [slice(i * D, (i + 1) * D) for i in range(6)]

    def ada_phase(l):
        new_phase()
        ring = Ring([carve([128, 8, 512], F32) for _ in range(2)], "adaw")
        tmpw = carve([128, D], F32)
        b_tmpw = Buf("tmpw")
        dma("sync", mod_bc[:], di["ada_b"][l, :].partition_broadcast(128), "modb", [], [b_mod])
        for n in range(12):
            sl, bsl = ring.next()
            dma("sync", sl, di["ada_w"][l, :, n * 512:(n + 1) * 512].rearrange("(k p) n -> p k n", p=128),
                "adaw%d" % (n % 2), [], [bsl])
            for k in range(8):
                mm(pb[n % 2][:], cactB[:, k, :], sl[:, k, :], k == 0, k == 7, [bsl, b_const], [b_pb[n % 2]])
            tt("dve", mod_bc[:, n * 512:(n + 1) * 512], pb[n % 2][:], mod_bc[:, n * 512:(n + 1) * 512], ALU.add,
               [b_pb[n % 2], b_mod], [b_mod])
        for (nm, sl_) in (("norm1_w", A1), ("norm2_w", A2)):
            dma("sync", tmpw, di[nm][l, :].partition_broadcast(128), "tmpw", [], [b_tmpw])
            stt("dve", mod_bc[:, sl_], mod_bc[:, sl_], 1.0, tmpw, ALU.add, ALU.mult, [b_mod, b_tmpw], [b_mod])

    def make_norm(src_rows, src_bufs, Asl, Ssl, lean=False):
        st = {}
        st["xin"] = Ring([carve([128, D], F32) for _ in range(2)], "xin")
        st["hf"] = None if lean else Ring([carve([128, D], F32)], "hf")
        st["hb"] = Ring([carve([128, D], BF16) for _ in range(2)], "hb")
        st["junk"] = (carve([128, D], BF16), Buf("junk"))
        st["sm"] = Ring([carve([128, 4], F32) for _ in range(2)], "nsm")
        st["cnt"] = 0

        def norm_ew(blk):
            xin, bx = st["xin"].next()
            hb, bhb = st["hb"].next()
            hf, bhf = (hb, bhb) if lean else st["hf"].next()
            junk, bj = st["junk"]
            sm, bsm = st["sm"].next()
            dma("sync", xin, src_rows(blk), "xin%d" % ((st["xin"].i - 1) % 2), [src_bufs[blk]], [bx])
            memset("dve", sm[:, 0:1], 0.0, [bsm])
            act(junk, xin, AF.Square, [bx, bsm], [bj, bsm], accum_out=sm[:, 0:1])
            act(sm[:, 1:2], sm[:, 0:1], AF.Ln, [bsm], [bsm], bias=EPS, scale=1.0 / D)
            act(sm[:, 2:3], sm[:, 1:2], AF.Exp, [bsm], [bsm], scale=-0.5)
            stt("dve", hf, xin, sm[:, 2:3], mod_bc[:, Asl], ALU.mult, ALU.mult, [bx, bsm, b_mod], [bhf])
            tt("dve", hb, hf, mod_bc[:, Ssl], ALU.add, [bhf, b_mod] if not lean else [bhb, b_mod], [bhb])
            return hb, bhb

        def norm_tr(hb, bhb, hT, hT_buf, col0):
            pbank = pc[1 + st["cnt"] % 2]
            bbank = b_pc[1 + st["cnt"] % 2]
            st["cnt"] += 1
            pv = pbank[:].bitcast(BF16)
            for k in range(8):
                trn(pv[:, k * 128:(k + 1) * 128], hb[:, k * 128:(k + 1) * 128], [bhb, b_const], [bbank])
            act(hT[:, :, col0:col0 + 128], pv.rearrange("p (k t) -> p k t", k=8), AF.Copy, [bbank], [hT_buf])

        def norm_block(blk, hT, hT_buf, col0):
            hb, bhb = norm_ew(blk)
            norm_tr(hb, bhb, hT, hT_buf, col0)
        norm_block.ew = norm_ew
        norm_block.tr = norm_tr
        return norm_block

    def moe_phase(l, src_rows, src_bufs):
        queue_precast("moe", l)
        bg_flush()
        new_phase()
        w1s, w3s, w2s = w1s_[l % 2], w3s_[l % 2], w2s_[l % 2]
        b_w1s, b_w3s, b_w2s = b_w1s_[l % 2], b_w3s_[l % 2], b_w2s_[l % 2]
        if l + 1 in layers and (l + 1) % 2 == 1:
            if do_mixer:
                queue_precast("ssd", l + 1)
            if do_moe:
                queue_precast("moe", l + 1)
        per_tile = (len(bg["steps"]) + NTc - 1) // max(NTc, 1)
        hTs = [carve([128, 8, 512], BF16) for _ in range(2)]
        hTbs = [[Buf("hT%d_%d" % (i, j)) for j in range(4)] for i in range(2)]
        norm_block = make_norm(src_rows, src_bufs, A2, SH2)
        wr = carve([128, 8, 36], BF16)
        b_wr = Buf("wr")
        rb_bc = carve([128, 36], F32)
        hid = carve([128, 32, 512], BF16)
        hid_b = [Buf("hid%d" % e) for e in range(32)]
        w2r = Ring([carve([128, 32, 256], BF16) for _ in range(3)], "w2q")
        w13 = Ring([carve([128, 2, 8, 128], BF16) for _ in range(3)], "w13")
        sTr = Ring([carve([128, 512], BF16) for _ in range(2)], "sT")
        tTr = Ring([carve([128, 512], BF16) for _ in range(2)], "tT")
        combT = Ring([carve([32, 512], BF16) for _ in range(2)], "combT")
        xout = carve([128, 4, D], F32)
        xout_b = [Buf("xout%d" % j) for j in range(4)]
        tmpq = Ring([carve([128, 256], F32) for _ in range(2)], "tmpq")
        rs = Ring([carve([128, 160], F32) for _ in range(2)], "rs")
        combb = Ring([carve([128, 32], BF16) for _ in range(2)], "combb")

        dma("pool", wr[:, :, 0:4], di["moe_w_group"][l].rearrange("(k p) g -> p k g", p=128), "wr", [], [b_wr])
        dma("pool", wr[:, :, 4:36], di["moe_w_expert"][l].rearrange("(k p) g e -> p k (g e)", p=128), "wr", [], [b_wr])
        dma("sync", rb_bc[:, 0:4], di["moe_b_group"][l, :].partition_broadcast(128), "rbbc", [], [b_wr])
        dma("sync", rb_bc[:, 4:36], di["moe_b_expert"][l].rearrange("g e -> (g e)").partition_broadcast(128),
            "rbbc", [], [b_wr])

        def nr_parts(t):
            hT, hTb = hTs[t % 2], hTbs[t % 2]
            cT_, b_cT = combT.next()
            stt_ = {}

            def A(j):
                stt_[("hb", j)] = norm_block.ew(t * 4 + j)

            def B(j):
                hb, bhb = stt_[("hb", j)]
                norm_block.tr(hb, bhb, hT, hTb[j], j * 128)
                r, br = rs.next()
                lg = r[:, 0:36]
                for k in range(8):
                    mm(pc[0][:, 0:36], hT[:, k, j * 128:(j + 1) * 128], wr[:, k, :], k == 0, k == 7,
                       [hTb[j], b_wr], [b_pc[0]])
                tt("dve", lg, pc[0][:, 0:36], rb_bc, ALU.add, [b_pc[0], b_wr], [br])
                gmax = r[:, 40:41]; ngmax = r[:, 41:42]; gsum = r[:, 42:43]; gval = r[:, 43:44]
                P.op("dve", lambda e, o=gmax, i=r[:, 0:4]: e.tensor_reduce(out=o, in_=i, axis=AX.X, op=ALU.max), [br], [br])
                ts("dve", ngmax, gmax, -1.0, ALU.mult, [br], [br])
                memset("dve", gsum, 0.0, [br])
                act(r[:, 44:48], r[:, 0:4], AF.Exp, [br], [br], bias=ngmax, scale=1.0, accum_out=gsum)
                P.op("dve", lambda e, o=gval, i=gsum: e.reciprocal(out=o, in_=i), [br], [br])
                gmask = r[:, 48:52]
                ts("dve", gmask, r[:, 0:4], gmax, ALU.is_equal, [br], [br])
                ts("dve", gmask, gmask, 1.0, ALU.subtract, [br], [br], s2=30000.0, op1=ALU.mult)
                em = r[:, 56:88]
                tt("dve", em.rearrange("p (g e) -> p g e", g=4), r[:, 4:36].rearrange("p (g e) -> p g e", g=4),
                   gmask.unsqueeze(2).to_broadcast([128, 4, 8]), ALU.add, [br], [br])
                top8 = r[:, 88:96]
                P.op("dve", lambda e, o=top8, i=em: e.max(out=o, in_=i), [br], [br])
                eq1 = r[:, 96:128]; eq2 = r[:, 128:160]
                ts("dve", eq1, em, top8[:, 0:1], ALU.is_equal, [br], [br])
                ts("dve", eq2, em, top8[:, 1:2], ALU.is_equal, [br], [br])
                dd = r[:, 36:37]; ex = r[:, 37:38]; w1g = r[:, 38:39]; w2g = r[:, 39:40]
                tt("dve", dd, top8[:, 1:2], top8[:, 0:1], ALU.subtract, [br], [br])
                act(ex, dd, AF.Exp, [br], [br])
                ts("dve", w1g, ex, 1.0, ALU.add, [br], [br])
                P.op("dve", lambda e, o=w1g, i=w1g: e.reciprocal(out=o, in_=i), [br], [br])
                tt("dve", w2g, ex, w1g, ALU.mult, [br], [br])
                tt("dve", w1g, w1g, gval, ALU.mult, [br], [br])
                tt("dve", w2g, w2g, gval, ALU.mult, [br], [br])
                ts("dve", eq1, eq1, w1g, ALU.mult, [br], [br])
                stt("dve", eq1, eq2, w2g, eq1, ALU.mult, ALU.add, [br], [br])
                cb_, bcb = combb.next()
                cp("dve", cb_, eq1, [br], [bcb])
                stt_[("cb", j)] = (cb_, bcb)

            def C(j):
                cb_, bcb = stt_[("cb", j)]
                pv = pc[0][:].bitcast(BF16)
                P.op("pe", lambda e, o=pv[0:32, 128:256], i=cb_: e.transpose(o, i, ident_bf[:]), [bcb, b_const], [b_pc[0]])
                cp("dve", cT_[:, j * 128:(j + 1) * 128], pv[0:32, 128:256], [b_pc[0]], [b_cT])
            return A, B, C, (cT_, b_cT)

        def norm_router(t):
            A, B, C, ret = nr_parts(t)
            for j in range(4):
                A(j)
                B(j)
                C(j)
            return ret

        def pass1(t, cT_, b_cT):
            hT, hTb = hTs[t % 2], hTbs[t % 2]
            for e_ in range(32):
                w_, bw = w13.next()
                key = "w13_%d" % ((w13.i - 1) % 3)
                dma("sync", w_[:, 0], w1s[e_].rearrange("p (k f) -> p k f", k=8), key, [b_w1s[e_]], [bw])
                dma("sync", w_[:, 1], w3s[e_].rearrange("p (k f) -> p k f", k=8), key, [b_w3s[e_]], [bw])
                if e_ % 2 == 0:
                    h1, bh1, h3, bh3, cbk, bcbk = pb[0][:], b_pb[0], pb[1][:], b_pb[1], pb[2][:], b_pb[2]
                else:
                    h1, bh1, h3, bh3, cbk, bcbk = pS[:, 0:512], b_pS[0], pS[:, 512:1024], b_pS[1], pc[0][:], b_pc[0]
                for k in range(8):
                    mm(h1, w_[:, 0, k, :], hT[:, k, :], k == 0, k == 7, [bw] + hTb, [bh1])
                for k in range(8):
                    mm(h3, w_[:, 1, k, :], hT[:, k, :], k == 0, k == 7, [bw] + hTb, [bh3])
                mm(cbk, SelE[:, e_, :], cT_, True, True, [b_const, b_cT], [bcbk])
                sT, bsT = sTr.next()
                tT, btT = tTr.next()
                act(sT, h1, AF.Silu, [bh1], [bsT])
                tt("dve", tT, h3, sT, ALU.mult, [bh3, bsT], [btT])
                tt("dve", hid[:, e_, :], cbk, tT, ALU.mult, [bcbk, btT], [hid_b[e_]])

        w2slots = {}

        def load_w2(t, q):
            w2_, bw2 = w2r.next()
            dma("sync", w2_, w2s[q], "w2q_%d" % ((w2r.i - 1) % 3), [b_w2s[q]], [bw2])
            w2slots[(t, q)] = (w2_, bw2)

        def pass2(t, nxt=None):
            bg_pump(per_tile)
            for j in range(4):
                blk = t * 4 + j
                dma("sync", xout[:, j, :], src_rows(blk), "xout%d" % j, [src_bufs[blk]], [xout_b[j]])
            for q in range(4):
                if nxt is not None:
                    nxt[0](q)
                w2_, bw2 = w2slots[(t, q)]
                if q + 2 < 4:
                    load_w2(t, q + 2)
                elif t + 1 < NTc:
                    load_w2(t + 1, q - 2)
                for j in range(4):
                    bi = 1 + (q * 4 + j) % 2
                    for e_ in range(32):
                        mm(pc[bi][:, 0:256], hid[:, e_, j * 128:(j + 1) * 128], w2_[:, e_, :], e_ == 0, e_ == 31,
                           [hid_b[e_], bw2], [b_pc[bi]])
                    tq, btq = tmpq.next()
                    tt("dve", tq, pc[bi][:, 0:256], mod_bc[:, 5 * D + q * 256:5 * D + (q + 1) * 256], ALU.mult,
                       [b_pc[bi], b_mod], [btq])
                    tt("dve", xout[:, j, q * 256:(q + 1) * 256], tq, xout[:, j, q * 256:(q + 1) * 256], ALU.add,
                       [btq, xout_b[j]], [xout_b[j]])
                if nxt is not None:
                    nxt[1](q)
                    if q >= 1:
                        nxt[2](q - 1)
            for j in range(4):
                blk = t * 4 + j
                dma("sync", xr[blk * 128:(blk + 1) * 128, :], xout[:, j, :], "xo%d" % j, [xout_b[j]], [xr_b[blk]])

        pend = norm_router(0)
        load_w2(0, 0)
        load_w2(0, 1)
        for t in range(NTc):
            pass1(t, *pend)
            if t + 1 < NTc:
                A, B, C, pend = nr_parts(t + 1)
                pass2(t, (A, B, C))
                C(3)
            else:
                pass2(t)

    def even_phase(l, src_rows, src_bufs):
        i_ = l // 2
        new_phase()
        if do_moe:
            queue_precast("moe", l)
        per_tile = (len(bg["steps"]) + NTc - 1) // max(NTc, 1)
        hT = carve([128, 8, 512], BF16)
        hTb = [Buf("hT%d" % j) for j in range(4)]
        norm_block = make_norm(src_rows, src_bufs, A1, SH1)
        Win = carve([128, 8, 2048], BF16); b_win = Buf("win")
        Wout = carve([128, 8, D], BF16); b_wout = Buf("wout")
        poolw = carve([128, 4, 128], BF16)
        pscale = carve([128, 4], F32)
        expb = carve([128, 8, 640], BF16); b_expb = Buf("expb")
        kT = carve([128, 4, 1024], BF16)
        kT_b = [Buf("kT%d" % s_) for s_ in range(8)]
        vaug = carve([128, 8, 4, 2, 128], BF16)
        va_b = [Buf("va%d" % s_) for s_ in range(8)]
        qT = carve([128, 4, 512], BF16); b_q = Buf("qT")
        ubuf = carve([128, 4, 527], F32); b_u = [Buf("u%d" % m) for m in range(4)]
        sA = carve([128, 527], F32); sB = carve([128, 527], F32); b_sA = Buf("sA"); b_sB = Buf("sB")
        pooled = Ring([carve([128, 512], BF16) for _ in range(4)], "pooled")
        invc0 = carve([128, 4, 16], F32)
        catT = carve([128, 8, 512], BF16); cat_b = [Buf("cat%d" % m) for m in range(8)]
        Pexp = Ring([carve([128, 640], BF16) for _ in range(2)], "Pexp")
        PT = Ring([carve([128, 640], BF16) for _ in range(2)], "PT")
        rsr = Ring([carve([128, 128], F32) for _ in range(2)], "rsr")
        xres = Ring([carve([128, D], F32) for _ in range(1)], "xres")
        xo = Ring([carve([128, D], F32) for _ in range(1)], "xo")
        hk = carve([128, 5, 128], F32); b_hk = Buf("hk")
        rb8 = xo.slots[0][0][0:8, :]; b_rb8 = xo.slots[0][1]

        for k in range(8):
            dma("pool", Win[:, k, :], di["mix_w_in"][i_, k * 128:(k + 1) * 128, :], "win", [], [b_win])
            dma("pool", Wout[:, k, :], di["mix_w_out"][i_, k * 128:(k + 1) * 128, :], "wout", [], [b_wout])
        dma("pool", poolw, di["pool_w"][i_].rearrange("g c d -> c g d"), "win", [], [b_win])
        dma("sync", pscale, di["pool_scale"][i_, :].rearrange("(m p) -> p m", p=128), "pscale", [], [b_win],
            allow_slow_non_contiguous=True)
        memset("pool", vaug[:, :, :, 0, 64:128], 1.0, va_b)
        memset("pool", vaug[:, :, :, 1, 0:64], 1.0, va_b)
        memset("pool", ubuf[:], 0.0, b_u)
        for m in range(4):
            w = 2 ** (m + 1)
            memset("pool", invc0[:, m, :], 1.0 / w, [b_const])
            for pos in range(w - 1):
                memset("pool", invc0[:, m, pos:pos + 1], 1.0 / (pos + 1), [b_const])
        dma("sync", rb8[:, 0:513], di["rel_bias"][i_], "rb8", [], [b_rb8])
        cp("dve", rb8[:, 513:897], rb8[:, 512:513].to_broadcast([8, 384]), [b_rb8], [b_rb8])
        dma("sync", rbx[:, 0:768], rb8[:, 129:897], "rbx", [b_rb8], [b_rbx])
        for h in range(8):
            src = bass.AP(rbx.tensor, h * 1024, [[1, 128], [128, 5], [1, 128]])
            dma("sync", hk, src, "hk", [b_rbx], [b_hk])
            hk2 = hk.rearrange("p a b -> p (a b)")
            mm(pS[:, 0:512], Jm[:], hk2[:, 0:512], True, True, [b_const, b_hk], [b_pS[0]])
            mm(pS[:, 512:640], Jm[:], hk2[:, 512:640], True, True, [b_const, b_hk], [b_pS[1]])
            act(expb[:, h, :], pS[:, 0:640], AF.Exp, b_pS, [b_expb])
        memset("pool", expb[64:128, :, 0:64], 0.0, [b_expb])
        memset("pool", expb[0:64, :, 4 * 128 + 64:5 * 128], 0.0, [b_expb])

        obr = Ring([pc[0][:, i * 128:(i + 1) * 128] for i in range(4)], "ob")
        pcnt = [0]

        def pbank():
            i = pcnt[0] % 3
            pcnt[0] += 1
            return pb[i], b_pb[i]

        for t in range(NTc):
            if t == 0:
                for j in range(4):
                    norm_block(t * 4 + j, hT, hTb[j], j * 128)
            bg_pump(per_tile)
            pool_defer = []
            for m in range(4):
                bank, bb = pbank()
                for k in range(8):
                    mm(bank[:], Win[:, k, m * 128:(m + 1) * 128], hT[:, k, :], k == 0, k == 7, [b_win] + hTb, [bb])
                if t > 0:
                    cp("dve", ubuf[:, m, 0:15], ubuf[:, m, 512:527], [b_u[m]], [b_u[m]])
                act(ubuf[:, m, 15:527], bank[:], AF.Copy, [bb], [b_u[m]])
                u = ubuf[:, m, :]
                tt("dve", sA[:, 1:527], u[:, 1:527], u[:, 0:526], ALU.add, [b_u[m]], [b_sA])
                cur, bcur = sA, b_sA
                if m >= 1:
                    tt("dve", sB[:, 3:527], sA[:, 3:527], sA[:, 1:525], ALU.add, [b_sA], [b_sB])
                    cur, bcur = sB, b_sB
                if m >= 2:
                    tt("dve", sA[:, 7:527], sB[:, 7:527], sB[:, 3:523], ALU.add, [b_sB], [b_sA])
                    cur, bcur = sA, b_sA
                if m >= 3:
                    tt("dve", sB[:, 15:527], sA[:, 15:527], sA[:, 7:519], ALU.add, [b_sA], [b_sB])
                    cur, bcur = sB, b_sB
                pl, bpl = pooled.next()
                w = 2 ** (m + 1)
                stt("dve", pl, cur[:, 15:527], 1.0 / w, u[:, 15:527], ALU.mult, ALU.subtract, [bcur, b_u[m]], [bpl])
                if t == 0:
                    tt("dve", cur[:, 15:31], cur[:, 15:31], invc0[:, m, :], ALU.mult, [bcur, b_const], [bcur])
                    tt("dve", pl[:, 0:16], cur[:, 15:31], u[:, 15:31], ALU.subtract, [bcur, b_u[m]], [bpl])
                pool_defer.append((m, pl, bpl))
            for m in range(4):
                bank, bb = pbank()
                for k in range(8):
                    mm(bank[:], Win[:, k, 512 + m * 128:512 + (m + 1) * 128], hT[:, k, :], k == 0, k == 7,
                       [b_win] + hTb, [bb])
                act(qT[:, m, :], bank[:], AF.Copy, [bb], [b_q])
            ks0 = (t % 2) * 4
            for m in range(4):
                bank, bb = pbank()
                for k in range(8):
                    mm(bank[:], Win[:, k, 1024 + m * 128:1024 + (m + 1) * 128], hT[:, k, :], k == 0, k == 7,
                       [b_win] + hTb, [bb])
                act(kT[:, m, ks0 * 128:(ks0 + 4) * 128], bank[:], AF.Copy, [bb], kT_b[ks0:ks0 + 4])
            for j in range(4):
                sl = ks0 + j
                bank, bb = pbank()
                for k in range(8):
                    mm(bank[:], hT[:, k, j * 128:(j + 1) * 128], Win[:, k, 1536:2048], k == 0, k == 7,
                       [b_win, hTb[j]], [bb])
                pvv = bank[:].rearrange("p (c two d) -> p c two d", c=4, two=2)
                cp("dve", vaug[:, sl, :, 0, 0:64], pvv[:, :, 0, :], [bb], [va_b[sl]])
                cp("dve", vaug[:, sl, :, 1, 64:128], pvv[:, :, 1, :], [bb], [va_b[sl]])
            for (m, pl, bpl) in pool_defer:
                bank, bb = pbank()
                mm(bank[:], poolw[:, m, :], pl, True, True, [b_win, bpl], [bb])
                ts("dve", catT[:, m, :], bank[:], pscale[:, m:m + 1], ALU.mult, [bb, b_win], [cat_b[m]])
            for j in range(4):
                jq = t * 4 + j
                nxt_hb = norm_block.ew((t + 1) * 4 + j) if t + 1 < NTc else None
                blocks = [i for i in range(jq - 4, jq + 1) if i >= 0]
                for h in range(8):
                    c, hp = h // 2, h % 2
                    p0 = 64 * hp
                    for i in blocks:
                        o = jq - i
                        sl = i % 8
                        mm(pS[:, o * 128:(o + 1) * 128], kT[p0:p0 + 64, c, sl * 128:(sl + 1) * 128],
                           qT[p0:p0 + 64, c, j * 128:(j + 1) * 128], True, True, [kT_b[sl], b_q], [b_pS[o // 4]])
                    o_lo = jq - blocks[-1]
                    o_hi = jq - blocks[0]
                    c0, c1 = o_lo * 128, (o_hi + 1) * 128
                    pe_, bpe = Pexp.next()
                    pt_, bpt = PT.next()
                    rd = [b_pS[0]] + ([b_pS[1]] if o_hi == 4 else [])
                    act(pe_[:, c0:c1], pS[:, c0:c1], AF.Exp, rd, [bpe], scale=0.125)
                    tt("dve", pt_[:, c0:c1], pe_[:, c0:c1], expb[:, h, c0:c1], ALU.mult, [bpe, b_expb], [bpt])
                    ob, _ = obr.next()
                    bob = b_pc[0]
                    for n_, i in enumerate(blocks):
                        o = jq - i
                        sl = i % 8
                        mm(ob, vaug[:, sl, c, hp, :], pt_[:, o * 128:(o + 1) * 128], n_ == 0,
                           n_ == len(blocks) - 1, [va_b[sl], bpt], [bob])
                    r_, br_ = rsr.next()
                    q0 = 64 * (1 - hp)
                    P.op("dve", lambda e, o_=r_[p0:p0 + 64, :], i_2=ob[q0:q0 + 64, :]: e.reciprocal(out=o_, in_=i_2),
                         [bob], [br_])
                    tt("dve", catT[p0:p0 + 64, 4 + c, j * 128:(j + 1) * 128], ob[p0:p0 + 64, :], r_[p0:p0 + 64, :],
                       ALU.mult, [bob, br_], [cat_b[4 + c]])
                if nxt_hb is not None:
                    norm_block.tr(nxt_hb[0], nxt_hb[1], hT, hTb[j], j * 128)
            for j in range(4):
                blk = t * 4 + j
                xr_, bxr = xres.next()
                xo_, bxo = xo.next()
                dma("sync", xr_, src_rows(blk), "xres0", [src_bufs[blk]], [bxr])
                for half in range(2):
                    bank, bb = pbank()
                    for kk in range(8):
                        mm(bank[:], catT[:, kk, j * 128:(j + 1) * 128], Wout[:, kk, half * 512:(half + 1) * 512],
                           kk == 0, kk == 7, [cat_b[kk], b_wout], [bb])
                    tt("dve", xo_[:, half * 512:(half + 1) * 512], bank[:],
                       mod_bc[:, 2 * D + half * 512:2 * D + (half + 1) * 512], ALU.mult, [bb, b_mod], [bxo])
                    tt("dve", xo_[:, half * 512:(half + 1) * 512], xo_[:, half * 512:(half + 1) * 512],
                       xr_[:, half * 512:(half + 1) * 512], ALU.add, [bxo, bxr], [bxo])
                dma("sync", xr[blk * 128:(blk + 1) * 128, :], xo_, "xo0", [bxo], [xr_b[blk]])

    def ssd_phase(l, src_rows, src_bufs):
        i_ = l // 2
        queue_precast("ssd", l)
        bg_flush()
        new_phase()
        per_tile = (len(bg["steps"]) + NTc * 2 - 1) // max(NTc * 2, 1)
        TT_ = 256
        hT = carve([128, 8, TT_], BF16)
        hTb = [Buf("hT%d" % j) for j in range(2)]
        norm_block = make_norm(src_rows, src_bufs, A1, SH1, lean=True)
        Wout = carve([128, 16, D], BF16); b_wout = Buf("wout")
        wch = Ring([carve([128, 8, 128], BF16) for _ in range(3)], "wch")
        wz = Ring([carve([128, 2, 8, 128], BF16) for _ in range(2)], "wz")
        wdt = carve([128, 8, 32], BF16); b_wdt = Buf("wdt")
        cw = carve([128, 4, 32], F32); cbias = carve([128, 32], F32); b_cw = Buf("cw")
        halo = carve([128, 32, 3], F32); b_halo = [Buf("halo%d" % m) for m in range(32)]
        rawb = Ring([carve([128, TT_ + 3], F32) for _ in range(2)], "rawb")
        accr = Ring([carve([128, TT_], F32) for _ in range(2)], "acc")
        xT = carve([128, 16, TT_], BF16); BT = carve([128, 8, TT_], BF16); CT = carve([128, 8, TT_], BF16)
        xT_b = [Buf("xT%d" % m) for m in range(16)]; BT_b = [Buf("BT%d" % m) for m in range(8)]
        CT_b = [Buf("CT%d" % m) for m in range(8)]
        xtm = Ring([carve([128, 2048], BF16)], "xtm")
        xdt = Ring([carve([128, 2048], BF16)], "xdt")
        xdd = Ring([carve([128, 2048], BF16)], "xdd")
        Btm = Ring([carve([128, 1024], BF16)], "Btm")
        zs = Ring([carve([128, 2048], BF16) for _ in range(2)], "zs")
        smr = Ring([carve([128, 320], F32) for _ in range(2)], "ssm_sm")
        ahl = Ring([carve([128, 64], BF16) for _ in range(2)], "ahl")
        LTr = Ring([carve([128, 4, 128], BF16) for _ in range(2)], "LT")
        MTr = Ring([carve([128, 4, 128], BF16) for _ in range(2)], "MT")
        Gsr = Ring([carve([128, 128], BF16) for _ in range(2)], "Gs")
        state = carve([128, 8, 256], F32); stateb = carve([128, 8, 256], BF16)
        st_b = [Buf("st%d" % g) for g in range(8)]; stb_b = [Buf("stb%d" % g) for g in range(8)]
        ya = carve([128, 2048], F32); b_ya = [Buf("ya%d" % g) for g in range(8)]
        xD = carve([128, 2048], BF16); b_xD = Buf("xD")
        gn = carve([128, 2048], BF16); b_gn_h = [Buf("gn0"), Buf("gn1")]
        gnT = carve([128, 16, 128], BF16); b_gnT = [Buf("gnT0"), Buf("gnT1")]
        nw_bc = carve([128, 2048], BF16); vec_bc = carve([128, 96], F32); b_vec = Buf("vec")
        xres = Ring([carve([128, D], F32)], "xres")
        xo = Ring([carve([128, D], F32)], "xo")
        junk2 = carve([128, 256], BF16); b_j2 = Buf("junk2")

        for k in range(16):
            dma("pool", Wout[:, k, :], di["ssm_w_out"][i_, k * 128:(k + 1) * 128, :], "wout", [], [b_wout])
        dma("pool", wdt, di["ssm_w_in"][i_, :, 6144:6176].rearrange("(k p) f -> p k f", p=128), "wdt", [], [b_wdt])
        cwr = xo.slots[0][0][0:32, 0:640].rearrange("p (t c) -> p t c", t=5)
        b_cwr = xo.slots[0][1]
        for tap in range(4):
            dma("sync", cwr[:, tap, :], di["ssm_conv_w"][i_, tap, :].rearrange("(m p) -> m p", p=128), "cwr", [], [b_cwr])
        dma("sync", cwr[:, 4, :], di["ssm_conv_b"][i_, :].rearrange("(m p) -> m p", p=128), "cwr", [], [b_cwr])
        for tap in range(5):
            P.op("pe", (lambda t_: lambda e: e.transpose(pb[0][:, t_ * 32:(t_ + 1) * 32], cwr[:, t_, :], ident_f[0:32, 0:32]))(tap),
                 [b_cwr, b_const], [b_pb[0]])
        cp("dve", cw.rearrange("p t m -> p (t m)"), pb[0][:, 0:128], [b_pb[0]], [b_cw])
        cp("dve", cbias, pb[0][:, 128:160], [b_pb[0]], [b_cw])
        dma("pool", nw_bc, di["ssm_norm_w"][i_, :].partition_broadcast(128), "vecn", [], [b_vec])
        dtb_bc = vec_bc[:, 0:32]; A_bc = vec_bc[:, 32:64]; D_bc = vec_bc[:, 64:96]
        dma("sync", dtb_bc, di["ssm_dt_bias"][i_, :].partition_broadcast(128), "vec", [], [b_vec])
        dma("sync", A_bc, di["ssm_A_log"][i_, :].partition_broadcast(128), "vec", [], [b_vec])
        dma("sync", D_bc, di["ssm_D"][i_, :].partition_broadcast(128), "vec", [], [b_vec])
        act(A_bc, A_bc, AF.Exp, [b_vec], [b_vec])
        ts("dve", A_bc, A_bc, -1.0, ALU.mult, [b_vec], [b_vec])
        memset("pool", halo[:], 0.0, b_halo)
        memset("pool", state[:], 0.0, st_b)
        memset("pool", stateb[:], 0.0, stb_b)

        G_ps = pb[0][:, 0:128]
        b_small = b_pb[0]
        st_ps, b_stp = pb[0][:, 256:512], b_pb[0]
        yd_ps = pc[0][:, 0:256]
        yo_ps, b_yo = pc[0][:, 256:512], b_pc[0]
        lcnt = [0]
        hcnt = [0]

        def half():
            i = hcnt[0] % 2
            hcnt[0] += 1
            return pS[:, i * 512:(i + 1) * 512], b_pS[i]

        stop_ = cfg.get("ssd_stop", 99)
        if stop_ <= 1:
            return
        for t in range(NTc * 2):
            if t == 0:
                for j in range(2):
                    norm_block(t * 2 + j, hT, hTb[j], j * 128)
            bg_pump(per_tile)
            smv = []
            zv = []
            for j in range(2):
                jc = slice(j * 128, (j + 1) * 128)
                sm, bsm = smr.next()
                dt_ = sm[:, 0:32]; a_ = sm[:, 32:64]; acs = sm[:, 64:96]; nacs = sm[:, 96:128]
                tot = sm[:, 128:160]; eacs = sm[:, 160:192]; dstt = sm[:, 192:224]; cdec = sm[:, 224:256]
                ssq = sm[:, 256:264]; lnv = sm[:, 264:272]; rstd = sm[:, 272:280]
                sp = pb[0][:, 128:160]
                for k in range(8):
                    mm(sp, hT[:, k, jc], wdt[:, k, :], k == 0, k == 7, [hTb[j], b_wdt], [b_small])
                tt("dve", dt_, sp, dtb_bc, ALU.add, [b_small, b_vec], [bsm])
                act(dt_, dt_, AF.Exp, [bsm], [bsm])
                act(dt_, dt_, AF.Ln, [bsm], [bsm], bias=1.0)
                tt("dve", a_, dt_, A_bc, ALU.mult, [bsm, b_vec], [bsm])
                mm(pb[0][:, 160:192], Tm[:], a_, True, True, [b_const, bsm], [b_small])
                mm(pb[0][:, 192:224], ones_f[:], a_, True, True, [b_const, bsm], [b_small])
                cp("dve", acs, pb[0][:, 160:192], [b_small], [bsm])
                ts("dve", nacs, acs, -1.0, ALU.mult, [bsm], [bsm])
                cp("dve", tot, pb[0][:, 192:224], [b_small], [bsm])
                act(eacs, acs, AF.Exp, [bsm], [bsm])
                tt("dve", dstt, tot, acs, ALU.subtract, [bsm], [bsm])
                act(dstt, dstt, AF.Exp, [bsm], [bsm])
                act(cdec, tot, AF.Exp, [bsm], [bsm])
                ah_, bah = ahl.next()
                cp("dve", ah_[:, 0:32], a_, [bsm], [bah])
                tt("dve", sm[:, 288:320], a_, ah_[:, 0:32], ALU.subtract, [bsm, bah], [bsm])
                cp("dve", ah_[:, 32:64], sm[:, 288:320], [bsm], [bah])
                smv.append((sm, bsm, ah_, bah))
                z_, bz = zs.next()
                for pz in range(8):
                    wz_, bwz = wz.next()
                    dma("sync", wz_, wins[2 * pz:2 * pz + 2].rearrange("c p (k f) -> p c k f", k=8),
                        "wz%d" % ((wz.i - 1) % 2), [b_wins[2 * pz], b_wins[2 * pz + 1]], [bwz])
                    bank, bb = half()
                    for k in range(8):
                        mm(bank[:, 0:256].rearrange("p (c f) -> p c f", c=2), hT[:, k, jc], wz_[:, :, k, :], k == 0, k == 7,
                           [hTb[j], bwz], [bb])
                    act(z_[:, pz * 256:(pz + 1) * 256], bank[:, 0:256], AF.Silu, [bb], [bz])
                zv.append((z_, bz))
            pend_silu = None
            for m in range(32):
                w_, bw = wch.next()
                dma("sync", w_, wins[16 + m].rearrange("p (k f) -> p k f", k=8), "wch%d" % ((wch.i - 1) % 3),
                    [b_wins[16 + m]], [bw])
                bank, bb = half()
                for k in range(8):
                    mm(bank[:, 0:TT_], w_[:, k, :], hT[:, k, :], k == 0, k == 7, [bw] + hTb, [bb])
                rw, brw = rawb.next()
                ac, bac = accr.next()
                cp("dve", rw[:, 0:3], halo[:, m, :], [b_halo[m]], [brw])
                act(rw[:, 3:TT_ + 3], bank[:, 0:TT_], AF.Copy, [bb], [brw])
                if pend_silu is not None:
                    pend_silu()
                cp("dve", halo[:, m, :], rw[:, TT_:TT_ + 3], [brw], [b_halo[m]])
                ts("dve", ac, rw[:, 0:TT_], cw[:, 0, m:m + 1], ALU.mult, [brw, b_cw], [bac])
                for tap in range(1, 4):
                    stt("dve", ac, rw[:, tap:TT_ + tap], cw[:, tap, m:m + 1], ac, ALU.mult, ALU.add,
                        [brw, b_cw, bac], [bac])
                if m < 16:
                    dst_, bd = xT[:, m, :], xT_b[m]
                elif m < 24:
                    dst_, bd = BT[:, m - 16, :], BT_b[m - 16]
                else:
                    dst_, bd = CT[:, m - 24, :], CT_b[m - 24]

                def _silu(dst_=dst_, ac=ac, bac=bac, bd=bd, m=m):
                    act(dst_, ac, AF.Silu, [bac, b_cw], [bd], bias=cbias[:, m:m + 1])
                pend_silu = _silu
            pend_silu()
            if stop_ <= 2:
                return
            for j in range(2):
                blk = t * 2 + j
                jc = slice(j * 128, (j + 1) * 128)
                sm, bsm, ah_, bah = smv[j]
                z_, bz = zv[j]
                dt_ = sm[:, 0:32]; a_ = sm[:, 32:64]; acs = sm[:, 64:96]; nacs = sm[:, 96:128]
                tot = sm[:, 128:160]; eacs = sm[:, 160:192]; dstt = sm[:, 192:224]; cdec = sm[:, 224:256]
                ssq = sm[:, 256:264]; lnv = sm[:, 264:272]; rstd = sm[:, 272:280]
                if stop_ <= 3:
                    return
                xt_, bxt = xtm.next(); xd_, bxd = xdt.next(); xdd_, bxdd = xdd.next(); bt_, bbt = Btm.next()
                for hf_ in range(2):
                    pbank, bbank = pc[1 + hf_], b_pc[1 + hf_]
                    pv = pbank[:].bitcast(BF16)
                    for k in range(8):
                        trn(pv[:, k * 128:(k + 1) * 128], xT[:, hf_ * 8 + k, jc], [xT_b[hf_ * 8 + k], b_const], [bbank])
                    cs = slice(hf_ * 1024, (hf_ + 1) * 1024)
                    act(xt_[:, cs], pv, AF.Copy, [bbank], [bxt])
                    if stop_ <= 3.2:
                        continue
                    tt("dve", xd_[:, cs].rearrange("p (h d) -> p h d", h=16), xt_[:, cs].rearrange("p (h d) -> p h d", h=16),
                       dt_[:, hf_ * 16:(hf_ + 1) * 16].unsqueeze(2).to_broadcast([128, 16, 64]), ALU.mult,
                       [bxt, bsm], [bxd])
                if stop_ <= 3.4:
                    return
                pv = pc[1][:].bitcast(BF16)
                for g in range(8):
                    trn(pv[:, g * 128:(g + 1) * 128], BT[:, g, jc], [BT_b[g], b_const], [b_pc[1]])
                act(bt_, pv, AF.Copy, [b_pc[1]], [bbt])
                tt("dve", xdd_.rearrange("p (h d) -> p h d", h=32), xd_.rearrange("p (h d) -> p h d", h=32),
                   dstt.unsqueeze(2).to_broadcast([128, 32, 64]), ALU.mult, [bxd, bsm], [bxdd])
                tt("dve", xD.rearrange("p (h d) -> p h d", h=32), xt_.rearrange("p (h d) -> p h d", h=32),
                   D_bc.unsqueeze(2).to_broadcast([128, 32, 64]), ALU.mult, [bxt, b_vec], [b_xD])
                if stop_ <= 4:
                    return
                nxt_hb = norm_block.ew((t + 1) * 2 + j) if t + 1 < NTc * 2 else None
                def banks(g):
                    if g % 2 == 0:
                        return (pb[0][:, 0:128], b_pb[0], pb[0][:, 256:512], b_pb[0], pb[1], b_pb[1],
                                pc[0][:, 0:256], b_pc[0], pc[0][:, 256:512], b_pc[0])
                    return (pS[:, 0:128], b_pS[0], pS[:, 256:512], b_pS[0], pb[2], b_pb[2],
                            pS[:, 512:768], b_pS[1], pS[:, 768:1024], b_pS[1])

                def preA(g):
                    Gp, bG, st_ps, b_stp, Dp, bDp, yd, byd, yo_ps, b_yo = banks(g)
                    mm(Gp, BT[:, g, jc], CT[:, g, jc], True, True, [BT_b[g], CT_b[g]], [bG])
                    LT, bLT = LTr.next()
                    for r in range(4):
                        h = 4 * g + r
                        reg = Dp[:, r * 128:(r + 1) * 128]
                        mm(reg, ah_[:, h:h + 1].to_broadcast([128, 128]), Tm_bf[:], True, False, [bah, b_const], [bDp])
                        mm(reg, ah_[:, 32 + h:33 + h].to_broadcast([128, 128]), Tm_bf[:], False, False, [bah, b_const], [bDp])
                        mm(reg, ident_bf[:], negm[:], False, True, [b_const], [bDp])
                    for r in range(4):
                        h = 4 * g + r
                        act(LT[:, r, :], Dp[:, r * 128:(r + 1) * 128], AF.Exp, [bDp, bsm], [bLT], bias=nacs[:, h:h + 1])
                    Gs, bGs = Gsr.next()
                    act(Gs, Gp, AF.Copy, [bG], [bGs])
                    return LT, bLT, Gs, bGs

                def preB(g, LT, bLT, Gs, bGs):
                    MT, bMT = MTr.next()
                    tt("dve", MT, LT, Gs.unsqueeze(1).to_broadcast([128, 4, 128]), ALU.mult, [bLT, bGs], [bMT])
                    return MT, bMT

                def post(g, MT, bMT):
                    Gp, bG, st_ps, b_stp, Dp, bDp, yd, byd, yo_ps, b_yo = banks(g)
                    for r in range(4):
                        h = 4 * g + r
                        mm(yd[:, r * 64:(r + 1) * 64], MT[:, r, :], xd_[:, h * 64:(h + 1) * 64], True, True, [bMT, bxd], [byd])
                    gs = slice(g * 256, (g + 1) * 256)
                    mm(yo_ps, CT[:, g, jc], stateb[:, g, :], True, True, [CT_b[g], stb_b[g]], [b_yo])
                    mm(st_ps, bt_[:, g * 128:(g + 1) * 128], xdd_[:, gs], True, True, [bbt, bxdd], [b_stp])

                def post_ew(g):
                    Gp, bG, st_ps, b_stp, Dp, bDp, yd, byd, yo_ps, b_yo = banks(g)
                    gs = slice(g * 256, (g + 1) * 256)
                    tt("dve", ya[:, gs].rearrange("p (h d) -> p h d", h=4), yo_ps.rearrange("p (h d) -> p h d", h=4),
                       eacs[:, 4 * g:4 * g + 4].unsqueeze(2).to_broadcast([128, 4, 64]), ALU.mult, [b_yo, bsm], [b_ya[g]])
                    tt("dve", ya[:, gs], ya[:, gs], yd, ALU.add, [b_ya[g], byd], [b_ya[g]])
                    tt("dve", state[:, g, :].rearrange("p (h d) -> p h d", h=4),
                       state[:, g, :].rearrange("p (h d) -> p h d", h=4),
                       cdec[:, 4 * g:4 * g + 4].unsqueeze(2).to_broadcast([128, 4, 64]), ALU.mult, [st_b[g], bsm], [st_b[g]])
                    tt("dve", state[:, g, :], state[:, g, :], st_ps, ALU.add, [st_b[g], b_stp], [st_b[g]])
                    cp("pool", stateb[:, g, :], state[:, g, :], [st_b[g]], [stb_b[g]])
                    tt("dve", ya[:, gs], ya[:, gs], xD[:, gs], ALU.add, [b_ya[g], b_xD], [b_ya[g]])
                    tt("dve", ya[:, gs], ya[:, gs], z_[:, gs], ALU.mult, [b_ya[g], bz], [b_ya[g]])
                    act(junk2, ya[:, gs], AF.Square, [b_ya[g], bsm], [b_j2, bsm], accum_out=ssq[:, g:g + 1])
                    tt("pool", ya[:, gs], ya[:, gs], nw_bc[:, gs], ALU.mult, [b_ya[g], b_vec], [b_ya[g]])

                memset("dve", ssq, 0.0, [bsm])
                pa = preA(0)
                mt = preB(0, *pa)
                for g in range(8):
                    if g + 1 < 8:
                        pa = preA(g + 1)
                    post(g, *mt)
                    post_ew(g)
                    if g + 1 < 8:
                        mt = preB(g + 1, *pa)
                if nxt_hb is not None:
                    norm_block.tr(nxt_hb[0], nxt_hb[1], hT, hTb[j], j * 128)
                if stop_ <= 5:
                    return
                act(lnv, ssq, AF.Ln, [bsm], [bsm], bias=EPS, scale=1.0 / 256)
                act(rstd, lnv, AF.Exp, [bsm], [bsm], scale=-0.5)
                for g in range(8):
                    gs = slice(g * 256, (g + 1) * 256)
                    ts("dve", gn[:, gs], ya[:, gs], rstd[:, g:g + 1], ALU.mult,
                       [b_ya[g], bsm], [b_gn_h[g // 4]])
                for hf_ in range(2):
                    pbank, bbank = pc[1 + hf_], b_pc[1 + hf_]
                    pv = pbank[:].bitcast(BF16)
                    for k in range(8):
                        trn(pv[:, k * 128:(k + 1) * 128], gn[:, (hf_ * 8 + k) * 128:(hf_ * 8 + k + 1) * 128],
                            [b_gn_h[hf_], b_const], [bbank])
                    act(gnT[:, hf_ * 8:(hf_ + 1) * 8, :], pv.rearrange("p (k t) -> p k t", k=8), AF.Copy, [bbank],
                        [b_gnT[hf_]])
                xr_, bxr = xres.next()
                xo_, bxo = xo.next()
                dma("sync", xr_, src_rows(blk), "xres0", [src_bufs[blk]], [bxr])
                for hh in range(2):
                    bank, bb = half()
                    for kk in range(16):
                        mm(bank, gnT[:, kk, :], Wout[:, kk, hh * 512:(hh + 1) * 512], kk == 0, kk == 15,
                           b_gnT + [b_wout], [bb])
                    cs = slice(hh * 512, (hh + 1) * 512)
                    tt("dve", xo_[:, cs], bank, mod_bc[:, 2 * D + hh * 512:2 * D + (hh + 1) * 512], ALU.mult,
                       [bb, b_mod], [bxo])
                    tt("dve", xo_[:, cs], xo_[:, cs], xr_[:, cs], ALU.add, [bxo, bxr], [bxo])
                dma("sync", xr[blk * 128:(blk + 1) * 128, :], xo_, "xo0", [bxo], [xr_b[blk]])

    def final_phase(src_rows, src_bufs):
        new_phase()
        xin = Ring([carve([128, D], F32) for _ in range(2)], "fxin")
        xo = Ring([carve([128, D], F32) for _ in range(2)], "fxo")
        junk = carve([128, D], BF16); bj = Buf("fj")
        sm = Ring([carve([128, 4], F32) for _ in range(2)], "fsm")
        fw = carve([128, D], F32); b_fw = Buf("fw")
        dma("sync", fw, di["final_norm_w"][0, :].partition_broadcast(128), "fw", [], [b_fw])
        for blk in range(NTc * 4):
            x_, bx = xin.next()
            o_, bo = xo.next()
            s_, bs = sm.next()
            dma("sync", x_, src_rows(blk), "fxin%d" % (blk % 2), [src_bufs[blk]], [bx])
            if do_final:
                memset("dve", s_[:, 0:1], 0.0, [bs])
                act(junk, x_, AF.Square, [bx, bs], [bj, bs], accum_out=s_[:, 0:1])
                act(s_[:, 1:2], s_[:, 0:1], AF.Ln, [bs], [bs], bias=EPS, scale=1.0 / D)
                act(s_[:, 2:3], s_[:, 1:2], AF.Exp, [bs], [bs], scale=-0.5)
                stt("dve", o_, x_, s_[:, 2:3], fw, ALU.mult, ALU.mult, [bx, bs, b_fw], [bo])
            else:
                cp("dve", o_, x_, [bx], [bo])
            dma("sync", y_d[blk * 128:(blk + 1) * 128, :], o_, "fxo%d" % (blk % 2), [bo], [y_b[blk]])

    src = lambda blk: di["x"][blk * 128:(blk + 1) * 128, :]
    src_b = [Buf("xsrc%d" % i) for i in range(NBLK)]
    xr_rows = lambda blk: xr[blk * 128:(blk + 1) * 128, :]
    for l in layers:
        ada_phase(l)
        if do_mixer:
            if l % 2 == 0:
                even_phase(l, src, src_b)
            else:
                ssd_phase(l, src, src_b)
            src, src_b = xr_rows, xr_b
        if do_moe:
            moe_phase(l, src, src_b)
            src, src_b = xr_rows, xr_b
    bg_flush()
    final_phase(src, src_b)
    P.barrier()
    P.emit()
    return nc


_CACHE = {}


def kernel(**inputs):
    cfg = inputs.pop("_cfg", None) or {}
    ncores = cfg.get("ncores", 8)
    key = repr(sorted(cfg.items()))
    if key not in _CACHE:
        _CACHE[key] = build(cfg)
    nc = _CACHE[key]
    shared = {}
    for name, shp in INPUT_SHAPES:
        if name in ("x", "c"):
            continue
        shared[name] = np.ascontiguousarray(np.asarray(inputs[name], dtype=np.float32).reshape(shp))
    x = np.asarray(inputs["x"], dtype=np.float32)
    c = np.asarray(inputs["c"], dtype=np.float32)
    in_maps = []
    for b in range(ncores):
        m = dict(shared)
        m["x"] = np.ascontiguousarray(x[b])
        m["c"] = np.ascontiguousarray(c[b:b + 1])
        in_maps.append(m)
    res = run_bass_kernel_spmd(nc, in_maps, core_ids=list(range(ncores)))
    out = np.stack([np.asarray(res.results[b]["y"], dtype=np.float32).reshape(S, D) for b in range(ncores)], axis=0)
    return out
```
